# Optimizing a Trainium2 kernel written in Bass

```python
import math
import jax
import jax.numpy as jnp
from jax import lax
import numpy as np

D_MODEL = 4096
BATCH = 2
SEQ = 4096
DEPTH = 2

D_MIX = D_MODEL
D_SSD = D_MIX // 2
D_GMLP = D_MIX - D_SSD
SSD_HEAD_DIM = 64
N_SSD_HEADS = D_SSD // SSD_HEAD_DIM
SSD_GROUPS = 8
D_STATE = 128
D_CONV = 5
SSD_CHUNK = 128
CONV_CH = D_SSD + 2 * SSD_GROUPS * D_STATE
GMLP_CHUNK = 128
GMLP_GROUP_WIDTH = 128
N_GMLP_GROUPS = D_GMLP // GMLP_GROUP_WIDTH
D_IN = D_SSD + CONV_CH + 2 * N_SSD_HEADS + 2 * D_GMLP
MEM_LEN = 256
XATTN_HEADS = 4
XATTN_HEAD_DIM = D_MODEL // XATTN_HEADS
N_EXPERTS = 16
CAPACITY_FACTOR = 2
D_EXPERT = 3 * D_MODEL // 8
RMS_EPS = 1e-6
DT_MIN = 1e-3
DT_MAX = 1e-1

kernel_name = "hybrid_ssd_gmlp_memxattn_ecmoe_encoder"


def rmsnorm(x, g):
    x32 = x.astype(jnp.float32)
    y = x32 * lax.rsqrt(jnp.mean(x32 * x32, axis=-1, keepdims=True) + RMS_EPS)
    return (y * g.astype(jnp.float32)).astype(x.dtype)


def depthwise_conv_centred(x, w, b):
    c = x.shape[-1]
    y = lax.conv_general_dilated(
        x, w[:, None, :].astype(x.dtype), window_strides=(1,),
        padding=[(D_CONV // 2, D_CONV // 2)],
        dimension_numbers=('NWC', 'WIO', 'NWC'), feature_group_count=c)
    return y + b.astype(x.dtype)


def ssd_scan(x, dt, a, b_mat, c_mat):
    out_dtype = x.dtype
    bsz, s, h, p = x.shape
    g, n = b_mat.shape[2], b_mat.shape[3]
    r = h // g
    nc = s // SSD_CHUNK
    f32 = jnp.float32
    xc = x.astype(f32).reshape(bsz, nc, SSD_CHUNK, g, r, p)
    dtc = dt.astype(f32).reshape(bsz, nc, SSD_CHUNK, g, r)
    bc = b_mat.astype(f32).reshape(bsz, nc, SSD_CHUNK, g, n)
    cc = c_mat.astype(f32).reshape(bsz, nc, SSD_CHUNK, g, n)
    a_cs = jnp.cumsum(dtc * a.astype(f32).reshape(g, r), axis=2)
    xdt = xc * dtc[..., None]
    lower = jnp.tril(jnp.ones((SSD_CHUNK, SSD_CHUNK), bool))[:, :, None, None]
    seg = a_cs[:, :, :, None] - a_cs[:, :, None, :]
    decay = jnp.exp(jnp.where(lower, seg, -jnp.inf))
    cb = jnp.einsum('bclgn,bcsgn->bclsg', cc, bc)
    y_diag = jnp.einsum('bclsgr,bcsgrp->bclgrp', cb[..., None] * decay, xdt)
    decay_to_end = jnp.exp(a_cs[:, :, -1:] - a_cs)
    chunk_states = jnp.einsum('bclgn,bclgr,bclgrp->bcgrpn', bc, decay_to_end, xdt)
    chunk_decay = jnp.exp(a_cs[:, :, -1])

    def step(state, inp):
        st, dec = inp
        return state * dec[..., None, None] + st, state

    init = jnp.zeros((bsz, g, r, p, n), f32)
    _, prev = lax.scan(step, init, (jnp.moveaxis(chunk_states, 1, 0), jnp.moveaxis(chunk_decay, 1, 0)))
    prev = jnp.moveaxis(prev, 0, 1)
    y_off = jnp.einsum('bclgn,bcgrpn,bclgr->bclgrp', cc, prev, jnp.exp(a_cs))
    return (y_diag + y_off).reshape(bsz, s, h, p).astype(out_dtype)


def hybrid_mixer(h, w_in, conv_w, conv_b, dt_bias, a_log, d_skip, ssd_norm_g,
                 gmlp_norm_g, gmlp_ws, gmlp_bs, w_out):
    bsz, s, _ = h.shape
    proj = jnp.einsum('bsd,de->bse', h, w_in)
    o1 = D_SSD
    o2 = o1 + CONV_CH
    o3 = o2 + 2 * N_SSD_HEADS
    o4 = o3 + D_GMLP
    z, xbc, dt_raw, u, v = proj[..., :o1], proj[..., o1:o2], proj[..., o2:o3], proj[..., o3:o4], proj[..., o4:]

    xbc = jax.nn.silu(depthwise_conv_centred(xbc, conv_w, conv_b))
    xs = xbc[..., :D_SSD].reshape(bsz, s, N_SSD_HEADS, SSD_HEAD_DIM)
    bm = xbc[..., D_SSD:D_SSD + SSD_GROUPS * D_STATE].reshape(bsz, s, SSD_GROUPS, D_STATE)
    cm = xbc[..., D_SSD + SSD_GROUPS * D_STATE:].reshape(bsz, s, SSD_GROUPS, D_STATE)
    dt = jax.nn.softplus(dt_raw.reshape(bsz, s, 2, N_SSD_HEADS).astype(jnp.float32)
                         + dt_bias.astype(jnp.float32))
    a = -jnp.exp(a_log.astype(jnp.float32))
    y_fwd = ssd_scan(xs, dt[:, :, 0], a[0], bm, cm)
    y_bwd = jnp.flip(ssd_scan(jnp.flip(xs, 1), jnp.flip(dt[:, :, 1], 1), a[1],
                              jnp.flip(bm, 1), jnp.flip(cm, 1)), 1)
    y = y_fwd + y_bwd + xs * d_skip[:, None].astype(xs.dtype)
    y = y.reshape(bsz, s, D_SSD) * jax.nn.silu(z)
    y_ssd = rmsnorm(y.reshape(bsz, s, SSD_GROUPS, D_SSD // SSD_GROUPS),
                    ssd_norm_g.reshape(SSD_GROUPS, D_SSD // SSD_GROUPS)).reshape(bsz, s, D_SSD)

    u = jax.nn.gelu(u, approximate=False)
    v = rmsnorm(jax.nn.gelu(v, approximate=False), gmlp_norm_g)
    nc = s // GMLP_CHUNK
    vc = v.reshape(bsz, nc, GMLP_CHUNK, N_GMLP_GROUPS, GMLP_GROUP_WIDTH)
    sp = jnp.einsum('gts,bcsgd->bctgd', gmlp_ws, vc) + gmlp_bs.T[None, None, :, :, None]
    y_gmlp = u * sp.reshape(bsz, s, D_GMLP)

    y_cat = jnp.concatenate([y_ssd, y_gmlp], axis=-1)
    return jnp.einsum('bse,ed->bsd', y_cat, w_out)


def memory_cross_attention(h, mem, w_q, w_kv, w_o):
    bsz, s, _ = h.shape
    m = mem.shape[1]
    q = jnp.einsum('bsd,de->bse', h, w_q).reshape(bsz, s, XATTN_HEADS, XATTN_HEAD_DIM)
    kv = jnp.einsum('bmd,de->bme', mem, w_kv)
    k = kv[..., :D_MODEL].reshape(bsz, m, XATTN_HEADS, XATTN_HEAD_DIM)
    vv = kv[..., D_MODEL:].reshape(bsz, m, XATTN_HEADS, XATTN_HEAD_DIM)
    scores = jnp.einsum('bshd,bmhd->bhsm', q, k).astype(jnp.float32) * (XATTN_HEAD_DIM ** -0.5)
    probs = jax.nn.softmax(scores, axis=-1).astype(vv.dtype)
    o = jnp.einsum('bhsm,bmhd->bshd', probs, vv).reshape(bsz, s, D_MODEL)
    return jnp.einsum('bse,ed->bsd', o, w_o)


def expert_choice_moe(h, w_router, w_gate_up, w_down):
    bsz, s, d = h.shape
    cap = CAPACITY_FACTOR * s // N_EXPERTS
    logits = jnp.einsum('bsd,de->bse', h, w_router).astype(jnp.float32)
    aff = jax.nn.softmax(logits, axis=-1)
    gate, idx = lax.top_k(jnp.swapaxes(aff, 1, 2), cap)
    xs = jax.vmap(lambda hb, ib: hb[ib])(h, idx)
    gu = jnp.einsum('becd,edf->becf', xs, w_gate_up)
    act = jax.nn.silu(gu[..., :D_EXPERT]) * gu[..., D_EXPERT:]
    y = jnp.einsum('becf,efd->becd', act, w_down) * gate[..., None].astype(h.dtype)
    return jax.vmap(lambda yb, ib: jnp.zeros((s, d), yb.dtype).at[ib.reshape(-1)].add(yb.reshape(-1, d)))(y, idx)


def setup_inputs(seed: int = 0) -> dict:
    key = jax.random.key(seed)
    ks = jax.random.split(key, 24)
    L = DEPTH
    f32 = jnp.float32

    def nrm(k, shape, scale):
        return jax.random.normal(k, shape, f32) * scale

    def gain(k, shape):
        return 1.0 + 0.05 * jax.random.normal(k, shape, f32)

    dt0 = jnp.exp(jax.random.uniform(ks[6], (L, 2, N_SSD_HEADS), f32, math.log(DT_MIN), math.log(DT_MAX)))
    dt_bias = dt0 + jnp.log(-jnp.expm1(-dt0))
    a_log = jnp.log(jax.random.uniform(ks[7], (L, 2, N_SSD_HEADS), f32, 1.0, 16.0))
    return {
        'x': nrm(ks[0], (BATCH, SEQ, D_MODEL), 1.0),
        'mem': nrm(ks[1], (BATCH, MEM_LEN, D_MODEL), 1.0),
        'norm_mix_g': gain(ks[2], (L, D_MODEL)),
        'w_in': nrm(ks[3], (L, D_MODEL, D_IN), D_MODEL ** -0.5),
        'conv_w': nrm(ks[4], (L, D_CONV, CONV_CH), D_CONV ** -0.5),
        'conv_b': nrm(ks[5], (L, CONV_CH), 0.01),
        'dt_bias': dt_bias,
        'a_log': a_log,
        'd_skip': gain(ks[8], (L, N_SSD_HEADS)),
        'ssd_norm_g': gain(ks[9], (L, D_SSD)),
        'gmlp_norm_g': gain(ks[10], (L, D_GMLP)),
        'gmlp_ws': nrm(ks[11], (L, N_GMLP_GROUPS, GMLP_CHUNK, GMLP_CHUNK), GMLP_CHUNK ** -0.5),
        'gmlp_bs': 1.0 + nrm(ks[12], (L, N_GMLP_GROUPS, GMLP_CHUNK), 0.1),
        'w_out': nrm(ks[13], (L, D_MIX, D_MODEL), D_MIX ** -0.5),
        'norm_xattn_g': gain(ks[14], (L, D_MODEL)),
        'norm_mem_g': gain(ks[15], (L, D_MODEL)),
        'w_q': nrm(ks[16], (L, D_MODEL, D_MODEL), D_MODEL ** -0.5),
        'w_kv': nrm(ks[17], (L, D_MODEL, 2 * D_MODEL), D_MODEL ** -0.5),
        'w_o': nrm(ks[18], (L, D_MODEL, D_MODEL), D_MODEL ** -0.5),
        'norm_moe_g': gain(ks[19], (L, D_MODEL)),
        'w_router': nrm(ks[20], (L, D_MODEL, N_EXPERTS), D_MODEL ** -0.5),
        'w_gate_up': nrm(ks[21], (L, N_EXPERTS, D_MODEL, 2 * D_EXPERT), D_MODEL ** -0.5),
        'w_down': nrm(ks[22], (L, N_EXPERTS, D_EXPERT, D_MODEL), D_EXPERT ** -0.5),
        'final_norm_g': gain(ks[23], (D_MODEL,)),
    }


def reference(x, mem, norm_mix_g, w_in, conv_w, conv_b, dt_bias, a_log, d_skip,
              ssd_norm_g, gmlp_norm_g, gmlp_ws, gmlp_bs, w_out, norm_xattn_g,
              norm_mem_g, w_q, w_kv, w_o, norm_moe_g, w_router, w_gate_up, w_down,
              final_norm_g):
    for l in range(DEPTH):
        h = rmsnorm(x, norm_mix_g[l])
        x = x + hybrid_mixer(h, w_in[l], conv_w[l], conv_b[l], dt_bias[l], a_log[l],
                             d_skip[l], ssd_norm_g[l], gmlp_norm_g[l], gmlp_ws[l],
                             gmlp_bs[l], w_out[l])
        h = rmsnorm(x, norm_xattn_g[l])
        m = rmsnorm(mem, norm_mem_g[l])
        x = x + memory_cross_attention(h, m, w_q[l], w_kv[l], w_o[l])
        h = rmsnorm(x, norm_moe_g[l])
        x = x + expert_choice_moe(h, w_router[l], w_gate_up[l], w_down[l])
    return rmsnorm(x, final_norm_g)
```

```python
import numpy as np
from contextlib import ExitStack
import concourse.bass as bass
import concourse.mybir as mybir
from concourse.bass_utils import run_bass_kernel_spmd

F32 = mybir.dt.float32
BF16 = mybir.dt.bfloat16
FP8 = mybir.dt.float8e4
I32 = mybir.dt.int32
AF = mybir.ActivationFunctionType
ALU = mybir.AluOpType
AX = mybir.AxisListType
ENGS = ("pe", "act", "dve", "pool", "sp")
EPS = 1e-6


class Res:
    __slots__ = ("name", "w", "r")

    def __init__(self, name):
        self.name = name
        self.w = None
        self.r = []


class Prog:
    def __init__(self, nc):
        self.nc = nc
        self.q = {e: [] for e in ENGS}
        self.cnt = {}
        self.sems = {}
        self.known = {e: {} for e in ENGS}
        self._ctx = []
        self.res = {}
        self.rr = {}
        for e in ("pe", "act", "dve", "pool"):
            self.sems[e] = self._sem("s_" + e)
            self.cnt[e] = 0

    def _sem(self, name):
        cm = self.nc.semaphore(name)
        s = cm.__enter__()
        self._ctx.append(cm)
        return s

    def R(self, *key):
        r = self.res.get(key)
        if r is None:
            r = self.res[key] = Res(str(key))
        return r

    def _waits_for(self, eng, reads, writes):
        need = {}
        for r in reads:
            if r.w is not None and need.get(r.w[0], 0) < r.w[1]:
                need[r.w[0]] = r.w[1]
        for r in writes:
            for t in ([r.w] if r.w is not None else []) + r.r:
                if need.get(t[0], 0) < t[1]:
                    need[t[0]] = t[1]
        out = []
        kn = self.known[eng]
        for k, v in need.items():
            if kn.get(k, 0) >= v:
                continue
            kn[k] = v
            out.append((self.sems[k], v))
        return out

    def op(self, eng, fn, reads=(), writes=(), stream=None):
        waits = self._waits_for(eng, reads, writes)
        if stream is not None:
            npool = {"sp": 40, "pool": 12, "act": 8}[eng]
            idx = self.rr.get(eng, 0) % npool
            self.rr[eng] = self.rr.get(eng, 0) + 1
            key = "d_%s_%d" % (eng, idx)
            if key not in self.sems:
                self.sems[key] = self._sem(key)
                self.cnt[key] = 0
            prev = self.cnt[key]
            if prev > self.known[eng].get(key, 0):
                self.known[eng][key] = prev
                waits.append((self.sems[key], prev))
            inc = 16
        else:
            key = eng
            inc = 1
        self.cnt[key] += inc
        tok = (key, self.cnt[key])
        for r in reads:
            r.r.append(tok)
        for r in writes:
            r.w = tok
            r.r = []
        self.q[eng].append((waits, fn, self.sems[key], inc))

    def barrier(self):
        for e in ENGS:
            wl = []
            kn = self.known[e]
            for k, v in self.cnt.items():
                if v > kn.get(k, 0):
                    kn[k] = v
                    wl.append((self.sems[k], v))
            self.q[e].append((wl, None, None, 0))

    def mark(self, name):
        self.q["pe"].append(([], ("mark", name), None, 0))

    def wait_all(self, eng, resources):
        self.q[eng].append((self._waits_for(eng, resources, []), None, None, 0))

    def run(self):
        nc = self.nc
        with nc.Block() as block:
            def mk(name):
                def body(e):
                    for wl, fn, semh, inc in self.q[name]:
                        for s, v in wl:
                            e.wait_ge(s, v)
                        if isinstance(fn, tuple):
                            MARKS.append((fn[1], PECOUNT[0]))
                        elif fn is not None:
                            fn().then_inc(semh, inc)
                return body
            block.tensor(mk("pe"))
            block.scalar(mk("act"))
            block.vector(mk("dve"))
            block.gpsimd(mk("pool"))
            block.sync(mk("sp"))

    def close(self):
        for cm in reversed(self._ctx):
            cm.__exit__(None, None, None)


class Cfg:
    def __init__(self, D=4096, S=4096, ML=256, XH=4, NE=16, DE=1536, L=2, NG=8, NGG=16):
        self.D, self.S, self.ML, self.XH, self.NE, self.DE, self.L = D, S, ML, XH, NE, DE, L
        self.NG, self.NGG = NG, NGG
        self.NH = 4 * NG
        self.DS = self.NH * 64
        self.DG = NGG * 128
        assert self.DS + self.DG == D
        self.CC = self.DS + 2 * NG * 128
        self.DIN = self.DS + self.CC + 2 * self.NH + 2 * self.DG
        self.KT = D // 128
        self.NT = S // 128
        self.CAP = 2 * S // NE
        self.XD = D // XH


FULL = Cfg()
TAPS = set()
MARKS = []
PECOUNT = [0]
DBG = {}
LAST = {}


def build(C, stop_after=None):
    nc = bass.Bass("TRN2", target_bir_lowering=False)
    P = Prog(nc)
    R = P.R
    D, S, KT, NT, L = C.D, C.S, C.KT, C.NT, C.L
    DS, DG, NH, NG, NGG, CC = C.DS, C.DG, C.NH, C.NG, C.NGG, C.CC
    NB = NG * 128
    TB = min(1024, S)
    NTB = S // TB
    TPB = TB // 128
    SW = 512
    SWA = 256
    TBA = min(512, S)
    NTBA = S // TBA
    TPBA = TBA // 128
    NROW = 4 * NH + 2 * DS + DG
    CTN = CC // 128
    stages = ["mix", "xattn", "moe"]
    nstage = {None: 3, "none": 0, "mix": 1, "xattn": 2, "moe": 3}[stop_after]
    nlayers = L if stop_after is None else (0 if stop_after == "none" else 1)

    def din(name, shape, dt=F32):
        return nc.dram_tensor(name, list(shape), dt, kind="ExternalInput").ap()

    def dscr(name, shape, dt=F32):
        if name in TAPS:
            return nc.dram_tensor(name, list(shape), dt, kind="ExternalOutput").ap()
        return nc.dram_tensor(name, list(shape), dt).ap()

    x_in = din("x", [S, D])
    out = nc.dram_tensor("out", [S, D], F32, kind="ExternalOutput").ap()
    gk = din("gk", [128, (4 * L + 1) * KT])
    consts = din("consts", [128, 6 * 128])
    gfin_d = din("gfin_d", [1, D])
    xres = dscr("xres", [S, D])
    if nlayers > 0:
        w_in = din("w_in", [L, D, C.DIN])
        w_out = din("w_out", [L, D, D])
        convp = din("convp", [128, L * CTN * 6])
        rowp = din("rowp", [L, NROW])
        gws = din("gws", [L, 128, NGG * 128])
        gbs = din("gbs", [L, NGG * 128])
        ZS = dscr("ZS", [S, DS]); XBCT = dscr("XBCT", [CC, S]); DTR = dscr("DTR", [S, 2 * NH])
        UT = dscr("UT", [DG, S]); VG = dscr("VG", [S, DG]); XST = dscr("XST", [S, DS])
        BTOK = dscr("BTOK", [S, NB], BF16); BTd = dscr("BTd", [NB, S], BF16); CTd = dscr("CTd", [NB, S], BF16)
        CSd = dscr("CSd", [2, NT, 128, DS]); DECd = dscr("DECd", [NT, 128, 2 * NH])
        SPd = dscr("SPd", [2, NT, 128, DS], BF16)
        DTd = dscr("DTd", [S, 2 * NH]); DTAd = dscr("DTAd", [S, 2 * NH])
        YCT = dscr("YCT", [D, S], BF16)
    if nlayers > 0 and nstage >= 3:
        NE_, CAP_ = C.NE, C.CAP
        gmoe_row = din("gmoe_row", [L, D]); w_router = din("w_router", [L, D, NE_])
        w_gate_up = din("w_gate_up", [L, NE_, D, 2 * C.DE]); w_down = din("w_down", [L, NE_, C.DE, D])
        iota_in = din("iota_in", [128, CAP_])
        pidx_in = din("pidx_in", [128, 1])
        H3 = dscr("H3", [S, D], BF16); AFF = dscr("AFF", [S, NE_]); AFFT = dscr("AFFT", [NE_, S])
        SELd = dscr("SELd", [128, NT * NE_]); POSd = dscr("POSd", [128, NT * NE_]); AHLd = dscr("AHLd", [128, NT * NE_ * 4], BF16)
        YG = dscr("YG", [NE_ * CAP_, D], BF16); OHT = dscr("OHT", [NT, 128, NE_ * (CAP_ // 128) * 128], FP8)
    if nlayers > 0 and nstage >= 2:
        mem_in = din("mem", [C.ML, D])
        w_q = din("w_q", [L, D, D]); w_kv = din("w_kv", [L, D, 2 * D]); w_o = din("w_o", [L, D, D])
        KTd = dscr("KTd", [D, C.ML], BF16); Vd = dscr("Vd", [C.ML, D], BF16); QT = dscr("QT", [D, S], BF16)
        DBGY = dscr("DBGY", [S, DS]) if "DBGY" in TAPS else None
        DBG2 = dscr("DBG2", [128, 2048]) if "DBG2" in TAPS else None

    es = ExitStack()

    def sb(name, shape, dt=F32):
        return es.enter_context(nc.sbuf_tensor(name, list(shape), dt))

    NBIG = 43000
    BIG = sb("big", [128, NBIG])
    gk_sb = sb("gk_sb", [128, (4 * L + 1) * KT])
    c_sb = sb("c_sb", [128, 6 * 128])
    c_bf = sb("c_bf", [128, 6 * 128], BF16)
    st = sb("stat", [128, 64])
    ident_f, Tf, Uf, Tb, Ub, ones_f = [c_sb[:, i * 128:(i + 1) * 128] for i in range(6)]
    ident_b = c_bf[:, 0:128]
    ones_b = c_bf[:, 640:768]
    ps = [es.enter_context(nc.psum_tensor(f"ps{i}", [128, 512], F32)) for i in range(8)]
    cnt = {}
    arena = {"off": 0}

    def nxt(k, n):
        v = cnt.get(k, 0)
        cnt[k] = v + 1
        return v % n

    def phase():
        import inspect
        P.mark(inspect.stack()[1].function + ":" + str(inspect.stack()[1].lineno))
        P.barrier()
        arena["off"] = 0

    def al(shape, dt=F32):
        n = int(np.prod(shape))
        words = n if dt == F32 else ((n + 3) // 4 if dt == FP8 else (n + 1) // 2)
        words = (words + 1) // 2 * 2
        o = arena["off"]
        assert o + words <= NBIG, (o, words)
        arena["off"] = o + words
        v = BIG[:, o:o + words]
        if dt != F32:
            v = v.bitcast(dt)[:, 0:n]
        else:
            v = v[:, 0:n]
        if len(shape) == 2:
            v = v.rearrange("p (a b) -> p a b", a=shape[0])
        elif len(shape) == 3:
            v = v.rearrange("p (a b c) -> p a b c", a=shape[0], b=shape[1])
        return v

    def DMA(q, out_, in_, reads, writes, stream):
        eng = {"sp": nc.sync, "pool": nc.gpsimd, "act": nc.scalar}[q]
        P.op(q, lambda: eng.dma_start(out=out_, in_=in_), reads=reads, writes=writes, stream=stream)

    def V(fn, reads, writes):
        P.op("dve", fn, reads=reads, writes=writes)

    def A(fn, reads, writes):
        P.op("act", fn, reads=reads, writes=writes)

    def T(fn, reads, writes):
        P.op("pe", fn, reads=reads, writes=writes)

    DMA("sp", gk_sb[:], gk[:, :], [], [R("gk")], "c")
    DMA("sp", c_sb[:], consts[:, :], [], [R("c")], "c")
    V(lambda: nc.vector.tensor_copy(out=c_bf[:], in_=c_sb[:]), [R("c")], [R("c")])

    def rstd_from_ss(col_in, col_out, n, width):
        V(lambda: nc.vector.tensor_scalar(out=st[:, col_out:col_out + n], in0=st[:, col_in:col_in + n], scalar1=1.0 / width,
                                          scalar2=EPS, op0=ALU.mult, op1=ALU.add), [R("st")], [R("st")])
        A(lambda: nc.scalar.activation(out=st[:, col_out:col_out + n], in_=st[:, col_out:col_out + n], func=AF.Sqrt), [R("st")], [R("st")])
        V(lambda: nc.vector.reciprocal(out=st[:, col_out:col_out + n], in_=st[:, col_out:col_out + n]), [R("st")], [R("st")])

    def sumsq(x_ap, junk_ap, col, rx, rjunk):
        A(lambda: nc.scalar.activation(out=junk_ap, in_=x_ap, func=AF.Square, accum_out=st[:, col:col + 1]), [rx], [rjunk, R("st")])

    def norm_T(xt, xs, ri, src_dram, sname, row0, gi, dst, rdst, col0):
        DMA("sp", xt, src_dram[row0:row0 + 128, :], [R(sname, row0 // 128)], [R("xt", ri)], "ld")
        sumsq(xt, xs, 0, R("xt", ri), R("xs", ri))
        rstd_from_ss(0, 1, 1, D)
        V(lambda: nc.vector.tensor_scalar(out=xs, in0=xt, scalar1=st[:, 1:2], scalar2=None, op0=ALU.mult),
          [R("xt", ri), R("st")], [R("xs", ri)])
        for k0 in range(0, KT, 8):
            b = 6 + nxt("psb", 2)
            nk = min(8, KT - k0)
            pb = ps[b][:, :].bitcast(BF16)

            def tr(pb=pb, k0=k0, nk=nk):
                ins = None
                for k in range(nk):
                    ins = nc.tensor.transpose(pb[:, k * 128:(k + 1) * 128], xs[:, (k0 + k) * 128:(k0 + k + 1) * 128], ident_b)
                return ins
            T(tr, [R("xs", ri), R("c")], [R("ps", b)])
            for k in range(nk):
                kt = k0 + k
                V(lambda pb=pb, k=k, kt=kt: nc.vector.tensor_scalar(
                    out=dst[:, kt, col0:col0 + 128], in0=pb[:, k * 128:(k + 1) * 128],
                    scalar1=gk_sb[:, gi * KT + kt:gi * KT + kt + 1], scalar2=None, op0=ALU.mult),
                    [R("ps", b), R("gk")], [rdst])

    def load_w(wsl, wdram_l, c0, width, kts):
        i = nxt("w", 2)
        src = wdram_l[:, c0:c0 + width].rearrange("(kt p) c -> p kt c", p=128)
        DMA("pool", wsl[i][:, 0:kts, 0:width], src, [], [R("wsl", i)], "w")
        return i

    def mm_tok(psum_ap, act, t0, w_ap, c0, width, kts):
        def f():
            ins = None
            for kt in range(kts):
                ins = nc.tensor.matmul(psum_ap, lhsT=act[:, kt, t0:t0 + 128], rhs=w_ap[:, kt, c0:c0 + width],
                                       start=(kt == 0), stop=(kt == kts - 1))
            return ins
        return f

    def mm_feat(psum_ap, act, t0, tw, w_ap, c0, kts):
        def f():
            ins = None
            for kt in range(kts):
                ins = nc.tensor.matmul(psum_ap, lhsT=w_ap[:, kt, c0:c0 + 128], rhs=act[:, kt, t0:t0 + tw],
                                       start=(kt == 0), stop=(kt == kts - 1))
            return ins
        return f

    def proj_residual(act, ract, wsl, ev, tb, wdram_l):
        for c0 in range(0, D, SW):
            wi = load_w(wsl, wdram_l, c0, SW, KT)
            for t in range(TPB):
                tt = tb * TPB + t
                p = nxt("ps", 6)
                T(mm_tok(ps[p][:, 0:SW], act, t * 128, wsl[wi], 0, SW, KT), [ract, R("wsl", wi)], [R("ps", p)])
                e = nxt("ev", 3)
                DMA("sp", ev[e], xres[tt * 128:(tt + 1) * 128, c0:c0 + SW], [R("xres", tt)], [R("ev", e)], "ld")
                V(lambda p=p, e=e: nc.vector.tensor_tensor(out=ev[e], in0=ps[p][:, 0:SW], in1=ev[e], op=ALU.add),
                  [R("ps", p), R("ev", e)], [R("ev", e)])
                DMA("act", xres[tt * 128:(tt + 1) * 128, c0:c0 + SW], ev[e], [R("ev", e)], [R("xres", tt)], "st")

    def mixer(l):
        o1 = DS; o2 = o1 + CC; o3 = o2 + 2 * NH; o4 = o3 + DG
        def _ph0():
            phase()
            xt = [al([D]) for _ in range(2)]
            xs = [al([D], BF16) for _ in range(2)]
            hT = al([KT, TB], BF16)
            wsl = [al([KT, SWA], BF16) for _ in range(2)]
            ev = [al([SW]) for _ in range(3)]
            wl = w_in[l]
            for tb in range(NTB):
                for t in range(TPB):
                    i = nxt("xt", 2)
                    norm_T(xt[i], xs[i], i, xres, "xres", (tb * TPB + t) * 128, 4 * l + 0, hT, R("hT"), t * 128)
                segs = [("z", c, min(SWA, o1 - c)) for c in range(0, o1, SWA)]
                segs += [("xbc", c, min(SWA, o2 - c)) for c in range(o1, o2, SWA)]
                segs += [("dt", o2, 2 * NH)]
                segs += [("u", c, min(SWA, o4 - c)) for c in range(o3, o4, SWA)]
                segs += [("v", c, min(SWA, C.DIN - c)) for c in range(o4, C.DIN, SWA)]
                for kind, c0, w in segs:
                    wi = load_w(wsl, wl, c0, w, KT)
                    if kind in ("z", "dt", "v"):
                        for t in range(TPB):
                            tt = tb * TPB + t
                            p = nxt("ps", 6)
                            T(mm_tok(ps[p][:, 0:w], hT, t * 128, wsl[wi], 0, w, KT), [R("hT"), R("wsl", wi)], [R("ps", p)])
                            e = nxt("ev", 3)
                            if kind == "dt":
                                V(lambda p=p, e=e, w=w: nc.vector.tensor_copy(out=ev[e][:, 0:w], in_=ps[p][:, 0:w]), [R("ps", p)], [R("ev", e)])
                                dst = DTR[tt * 128:(tt + 1) * 128, :]
                            else:
                                fn = AF.Silu if kind == "z" else AF.Gelu
                                A(lambda p=p, e=e, w=w, fn=fn: nc.scalar.activation(out=ev[e][:, 0:w], in_=ps[p][:, 0:w], func=fn),
                                  [R("ps", p)], [R("ev", e)])
                                if kind == "z":
                                    dst = ZS[tt * 128:(tt + 1) * 128, c0:c0 + w]
                                else:
                                    dst = VG[tt * 128:(tt + 1) * 128, c0 - o4:c0 - o4 + w]
                            DMA("sp", dst, ev[e][:, 0:w], [R("ev", e)], [R(kind + "d", tt)], "st")
                    else:
                        for s0 in range(0, w, 128):
                            for h0 in range(0, TB, 512):
                                hw_ = min(512, TB - h0)
                                p = nxt("ps", 6)
                                T(mm_feat(ps[p][:, 0:hw_], hT, h0, hw_, wsl[wi], s0, KT), [R("hT"), R("wsl", wi)], [R("ps", p)])
                                e = nxt("ev", 3)
                                if kind == "xbc":
                                    V(lambda p=p, e=e, hw_=hw_: nc.vector.tensor_copy(out=ev[e][:, 0:hw_], in_=ps[p][:, 0:hw_]), [R("ps", p)], [R("ev", e)])
                                    ch = c0 - o1 + s0
                                    dst = XBCT[ch:ch + 128, tb * TB + h0:tb * TB + h0 + hw_]
                                    rr = R("xbcd", ch // 128)
                                else:
                                    A(lambda p=p, e=e, hw_=hw_: nc.scalar.activation(out=ev[e][:, 0:hw_], in_=ps[p][:, 0:hw_], func=AF.Gelu), [R("ps", p)], [R("ev", e)])
                                    ch = c0 - o3 + s0
                                    dst = UT[ch:ch + 128, tb * TB + h0:tb * TB + h0 + hw_]
                                    rr = R("ud")
                                DMA("sp", dst, ev[e][:, 0:hw_], [R("ev", e)], [rr], "st")
        _ph0()
        if DBG.get('ph', 99) <= 0:
            return
        def _ph1():
            phase()
            cp = al([L * CTN * 6])
            DMA("sp", cp, convp[:, :], [], [R("cp")], "c")
            xpad = [al([S + 4]) for _ in range(2)]
            acc = [al([S]) for _ in range(2)]
            silb = [al([S], BF16) for _ in range(2)]
            trf = al([NT, 128])
            trb = al([NT, 128], BF16)
            for i in range(2):
                V(lambda i=i: nc.vector.memset(xpad[i][:, 0:2], 0.0), [], [R("xpad", i)])
                V(lambda i=i: nc.vector.memset(xpad[i][:, S + 2:S + 4], 0.0), [], [R("xpad", i)])
            for ct in range(CTN):
                i = nxt("xpad", 2)
                DMA("sp", xpad[i][:, 2:S + 2], XBCT[ct * 128:(ct + 1) * 128, :], [R("xbcd", ct)], [R("xpad", i)], "ld")
                cb0 = (l * CTN + ct) * 6
                V(lambda i=i, cb0=cb0: nc.vector.tensor_scalar(out=acc[i], in0=xpad[i][:, 0:S], scalar1=cp[:, cb0:cb0 + 1],
                                                             scalar2=cp[:, cb0 + 5:cb0 + 6], op0=ALU.mult, op1=ALU.add),
                  [R("xpad", i), R("cp")], [R("acc", i)])
                for k in range(1, 5):
                    V(lambda i=i, cb0=cb0, k=k: nc.vector.scalar_tensor_tensor(out=acc[i], in0=xpad[i][:, k:k + S], scalar=cp[:, cb0 + k:cb0 + k + 1],
                                                                            in1=acc[i], op0=ALU.mult, op1=ALU.add),
                      [R("xpad", i), R("cp"), R("acc", i)], [R("acc", i)])
                A(lambda i=i: nc.scalar.activation(out=acc[i], in_=acc[i], func=AF.Silu), [R("acc", i)], [R("acc", i)])
                isx = ct < DS // 128
                isb = (not isx) and ct < (DS + NB) // 128
                if not isx:
                    V(lambda i=i: nc.vector.tensor_copy(out=silb[i], in_=acc[i]), [R("acc", i)], [R("silb", i)])
                    chb = ct * 128 - DS - (0 if isb else NB)
                    DMA("sp", (BTd if isb else CTd)[chb:chb + 128, :], silb[i], [R("silb", i)], [R("btd" if isb else "ctd")], "st")
                if isx or isb:
                    stg = trf if isx else trb
                    rs = R("trf") if isx else R("trb")
                    for t0 in range(0, NT, 4):
                        p = nxt("ps", 6)

                        def tr(p=p, t0=t0, i=i):
                            ins = None
                            for k in range(min(4, NT - t0)):
                                ins = nc.tensor.transpose(ps[p][:, k * 128:(k + 1) * 128], acc[i][:, (t0 + k) * 128:(t0 + k + 1) * 128], ident_f)
                            return ins
                        T(tr, [R("acc", i), R("c")], [R("ps", p)])
                        nk = min(4, NT - t0)
                        V(lambda p=p, t0=t0, nk=nk, stg=stg: nc.vector.tensor_copy(out=stg[:, t0:t0 + nk, :],
                                                                                  in_=ps[p][:, 0:nk * 128].rearrange("p (a b) -> p a b", a=nk)),
                          [R("ps", p)], [rs])
                    if isx:
                        DMA("sp", XST[:, ct * 128:(ct + 1) * 128].rearrange("(t p) c -> p t c", p=128), trf, [rs], [R("xst")], "st")
                    else:
                        cb_ = ct * 128 - DS
                        DMA("sp", BTOK[:, cb_:cb_ + 128].rearrange("(t p) c -> p t c", p=128), trb, [rs], [R("btok")], "st")

        _ph1()
        if DBG.get('ph', 99) <= 1:
            return
        def _ph2():
            phase()
            rp = al([NROW])
            DMA("sp", rp, rowp[l:l + 1, :].partition_broadcast(128), [], [R("rp")], "c")
            dtb_bc = rp[:, 0:2 * NH]
            A_bc = al([2 * NH])
            A(lambda: nc.scalar.activation(out=A_bc, in_=rp[:, 2 * NH:4 * NH], func=AF.Exp), [R("rp")], [R("Abc")])
            V(lambda: nc.vector.tensor_scalar(out=A_bc, in0=A_bc, scalar1=-1.0, scalar2=None, op0=ALU.mult), [R("Abc")], [R("Abc")])
            dsk_bc = rp[:, 4 * NH:4 * NH + DS]
            sng_bc = rp[:, 4 * NH + DS:4 * NH + 2 * DS]
            gng_bc = rp[:, 4 * NH + 2 * DS:NROW]
            dtr = [al([2 * NH]) for _ in range(2)]
            dtt = [al([2 * NH]) for _ in range(2)]
            dta = [al([2 * NH]) for _ in range(2)]
            dte = [al([2 * NH]) for _ in range(2)]
            decb = [al([2 * NH]) for _ in range(2)]
            xsc = [al([NH, 64]) for _ in range(2)]
            bc = [al([NB], BF16) for _ in range(2)]
            xdts = [al([NH, 64], BF16) for _ in range(2)]
            cse = [al([DS]) for _ in range(2)]
            H2 = 2 * NH
            for c in range(NT):
                i = nxt("c1", 2)
                rows = slice(c * 128, (c + 1) * 128)
                DMA("sp", dtr[i], DTR[rows, :], [R("dtd", c)], [R("dtr", i)], "ld")
                DMA("sp", xsc[i], XST[rows, :].rearrange("p (h d) -> p h d", d=64), [R("xst")], [R("xsc", i)], "ld")
                DMA("sp", bc[i], BTOK[rows, :], [R("btok")], [R("bc", i)], "ld")
                V(lambda i=i: nc.vector.tensor_tensor(out=dtr[i], in0=dtr[i], in1=dtb_bc, op=ALU.add), [R("dtr", i), R("rp")], [R("dtr", i)])
                A(lambda i=i: nc.scalar.activation(out=dtr[i], in_=dtr[i], func=AF.Exp), [R("dtr", i)], [R("dtr", i)])
                A(lambda i=i: nc.scalar.activation(out=dtt[i], in_=dtr[i], func=AF.Ln, bias=1.0), [R("dtr", i)], [R("dtt", i)])
                V(lambda i=i: nc.vector.tensor_tensor(out=dta[i], in0=dtt[i], in1=A_bc, op=ALU.mult), [R("dtt", i), R("Abc")], [R("dta", i)])
                DMA("sp", DTd[rows, :], dtt[i], [R("dtt", i)], [R("DTd", c)], "st")
                DMA("sp", DTAd[rows, :], dta[i], [R("dta", i)], [R("DTAd", c)], "st")
                p = nxt("ps", 6)

                def mmx(p=p, i=i):
                    nc.tensor.matmul(ps[p][:, 0:NH], lhsT=Uf, rhs=dta[i][:, 0:NH], start=True, stop=True)
                    nc.tensor.matmul(ps[p][:, NH:H2], lhsT=Ub, rhs=dta[i][:, NH:H2], start=True, stop=True)
                    return nc.tensor.matmul(ps[p][:, 64:64 + H2], lhsT=ones_f, rhs=dta[i], start=True, stop=True)
                T(mmx, [R("dta", i), R("c")], [R("ps", p)])
                A(lambda p=p, i=i: nc.scalar.activation(out=dte[i], in_=ps[p][:, 0:H2], func=AF.Exp), [R("ps", p)], [R("dte", i)])
                A(lambda p=p, i=i: nc.scalar.activation(out=decb[i], in_=ps[p][:, 64:64 + H2], func=AF.Exp), [R("ps", p)], [R("decb", i)])
                DMA("sp", DECd[c], decb[i], [R("decb", i)], [R("DECd", c)], "st")
                V(lambda i=i: nc.vector.tensor_tensor(out=dte[i], in0=dte[i], in1=dtt[i], op=ALU.mult), [R("dte", i), R("dtt", i)], [R("dte", i)])
                for d_ in range(2):
                    j = nxt("xdts", 2)
                    V(lambda i=i, j=j, d_=d_: nc.vector.tensor_tensor(out=xdts[j], in0=xsc[i], in1=dte[i][:, d_ * NH:(d_ + 1) * NH].to_broadcast([128, NH, 64]),
                                                                     op=ALU.mult), [R("xsc", i), R("dte", i)], [R("xdts", j)])
                    for g0 in range(0, NG, 2):
                        p = nxt("ps", 6)

                        def mms(p=p, i=i, j=j, g0=g0):
                            ins = None
                            for g in range(g0, min(g0 + 2, NG)):
                                ins = nc.tensor.matmul(ps[p][:, (g - g0) * 256:(g - g0 + 1) * 256], lhsT=bc[i][:, g * 128:(g + 1) * 128],
                                                       rhs=xdts[j][:, g * 4:(g + 1) * 4, :].rearrange("p h d -> p (h d)"), start=True, stop=True)
                            return ins
                        T(mms, [R("bc", i), R("xdts", j)], [R("ps", p)])
                        ng = min(2, NG - g0)
                        V(lambda p=p, j=j, g0=g0, ng=ng: nc.vector.tensor_copy(out=cse[j][:, g0 * 256:(g0 + ng) * 256], in_=ps[p][:, 0:ng * 256]),
                          [R("ps", p)], [R("cse", j)])
                    DMA("sp", CSd[d_, c], cse[j], [R("cse", j)], [R("CSd", d_, c)], "st")
            state = al([NH, 64])
            csl = [al([NH, 64]) for _ in range(2)]
            decl = [al([2 * NH]) for _ in range(2)]
            spb = [al([DS], BF16) for _ in range(2)]
            for d_ in range(2):
                V(lambda: nc.vector.memset(state, 0.0), [R("state")], [R("state")])
                order = range(NT) if d_ == 0 else range(NT - 1, -1, -1)
                for c in order:
                    i = nxt("rec", 2)
                    DMA("sp", csl[i], CSd[d_, c].rearrange("p (h d) -> p h d", d=64), [R("CSd", d_, c)], [R("csl", i)], "ld")
                    DMA("sp", decl[i], DECd[c], [R("DECd", c)], [R("decl", i)], "ld")
                    V(lambda i=i: nc.vector.tensor_copy(out=spb[i], in_=state.rearrange("p h d -> p (h d)")), [R("state")], [R("spb", i)])
                    DMA("sp", SPd[d_, c], spb[i], [R("spb", i)], [R("SPd", d_, c)], "st")
                    V(lambda i=i, d_=d_: nc.vector.tensor_tensor(out=state, in0=state, in1=decl[i][:, d_ * NH:(d_ + 1) * NH].to_broadcast([128, NH, 64]),
                                                               op=ALU.mult), [R("state"), R("decl", i)], [R("state")])
                    V(lambda i=i: nc.vector.tensor_tensor(out=state, in0=state, in1=csl[i], op=ALU.add), [R("state"), R("csl", i)], [R("state")])

        _ph2()
        if DBG.get('ph', 99) <= 2:
            return
        def _ph3():
            phase()
            rp2 = al([2 * DS])
            DMA("sp", rp2, rowp[l:l + 1, 4 * NH:4 * NH + 2 * DS].partition_broadcast(128), [], [R("rp")], "c")
            dsk_bc = rp2[:, 0:DS]
            sng_bc = rp2[:, DS:2 * DS]
            dtt = [al([2 * NH]) for _ in range(2)]
            dta = [al([2 * NH]) for _ in range(2)]
            xsc = [al([NH, 64]) for _ in range(2)]
            zsc = [al([DS]) for _ in range(2)]
            btc = [al([NG, 128], BF16) for _ in range(2)]
            ctc = [al([NG, 128], BF16) for _ in range(2)]
            spc = [[al([DS], BF16) for _ in range(2)] for _ in range(2)]
            xdt = [[al([NH, 64], BF16) for _ in range(2)] for _ in range(2)]
            xd = [al([DS]) for _ in range(2)]
            ysb = [al([DS]) for _ in range(2)]
            ybf = al([DS], BF16)
            ytr = al([DS // 128, 128], BF16)
            Rt = [[al([4, 128]) for _ in range(2)] for _ in range(2)]
            dcy = [[al([4, 128]) for _ in range(2)] for _ in range(2)]
            scl = [[al([4, 128]) for _ in range(2)] for _ in range(2)]
            Mb = [[al([4, 128], BF16) for _ in range(2)] for _ in range(2)]
            Cs = [[al([4, 128], BF16) for _ in range(2)] for _ in range(2)]
            cbm = [al([2, 128]) for _ in range(2)]
            Tm = [Tf, Tb]
            Um = [Uf, Ub]

            def s1(i, g, q):
                for d_ in range(2):
                    for r in range(4):
                        hcol = d_ * NH + g * 4 + r
                        if (r + d_) % 3 == 0:
                            P.op("pool", lambda i=i, q=q, r=r, hcol=hcol, d_=d_: nc.gpsimd.tensor_scalar(out=Rt[q][d_][:, r, :], in0=Tm[d_], scalar1=dta[i][:, hcol:hcol + 1],
                                                                                                      scalar2=0.0, op0=ALU.mult, op1=ALU.add),
                                 reads=[R("dta", i), R("c")], writes=[R("Rt", q, d_, r)])
                        else:
                            V(lambda i=i, q=q, r=r, hcol=hcol, d_=d_: nc.vector.tensor_scalar(out=Rt[q][d_][:, r, :], in0=Tm[d_], scalar1=dta[i][:, hcol:hcol + 1],
                                                                                           scalar2=None, op0=ALU.mult),
                              [R("dta", i), R("c")], [R("Rt", q, d_, r)])
                pp = []
                for d_ in range(2):
                    p1 = nxt("ps", 6)
                    p2 = nxt("ps", 6)
                    pp.append((p1, p2))

                    def mseg(p1=p1, p2=p2, q=q, d_=d_):
                        ins = None
                        for r in range(4):
                            nc.tensor.matmul(ps[p1][:, r * 128:(r + 1) * 128], lhsT=Um[d_], rhs=Rt[q][d_][:, r, :], start=True, stop=True)
                            ins = nc.tensor.matmul(ps[p2][:, r * 128:(r + 1) * 128], lhsT=ones_f, rhs=Rt[q][d_][:, r, :], start=True, stop=True)
                        return ins
                    T(mseg, [R("Rt", q, d_, 0), R("Rt", q, d_, 1), R("Rt", q, d_, 2), R("Rt", q, d_, 3), R("c")], [R("ps", p1), R("ps", p2)])
                for d_ in range(2):
                    p1, p2 = pp[d_]
                    A(lambda p1=p1, q=q, d_=d_: nc.scalar.activation(out=dcy[q][d_].rearrange("p a b -> p (a b)"), in_=ps[p1][:, :], func=AF.Exp),
                      [R("ps", p1)], [R("dcy", q, d_)])
                    A(lambda p2=p2, q=q, d_=d_: nc.scalar.activation(out=scl[q][d_].rearrange("p a b -> p (a b)"), in_=ps[p2][:, :], func=AF.Exp),
                      [R("ps", p2)], [R("scl", q, d_)])

            def s2(i, g, q):
                pcb = nxt("ps", 6)
                T(lambda pcb=pcb, i=i, g=g: nc.tensor.matmul(ps[pcb][:, 0:128], lhsT=btc[i][:, g, :], rhs=ctc[i][:, g, :], start=True, stop=True),
                  [R("btc", i), R("ctc", i)], [R("ps", pcb)])
                for d_ in range(2):
                    V(lambda pcb=pcb, q=q, d_=d_: nc.vector.tensor_tensor(out=cbm[q][:, d_, :], in0=ps[pcb][:, 0:128], in1=Tm[d_], op=ALU.mult),
                      [R("ps", pcb), R("c")], [R("cbm", q)])
                for d_ in range(2):
                    for r in range(4):
                        V(lambda q=q, r=r, d_=d_: nc.vector.tensor_tensor(out=Mb[q][d_][:, r, :], in0=dcy[q][d_][:, r, :], in1=cbm[q][:, d_, :], op=ALU.mult),
                          [R("dcy", q, d_), R("cbm", q)], [R("Mb", q, d_)])
                        V(lambda q=q, r=r, i=i, g=g, d_=d_: nc.vector.tensor_tensor(out=Cs[q][d_][:, r, :], in0=scl[q][d_][:, r, :], in1=ctc[i][:, g, :], op=ALU.mult),
                          [R("scl", q, d_), R("ctc", i)], [R("Cs", q, d_)])
                py = 6 + nxt("py", 2)

                def mmy(py=py, q=q, i=i, g=g):
                    ins = None
                    for r in range(4):
                        h = g * 4 + r
                        for d_ in range(2):
                            nc.tensor.matmul(ps[py][:, r * 64:(r + 1) * 64], lhsT=Mb[q][d_][:, r, :], rhs=xdt[d_][i][:, h, :],
                                             start=(d_ == 0), stop=False)
                            ins = nc.tensor.matmul(ps[py][:, r * 64:(r + 1) * 64], lhsT=Cs[q][d_][:, r, :], rhs=spc[d_][i][:, h * 64:(h + 1) * 64],
                                                   start=False, stop=(d_ == 1))
                    return ins
                T(mmy, [R("Mb", q, 0), R("Cs", q, 0), R("Mb", q, 1), R("Cs", q, 1), R("xdt", 0, i), R("xdt", 1, i), R("spc", 0, i), R("spc", 1, i)],
                  [R("ps", py)])
                V(lambda py=py, i=i, g=g: nc.vector.tensor_tensor(out=ysb[i][:, g * 256:(g + 1) * 256], in0=ps[py][:, 0:256],
                                                                 in1=xd[i][:, g * 256:(g + 1) * 256], op=ALU.add),
                  [R("ps", py), R("xd", i)], [R("ysb", i)])

            for c in range(NT):
                i = nxt("c2", 2)
                rows = slice(c * 128, (c + 1) * 128)
                cols = slice(c * 128, (c + 1) * 128)
                DMA("sp", dtt[i], DTd[rows, :], [R("DTd", c)], [R("dtt", i)], "ld")
                DMA("sp", dta[i], DTAd[rows, :], [R("DTAd", c)], [R("dta", i)], "ld")
                DMA("sp", xsc[i], XST[rows, :].rearrange("p (h d) -> p h d", d=64), [R("xst")], [R("xsc", i)], "ld")
                DMA("sp", zsc[i], ZS[rows, :], [R("zd", c)], [R("zsc", i)], "ld")
                DMA("sp", btc[i], BTd[:, cols].rearrange("(g n) s -> n g s", n=128), [R("btd")], [R("btc", i)], "ld")
                DMA("sp", ctc[i], CTd[:, cols].rearrange("(g n) s -> n g s", n=128), [R("ctd")], [R("ctc", i)], "ld")
                for d_ in range(2):
                    DMA("sp", spc[d_][i], SPd[d_, c], [R("SPd", d_, c)], [R("spc", d_, i)], "ld")
                s1(i, 0, 0)
                for d_ in range(2):
                    V(lambda i=i, d_=d_: nc.vector.tensor_tensor(out=xdt[d_][i], in0=xsc[i], in1=dtt[i][:, d_ * NH:(d_ + 1) * NH].to_broadcast([128, NH, 64]),
                                                               op=ALU.mult), [R("xsc", i), R("dtt", i)], [R("xdt", d_, i)])
                V(lambda i=i: nc.vector.tensor_tensor(out=xd[i], in0=xsc[i].rearrange("p h d -> p (h d)"), in1=dsk_bc, op=ALU.mult),
                  [R("xsc", i), R("rp")], [R("xd", i)])
                for g in range(NG):
                    if g + 1 < NG:
                        s1(i, g + 1, (g + 1) % 2)
                    s2(i, g, g % 2)
                if "DBGY" in TAPS:
                    DMA("sp", DBGY[rows, :], ysb[i], [R("ysb", i)], [R("dbgy")], "st")
                V(lambda i=i: nc.vector.tensor_tensor(out=ysb[i], in0=ysb[i], in1=zsc[i], op=ALU.mult), [R("ysb", i), R("zsc", i)], [R("ysb", i)])
                for g in range(NG):
                    sumsq(ysb[i][:, g * 256:(g + 1) * 256], xd[i][:, g * 256:(g + 1) * 256], 8 + g, R("ysb", i), R("xd", i))
                rstd_from_ss(8, 8 + NG, NG, 256)
                for g in range(NG):
                    V(lambda i=i, g=g: nc.vector.scalar_tensor_tensor(out=ybf[:, g * 256:(g + 1) * 256], in0=ysb[i][:, g * 256:(g + 1) * 256],
                                                                     scalar=st[:, 8 + NG + g:8 + NG + g + 1], in1=sng_bc[:, g * 256:(g + 1) * 256],
                                                                     op0=ALU.mult, op1=ALU.mult),
                      [R("ysb", i), R("st"), R("rp")], [R("ybf")])
                for k0 in range(0, DS // 128, 8):
                    b = nxt("ps", 6)
                    pb = ps[b][:, :].bitcast(BF16)
                    nk = min(8, DS // 128 - k0)

                    def tr2(pb=pb, k0=k0, nk=nk):
                        ins = None
                        for k in range(nk):
                            ins = nc.tensor.transpose(pb[:, k * 128:(k + 1) * 128], ybf[:, (k0 + k) * 128:(k0 + k + 1) * 128], ident_b)
                        return ins
                    T(tr2, [R("ybf"), R("c")], [R("ps", b)])
                    V(lambda pb=pb, k0=k0, nk=nk: nc.vector.tensor_copy(out=ytr[:, k0:k0 + nk, :], in_=pb[:, 0:nk * 128].rearrange("p (a b) -> p a b", a=nk)),
                      [R("ps", b)], [R("ytr")])
                DMA("sp", YCT[0:DS, cols].rearrange("(k p) t -> p k t", p=128), ytr, [R("ytr")], [R("yct")], "st")
        _ph3()
        if DBG.get('ph', 99) <= 3:
            return
        def _ph4():
            phase()
            rp = al([NROW])
            DMA("sp", rp, rowp[l:l + 1, :].partition_broadcast(128), [], [R("rp")], "c")
            gng_bc = rp[:, 4 * NH + 2 * DS:NROW]
            wsT = al([NGG, 128], BF16)
            bsr = al([NGG * 128], BF16)
            DMA("pool", wsT, gws[l].rearrange("s (g t) -> s g t", g=NGG), [], [R("wsT")], "w")
            DMA("pool", bsr[0:1, :], gbs[l:l + 1, :], [], [R("bsr")], "w")
            vg = [al([DG]) for _ in range(2)]
            vj = [al([DG]) for _ in range(2)]
            vb = [al([DG], BF16) for _ in range(2)]
            utc = [al([NGG, 128]) for _ in range(2)]
            ygt = [al([NGG, 128], BF16) for _ in range(2)]
            for c in range(NT):
                i = nxt("gm", 2)
                rows = slice(c * 128, (c + 1) * 128)
                DMA("sp", vg[i], VG[rows, :], [R("vd", c)], [R("vg", i)], "ld")
                DMA("sp", utc[i], UT[:, rows].rearrange("(g d) t -> d g t", d=128), [R("ud")], [R("utc", i)], "ld")
                sumsq(vg[i], vj[i], 0, R("vg", i), R("vj", i))
                rstd_from_ss(0, 1, 1, DG)
                V(lambda i=i: nc.vector.scalar_tensor_tensor(out=vb[i], in0=vg[i], scalar=st[:, 1:2], in1=gng_bc, op0=ALU.mult, op1=ALU.mult),
                  [R("vg", i), R("st"), R("rp")], [R("vb", i)])
                for g0 in range(0, NGG, 4):
                    p = nxt("ps", 6)

                    def mmg(p=p, i=i, g0=g0):
                        ins = None
                        for gg in range(g0, min(g0 + 4, NGG)):
                            o = (gg - g0) * 128
                            nc.tensor.matmul(ps[p][:, o:o + 128], lhsT=vb[i][:, gg * 128:(gg + 1) * 128], rhs=wsT[:, gg, :], start=(gg == g0), stop=False)
                            ins = nc.tensor.matmul(ps[p][:, o:o + 128], lhsT=ones_b[0:1, :], rhs=bsr[0:1, gg * 128:(gg + 1) * 128], start=False,
                                                   stop=(gg == min(g0 + 4, NGG) - 1))
                        return ins
                    T(mmg, [R("vb", i), R("wsT"), R("bsr"), R("c")], [R("ps", p)])
                    ng = min(4, NGG - g0)
                    V(lambda p=p, i=i, g0=g0, ng=ng: nc.vector.tensor_tensor(out=ygt[i][:, g0:g0 + ng, :], in0=ps[p][:, 0:ng * 128].rearrange("p (a b) -> p a b", a=ng),
                                                                            in1=utc[i][:, g0:g0 + ng, :], op=ALU.mult),
                      [R("ps", p), R("utc", i)], [R("ygt", i)])
                DMA("sp", YCT[DS:D, rows].rearrange("(g d) t -> d g t", d=128), ygt[i], [R("ygt", i)], [R("yct")], "st")

        _ph4()
        if DBG.get('ph', 99) <= 4:
            return
        def _ph5():
            phase()
            act = al([KT, TB], BF16)
            wsl = [al([KT, SW], BF16) for _ in range(2)]
            ev = [al([SW]) for _ in range(3)]
            for tb in range(NTB):
                DMA("sp", act, YCT[:, tb * TB:(tb + 1) * TB].rearrange("(k p) t -> p k t", p=128), [R("yct")], [R("act")], "ld")
                proj_residual(act, R("act"), wsl, ev, tb, w_out[l])


        _ph5()
    def xattn(l):
        ML, XH = C.ML, C.XH
        MT = ML // 128
        ET = KT // XH
        scale = float(C.XD) ** -0.5

        def _x1():
            phase()
            xt = [al([D]) for _ in range(2)]
            xs = [al([D], BF16) for _ in range(2)]
            memT = al([KT, ML], BF16)
            wsl = [al([KT, SW], BF16) for _ in range(2)]
            evb = [al([SW], BF16) for _ in range(3)]
            for t in range(MT):
                i = nxt("xt", 2)
                norm_T(xt[i], xs[i], i, mem_in, "mem", t * 128, 4 * l + 2, memT, R("memT"), t * 128)
            for c0 in range(0, D, SW):
                wi = load_w(wsl, w_kv[l], c0, SW, KT)
                for s0 in range(0, SW, 128):
                    p = nxt("ps", 6)
                    T(mm_feat(ps[p][:, 0:ML], memT, 0, ML, wsl[wi], s0, KT), [R("memT"), R("wsl", wi)], [R("ps", p)])
                    e = nxt("evb", 3)
                    V(lambda p=p, e=e: nc.vector.tensor_copy(out=evb[e][:, 0:ML], in_=ps[p][:, 0:ML]), [R("ps", p)], [R("evb", e)])
                    DMA("sp", KTd[c0 + s0:c0 + s0 + 128, :], evb[e][:, 0:ML], [R("evb", e)], [R("ktd")], "st")
            for c0 in range(0, D, SW):
                wi = load_w(wsl, w_kv[l], D + c0, SW, KT)
                for t in range(MT):
                    p = nxt("ps", 6)
                    T(mm_tok(ps[p][:, 0:SW], memT, t * 128, wsl[wi], 0, SW, KT), [R("memT"), R("wsl", wi)], [R("ps", p)])
                    e = nxt("evb", 3)
                    V(lambda p=p, e=e: nc.vector.tensor_copy(out=evb[e], in_=ps[p][:, 0:SW]), [R("ps", p)], [R("evb", e)])
                    DMA("sp", Vd[t * 128:(t + 1) * 128, c0:c0 + SW], evb[e], [R("evb", e)], [R("vdd")], "st")
        _x1()
        if DBG.get('xph', 99) <= 0:
            return

        def _x2a():
            phase()
            xt = [al([D]) for _ in range(2)]
            xs = [al([D], BF16) for _ in range(2)]
            hT = al([KT, TB], BF16)
            wsl = [al([KT, SWA], BF16) for _ in range(2)]
            evb = [al([512], BF16) for _ in range(3)]
            for tb in range(NTB):
                for t in range(TPB):
                    i = nxt("xt", 2)
                    norm_T(xt[i], xs[i], i, xres, "xres", (tb * TPB + t) * 128, 4 * l + 1, hT, R("hT"), t * 128)
                for c0 in range(0, D, SWA):
                    wi = load_w(wsl, w_q[l], c0, SWA, KT)
                    for s0 in range(0, SWA, 128):
                        for h0 in range(0, TB, 512):
                            hw_ = min(512, TB - h0)
                            p = nxt("ps", 6)
                            T(mm_feat(ps[p][:, 0:hw_], hT, h0, hw_, wsl[wi], s0, KT), [R("hT"), R("wsl", wi)], [R("ps", p)])
                            e = nxt("evb", 3)
                            V(lambda p=p, e=e, hw_=hw_: nc.vector.tensor_copy(out=evb[e][:, 0:hw_], in_=ps[p][:, 0:hw_]), [R("ps", p)], [R("evb", e)])
                            DMA("sp", QT[c0 + s0:c0 + s0 + 128, tb * TB + h0:tb * TB + h0 + hw_], evb[e][:, 0:hw_], [R("evb", e)], [R("qtd")], "st")
        _x2a()
        if DBG.get('xph', 99) <= 1:
            return

        def _x2b():
            phase()
            KTs = al([KT, ML], BF16)
            Vs = al([MT, D], BF16)
            DMA("sp", KTs, KTd.rearrange("(k p) m -> p k m", p=128), [R("ktd")], [R("KTs")], "ld")
            DMA("sp", Vs, Vd.rearrange("(m p) d -> p m d", p=128), [R("vdd")], [R("Vs")], "ld")
            qT = [al([KT, TBA], BF16) for _ in range(2)]
            oTb = [al([KT, TBA], BF16) for _ in range(2)]
            pT = [al([MT, TBA], BF16) for _ in range(2)]
            pr = [al([ML]) for _ in range(2)]
            pb = [al([ML], BF16) for _ in range(2)]
            for tb in range(NTBA):
                qi = nxt("qT", 2)
                DMA("sp", qT[qi], QT[:, tb * TBA:(tb + 1) * TBA].rearrange("(k p) t -> p k t", p=128), [R("qtd")], [R("qT", qi)], "ld")
                for hd in range(XH):
                    pi = nxt("pT", 2)
                    for t in range(TPBA):
                        p = nxt("ps", 6)

                        def mms(p=p, qi=qi, hd=hd, t=t):
                            ins = None
                            for e in range(ET):
                                k = hd * ET + e
                                ins = nc.tensor.matmul(ps[p][:, 0:ML], lhsT=qT[qi][:, k, t * 128:(t + 1) * 128], rhs=KTs[:, k, :],
                                                       start=(e == 0), stop=(e == ET - 1))
                            return ins
                        T(mms, [R("qT", qi), R("KTs")], [R("ps", p)])
                        k2 = nxt("pr", 2)
                        V(lambda p=p: nc.vector.tensor_reduce(out=st[:, 32:33], in_=ps[p][:, 0:ML], axis=AX.X, op=ALU.max), [R("ps", p)], [R("st")])
                        V(lambda: nc.vector.tensor_scalar(out=st[:, 33:34], in0=st[:, 32:33], scalar1=-scale, scalar2=None, op0=ALU.mult), [R("st")], [R("st")])
                        A(lambda p=p, k2=k2: nc.scalar.activation(out=pr[k2], in_=ps[p][:, 0:ML], func=AF.Exp, bias=st[:, 33:34], scale=scale,
                                                                 accum_out=st[:, 34:35]), [R("ps", p), R("st")], [R("pr", k2), R("st")])
                        V(lambda: nc.vector.reciprocal(out=st[:, 35:36], in_=st[:, 34:35]), [R("st")], [R("st")])
                        V(lambda k2=k2: nc.vector.tensor_scalar(out=pb[k2], in0=pr[k2], scalar1=st[:, 35:36], scalar2=None, op0=ALU.mult),
                          [R("pr", k2), R("st")], [R("pb", k2)])
                        b = 6 + nxt("psb", 2)
                        pbv = ps[b][:, :].bitcast(BF16)

                        def trp(pbv=pbv, k2=k2):
                            ins = None
                            for m in range(MT):
                                ins = nc.tensor.transpose(pbv[:, m * 128:(m + 1) * 128], pb[k2][:, m * 128:(m + 1) * 128], ident_b)
                            return ins
                        T(trp, [R("pb", k2), R("c")], [R("ps", b)])
                        V(lambda pbv=pbv, pi=pi, t=t: nc.vector.tensor_copy(out=pT[pi][:, :, t * 128:(t + 1) * 128],
                                                                            in_=pbv[:, 0:MT * 128].rearrange("p (a b) -> p a b", a=MT)),
                          [R("ps", b)], [R("pT", pi)])
                    for dv in range(ET):
                        p = nxt("ps", 6)
                        k = hd * ET + dv

                        def mmo(p=p, pi=pi, k=k):
                            ins = None
                            for m in range(MT):
                                ins = nc.tensor.matmul(ps[p][:, 0:TBA], lhsT=Vs[:, m, k * 128:(k + 1) * 128], rhs=pT[pi][:, m, :],
                                                       start=(m == 0), stop=(m == MT - 1))
                            return ins
                        T(mmo, [R("Vs"), R("pT", pi)], [R("ps", p)])
                        V(lambda p=p, qi=qi, k=k: nc.vector.tensor_copy(out=oTb[qi][:, k, :], in_=ps[p][:, 0:TBA]), [R("ps", p)], [R("oTb", qi)])
                DMA("sp", YCT[:, tb * TBA:(tb + 1) * TBA].rearrange("(k p) t -> p k t", p=128), oTb[qi], [R("oTb", qi)], [R("yct")], "st")
        _x2b()
        if DBG.get('xph', 99) <= 2:
            return

        def _x2c():
            phase()
            act = al([KT, TB], BF16)
            wsl = [al([KT, SW], BF16) for _ in range(2)]
            ev = [al([SW]) for _ in range(3)]
            for tb in range(NTB):
                DMA("sp", act, YCT[:, tb * TB:(tb + 1) * TB].rearrange("(k p) t -> p k t", p=128), [R("yct")], [R("act")], "ld")
                proj_residual(act, R("act"), wsl, ev, tb, w_o[l])
        _x2c()


    def moe(l):
        NE, DE, CAP = C.NE, C.DE, C.CAP
        JT = CAP // 128
        FT = DE // 128
        NQ = NE * JT
        Ub_b = c_bf[:, 512:640]

        def _m1():
            phase()
            gbc = al([D])
            DMA("sp", gbc, gmoe_row[l:l + 1, :].partition_broadcast(128), [], [R("gbc")], "c")
            wr = al([KT, NE])
            DMA("sp", wr, w_router[l].rearrange("(k p) e -> p k e", p=128), [], [R("wr")], "c")
            xt = [al([D]) for _ in range(2)]
            h32 = [al([D]) for _ in range(2)]
            hb = [al([D], BF16) for _ in range(2)]
            hT32 = al([KT, 128])
            lg = [al([NE]) for _ in range(2)]
            aft = [al([NE]) for _ in range(2)]
            afs = [al([128]) for _ in range(2)]
            for tt in range(NT):
                i = nxt("xt", 2)
                rows = slice(tt * 128, (tt + 1) * 128)
                DMA("sp", xt[i], xres[rows, :], [R("xres", tt)], [R("xt", i)], "ld")
                sumsq(xt[i], h32[i], 0, R("xt", i), R("h32", i))
                rstd_from_ss(0, 1, 1, D)
                V(lambda i=i: nc.vector.scalar_tensor_tensor(out=h32[i], in0=xt[i], scalar=st[:, 1:2], in1=gbc, op0=ALU.mult, op1=ALU.mult),
                  [R("xt", i), R("st"), R("gbc")], [R("h32", i)])
                A(lambda i=i: nc.scalar.activation(out=hb[i], in_=h32[i], func=AF.Copy), [R("h32", i)], [R("hb", i)])
                DMA("sp", H3[rows, :], hb[i], [R("hb", i)], [R("h3d")], "st")
                for k0 in range(0, KT, 4):
                    p = nxt("ps", 6)
                    nk = min(4, KT - k0)

                    def tr(p=p, k0=k0, nk=nk, i=i):
                        ins = None
                        for k in range(nk):
                            ins = nc.tensor.transpose(ps[p][:, k * 128:(k + 1) * 128], h32[i][:, (k0 + k) * 128:(k0 + k + 1) * 128], ident_f)
                        return ins
                    T(tr, [R("h32", i), R("c")], [R("ps", p)])
                    V(lambda p=p, k0=k0, nk=nk: nc.vector.tensor_copy(out=hT32[:, k0:k0 + nk, :], in_=ps[p][:, 0:nk * 128].rearrange("p (a b) -> p a b", a=nk)),
                      [R("ps", p)], [R("hT32")])
                p = nxt("ps", 6)

                def mml(p=p):
                    ins = None
                    for kt in range(KT):
                        ins = nc.tensor.matmul(ps[p][:, 0:NE], lhsT=hT32[:, kt, :], rhs=wr[:, kt, :], start=(kt == 0), stop=(kt == KT - 1))
                    return ins
                T(mml, [R("hT32"), R("wr")], [R("ps", p)])
                V(lambda p=p: nc.vector.tensor_reduce(out=st[:, 32:33], in_=ps[p][:, 0:NE], axis=AX.X, op=ALU.max), [R("ps", p)], [R("st")])
                V(lambda: nc.vector.tensor_scalar(out=st[:, 33:34], in0=st[:, 32:33], scalar1=-1.0, scalar2=None, op0=ALU.mult), [R("st")], [R("st")])
                A(lambda p=p, i=i: nc.scalar.activation(out=lg[i], in_=ps[p][:, 0:NE], func=AF.Exp, bias=st[:, 33:34], scale=1.0, accum_out=st[:, 34:35]),
                  [R("ps", p), R("st")], [R("lg", i), R("st")])
                V(lambda: nc.vector.reciprocal(out=st[:, 35:36], in_=st[:, 34:35]), [R("st")], [R("st")])
                V(lambda i=i: nc.vector.tensor_scalar(out=aft[i], in0=lg[i], scalar1=st[:, 35:36], scalar2=None, op0=ALU.mult),
                  [R("lg", i), R("st")], [R("aft", i)])
                DMA("sp", AFF[rows, :], aft[i], [R("aft", i)], [R("affd")], "st")
                p2 = nxt("ps", 6)
                T(lambda p2=p2, i=i: nc.tensor.transpose(ps[p2][0:NE, 0:128], aft[i], ident_f), [R("aft", i), R("c")], [R("ps", p2)])
                V(lambda p2=p2, i=i: nc.vector.tensor_copy(out=afs[i][0:NE, :], in_=ps[p2][0:NE, 0:128]), [R("ps", p2)], [R("afs", i)])
                DMA("sp", AFFT[:, rows], afs[i][0:NE, :], [R("afs", i)], [R("afftd")], "st")
        _m1()
        if DBG.get('mph', 99) <= 0:
            return

        def _m2():
            phase()
            affall = al([NT, NE])
            DMA("sp", affall, AFF.rearrange("(t p) e -> p t e", p=128), [R("affd")], [R("affall")], "ld")
            rowbc = [al([S]) for _ in range(2)]
            junk = al([S])
            junk2 = al([S])
            naff = al([NT, NE])
            V(lambda: nc.vector.tensor_scalar(out=naff, in0=affall, scalar1=-1.0, scalar2=None, op0=ALU.mult), [R("affall")], [R("naff")])
            rank = al([NT, NE])
            sel = al([NT, NE])
            pos = al([NT, NE])
            selb = al([NT, NE], BF16)
            ahi = al([NT, NE], BF16)
            hif = al([NT, NE])
            ahl = al([NT * NE, 4], BF16)
            pidx = al([1])
            DMA("sp", pidx, pidx_in[:, :], [], [R("pidx")], "c")
            for e in range(NE):
                i = nxt("rowbc", 2)
                DMA("sp", rowbc[i], AFFT[e:e + 1, :].partition_broadcast(128), [R("afftd")], [R("rowbc", i)], "ld")
                for tt in range(NT):
                    if tt % 9 < 5:
                        V(lambda i=i, tt=tt, e=e: nc.vector.tensor_scalar(out=junk, in0=rowbc[i], scalar1=affall[:, tt, e:e + 1], scalar2=None,
                                                                         op0=ALU.is_gt, op1=ALU.add, accum_out=rank[:, tt, e:e + 1]),
                          [R("rowbc", i), R("affall")], [R("junk"), R("rank")])
                    else:
                        A(lambda i=i, tt=tt, e=e: nc.scalar.activation(out=junk2, in_=rowbc[i], func=AF.Sign, bias=naff[:, tt, e:e + 1], scale=1.0,
                                                                      accum_out=rank[:, tt, e:e + 1]),
                          [R("rowbc", i), R("naff")], [R("junk2"), R("rank2")])
            for tt in range(NT):
                if tt % 9 >= 5:
                    V(lambda tt=tt: nc.vector.tensor_scalar(out=rank[:, tt, :], in0=rank[:, tt, :], scalar1=float(S - 1), scalar2=0.5, op0=ALU.add, op1=ALU.mult),
                      [R("rank"), R("rank2")], [R("rank"), R("rank2")])
            V(lambda: nc.vector.tensor_scalar(out=sel, in0=rank, scalar1=float(CAP), scalar2=None, op0=ALU.is_lt), [R("rank"), R("rank2")], [R("sel")])
            V(lambda: nc.vector.tensor_copy(out=selb, in_=sel), [R("sel")], [R("selb")])
            for tt in range(NT):
                p = nxt("ps", 6)

                def mmp(p=p, tt=tt):
                    ins = nc.tensor.matmul(ps[p][:, 0:NE], lhsT=Ub_b, rhs=selb[:, tt, :], start=True, stop=(tt == 0))
                    for t2 in range(tt):
                        ins = nc.tensor.matmul(ps[p][:, 0:NE], lhsT=ones_b, rhs=selb[:, t2, :], start=False, stop=(t2 == tt - 1))
                    return ins
                T(mmp, [R("selb"), R("c")], [R("ps", p)])
                V(lambda p=p, tt=tt: nc.vector.tensor_copy(out=pos[:, tt, :], in_=ps[p][:, 0:NE]), [R("ps", p)], [R("pos")])
            V(lambda: nc.vector.tensor_copy(out=ahi, in_=affall), [R("affall")], [R("ahi")])
            V(lambda: nc.vector.tensor_copy(out=hif, in_=ahi), [R("ahi")], [R("hif")])
            V(lambda: nc.vector.tensor_tensor(out=hif, in0=affall, in1=hif, op=ALU.subtract), [R("affall"), R("hif")], [R("hif")])
            V(lambda: nc.vector.tensor_copy(out=ahl[:, :, 0], in_=ahi.rearrange("p a b -> p (a b)")), [R("ahi")], [R("ahl")])
            V(lambda: nc.vector.tensor_copy(out=ahl[:, :, 1], in_=hif.rearrange("p a b -> p (a b)")), [R("hif")], [R("ahl")])
            for tt in range(NT):
                V(lambda tt=tt: nc.vector.memset(ahl[:, tt * NE:(tt + 1) * NE, 2], float(tt)), [R("ahl")], [R("ahl")])
            V(lambda: nc.vector.tensor_scalar(out=ahl[:, :, 3], in0=hif.rearrange("p a b -> p (a b)"), scalar1=0.0, scalar2=pidx[:, 0:1],
                                              op0=ALU.mult, op1=ALU.add), [R("hif"), R("pidx"), R("ahl")], [R("ahl")])
            DMA("sp", SELd[:, :], sel.rearrange("p a b -> p (a b)"), [R("sel")], [R("seld")], "st")
            DMA("sp", POSd[:, :], pos.rearrange("p a b -> p (a b)"), [R("pos")], [R("posd")], "st")
            DMA("sp", AHLd[:, :], ahl.rearrange("p a b -> p (a b)"), [R("ahl")], [R("ahld")], "st")
        _m2()
        if DBG.get('mph', 99) <= 1:
            return

        phase()
        sel = al([NT * NE]); pos = al([NT * NE]); ahl = al([NT * NE, 4], BF16); iot = al([CAP])
        DMA("sp", sel, SELd[:, :], [R("seld")], [R("sel3")], "ld")
        DMA("sp", pos, POSd[:, :], [R("posd")], [R("pos3")], "ld")
        DMA("sp", ahl, AHLd.rearrange("p (a b) -> p a b", b=4), [R("ahld")], [R("ahl3")], "ld")
        DMA("sp", iot, iota_in[:, :], [], [R("iot")], "c")
        xsT = al([KT, CAP], BF16)
        actT = al([FT, CAP], BF16)
        gl = al([JT])
        gtmp = al([4])
        idxf = al([2])
        GCH = min(1024, D // 2)
        NCH = D // GCH
        H3v = H3.rearrange("s (c g) -> (s c) g", g=GCH)
        idxc = al([NCH])
        idxi = al([JT * NCH]).bitcast(I32)
        xg = al([D], BF16)
        oh = al([NT, CAP], BF16)
        stg = [al([S], FP8) for _ in range(1)]
        bigb = [al([max(NT, KT), SW], BF16) for _ in range(2)]
        h3s = [bigb[i][:, 0:NT, :] for i in range(2)]
        wsl = [bigb[i][:, 0:KT, :] for i in range(2)]
        sg = [al([CAP]) for _ in range(1)]
        ygs = [al([SW], BF16) for _ in range(2)]

        def _m3a(e):
            for tt in range(NT):
                q = tt * NE + e
                V(lambda tt=tt, q=q: nc.vector.tensor_scalar(out=oh[:, tt, :], in0=iot, scalar1=pos[:, q:q + 1], scalar2=sel[:, q:q + 1],
                                                           op0=ALU.is_equal, op1=ALU.mult),
                  [R("iot"), R("pos3"), R("sel3")], [R("oh")])
            for jt in range(JT):
                p = nxt("ps", 6)

                def mmg(p=p, jt=jt):
                    ins = None
                    for tt in range(NT):
                        ins = nc.tensor.matmul(ps[p][:, 0:4], lhsT=oh[:, tt, jt * 128:(jt + 1) * 128], rhs=ahl[:, tt * NE + e, :],
                                               start=(tt == 0), stop=(tt == NT - 1))
                    return ins
                T(mmg, [R("oh"), R("ahl3")], [R("ps", p)])
                V(lambda p=p: nc.vector.tensor_copy(out=gtmp, in_=ps[p][:, 0:4]), [R("ps", p)], [R("gtmp")])
                V(lambda jt=jt: nc.vector.tensor_tensor(out=gl[:, jt:jt + 1], in0=gtmp[:, 0:1], in1=gtmp[:, 1:2], op=ALU.add), [R("gtmp")], [R("gl")])
                V(lambda: nc.vector.scalar_tensor_tensor(out=idxf[:, 0:1], in0=gtmp[:, 2:3], scalar=128.0, in1=gtmp[:, 3:4], op0=ALU.mult, op1=ALU.add),
                  [R("gtmp")], [R("idxf")])
                for c_ in range(NCH):
                    V(lambda c_=c_: nc.vector.tensor_scalar(out=idxc[:, c_:c_ + 1], in0=idxf[:, 0:1], scalar1=float(NCH), scalar2=float(c_),
                                                          op0=ALU.mult, op1=ALU.add), [R("idxf")], [R("idxc")])
                V(lambda jt=jt: nc.vector.tensor_copy(out=idxi[:, jt * NCH:(jt + 1) * NCH], in_=idxc), [R("idxc")], [R("idxi", jt)])
                for c_ in range(NCH):
                    P.op("pool", lambda jt=jt, c_=c_: nc.gpsimd.indirect_dma_start(out=xg[:, c_ * GCH:(c_ + 1) * GCH], out_offset=None, in_=H3v[:, :],
                                                                                 in_offset=bass.IndirectOffsetOnAxis(ap=idxi[:, jt * NCH + c_:jt * NCH + c_ + 1], axis=0),
                                                                                 bounds_check=None, oob_is_err=True),
                         reads=[R("idxi", jt), R("h3d")], writes=[R("xg")], stream="g")
                for k0 in range(0, KT, 8):
                    b2 = 6 + nxt("psb", 2)
                    pbv2 = ps[b2][:, :].bitcast(BF16)
                    nk2 = min(8, KT - k0)

                    def trg(pbv2=pbv2, k0=k0, nk2=nk2):
                        ins = None
                        for k in range(nk2):
                            ins = nc.tensor.transpose(pbv2[:, k * 128:(k + 1) * 128], xg[:, (k0 + k) * 128:(k0 + k + 1) * 128], ident_b)
                        return ins
                    T(trg, [R("xg"), R("c")], [R("ps", b2)])
                    V(lambda pbv2=pbv2, k0=k0, nk2=nk2, jt=jt: nc.vector.tensor_copy(out=xsT[:, k0:k0 + nk2, jt * 128:(jt + 1) * 128],
                                                                                   in_=pbv2[:, 0:nk2 * 128].rearrange("p (a b) -> p a b", a=nk2)),
                      [R("ps", b2)], [R("xsT")])
                si = nxt("stg", 1)
                for t0 in range(0, NT, 8):
                    b = 6 + nxt("psb", 2)
                    pbv = ps[b][:, :].bitcast(BF16)
                    nk = min(8, NT - t0)

                    def tro(pbv=pbv, t0=t0, nk=nk, jt=jt):
                        ins = None
                        for k in range(nk):
                            ins = nc.tensor.transpose(pbv[:, k * 128:(k + 1) * 128], oh[:, t0 + k, jt * 128:(jt + 1) * 128], ident_b)
                        return ins
                    T(tro, [R("oh"), R("c")], [R("ps", b)])
                    V(lambda pbv=pbv, t0=t0, nk=nk, si=si: nc.vector.tensor_copy(out=stg[si][:, t0 * 128:(t0 + nk) * 128], in_=pbv[:, 0:nk * 128], saturate=False),
                      [R("ps", b)], [R("stg", si)])
                qq = e * JT + jt
                DMA("sp", OHT[:, :, qq * 128:(qq + 1) * 128].rearrange("t p c -> p t c"), stg[si].rearrange("p (t c) -> p t c", c=128),
                    [R("stg", si)], [R("ohtd")], "st")

        def _m3b(e):
            wgu = w_gate_up[l, e]
            for f2 in range(DE // 256):
                wi = nxt("w", 2)
                DMA("pool", wsl[wi][:, :, 0:256], wgu[:, f2 * 256:(f2 + 1) * 256].rearrange("(kt p) c -> p kt c", p=128), [], [R("wsl", wi)], "w")
                DMA("pool", wsl[wi][:, :, 256:512], wgu[:, DE + f2 * 256:DE + (f2 + 1) * 256].rearrange("(kt p) c -> p kt c", p=128), [], [R("wsl", wi)], "w")
                for sub in range(2):
                    pg = nxt("ps", 6)
                    pu = nxt("ps", 6)
                    T(mm_feat(ps[pg][:, 0:CAP], xsT, 0, CAP, wsl[wi], sub * 128, KT), [R("xsT"), R("wsl", wi)], [R("ps", pg)])
                    T(mm_feat(ps[pu][:, 0:CAP], xsT, 0, CAP, wsl[wi], 256 + sub * 128, KT), [R("xsT"), R("wsl", wi)], [R("ps", pu)])
                    gi_ = nxt("sg", 1)
                    A(lambda pg=pg, gi_=gi_: nc.scalar.activation(out=sg[gi_], in_=ps[pg][:, 0:CAP], func=AF.Silu), [R("ps", pg)], [R("sg", gi_)])
                    fi = f2 * 2 + sub
                    V(lambda pu=pu, gi_=gi_, fi=fi: nc.vector.tensor_tensor(out=actT[:, fi, :], in0=sg[gi_], in1=ps[pu][:, 0:CAP], op=ALU.mult),
                      [R("sg", gi_), R("ps", pu)], [R("actT")])
            for dblk in range(D // SW):
                wi = load_w(wsl, w_down[l, e], dblk * SW, SW, FT)
                for jt in range(JT):
                    p = nxt("ps", 6)
                    T(mm_tok(ps[p][:, 0:SW], actT, jt * 128, wsl[wi], 0, SW, FT), [R("actT"), R("wsl", wi)], [R("ps", p)])
                    yi = nxt("ygs", 2)
                    V(lambda p=p, yi=yi, jt=jt: nc.vector.tensor_scalar(out=ygs[yi], in0=ps[p][:, 0:SW], scalar1=gl[:, jt:jt + 1], scalar2=None, op0=ALU.mult),
                      [R("ps", p), R("gl")], [R("ygs", yi)])
                    DMA("sp", YG[e * CAP + jt * 128:e * CAP + (jt + 1) * 128, dblk * SW:(dblk + 1) * SW], ygs[yi], [R("ygs", yi)], [R("ygd")], "st")

        for e in range(NE):
            _m3a(e)
            _m3b(e)
        if DBG.get('mph', 99) <= 2:
            return

        def _m4():
            phase()
            ygs = al([NQ, SW], BF16)
            oht = [al([NQ, 128], FP8) for _ in range(2)]
            ev = [al([SW]) for _ in range(3)]
            for dblk in range(D // SW):
                DMA("sp", ygs, YG[:, dblk * SW:(dblk + 1) * SW].rearrange("(q p) c -> p q c", p=128), [R("ygd")], [R("ygs4")], "ld")
                for tt in range(NT):
                    oi = nxt("oht", 2)
                    DMA("sp", oht[oi], OHT[tt].rearrange("p (q t) -> p q t", t=128), [R("ohtd")], [R("oht", oi)], "ld")
                    p = nxt("ps", 6)

                    def mmc(p=p, oi=oi):
                        ins = None
                        for q in range(NQ):
                            ins = nc.tensor.matmul(ps[p][:, 0:SW], lhsT=oht[oi][:, q, :], rhs=ygs[:, q, :], start=(q == 0), stop=(q == NQ - 1))
                        return ins
                    T(mmc, [R("oht", oi), R("ygs4")], [R("ps", p)])
                    ei = nxt("ev", 3)
                    DMA("sp", ev[ei], xres[tt * 128:(tt + 1) * 128, dblk * SW:(dblk + 1) * SW], [R("xres", tt)], [R("ev", ei)], "ld")
                    V(lambda p=p, ei=ei: nc.vector.tensor_tensor(out=ev[ei], in0=ps[p][:, 0:SW], in1=ev[ei], op=ALU.add),
                      [R("ps", p), R("ev", ei)], [R("ev", ei)])
                    DMA("act", xres[tt * 128:(tt + 1) * 128, dblk * SW:(dblk + 1) * SW], ev[ei], [R("ev", ei)], [R("xres", tt)], "st")
        _m4()


    for tt in range(NT):
        DMA("sp", xres[tt * 128:(tt + 1) * 128, :], x_in[tt * 128:(tt + 1) * 128, :], [], [R("xres", tt)], "cp")

    for l in range(nlayers):
        if nstage >= 1:
            mixer(l)
        if nstage >= 2:
            xattn(l)
        if nstage >= 3:
            moe(l)
    phase()
    gfin = al([D])
    xt2 = [al([D]) for _ in range(2)]
    xs2 = [al([D], BF16) for _ in range(2)]
    DMA("sp", gfin, gfin_d[0:1, :].partition_broadcast(128), [], [R("gfin")], "c")
    for tt in range(NT):
        i = nxt("xt", 2)
        DMA("sp", xt2[i], xres[tt * 128:(tt + 1) * 128, :], [R("xres", tt)], [R("xt", i)], "ld")
        sumsq(xt2[i], xs2[i], 0, R("xt", i), R("xs", i))
        rstd_from_ss(0, 1, 1, D)
        V(lambda i=i: nc.vector.scalar_tensor_tensor(out=xt2[i], in0=xt2[i], scalar=st[:, 1:2], in1=gfin, op0=ALU.mult, op1=ALU.mult),
          [R("xt", i), R("st"), R("gfin")], [R("xt", i)])
        DMA("sp", out[tt * 128:(tt + 1) * 128, :], xt2[i], [R("xt", i)], [R("out")], "out")
    P.wait_all("sp", [R("out")])
    P.run()
    es.close()
    P.close()
    return nc


def host_consts():
    i = np.arange(128)
    ident = np.eye(128, dtype=np.float32)
    Tf = (i[:, None] <= i[None, :]).astype(np.float32)
    Uf = (i[:, None] > i[None, :]).astype(np.float32)
    Tb = (i[:, None] >= i[None, :]).astype(np.float32)
    Ub = (i[:, None] < i[None, :]).astype(np.float32)
    ones = np.ones((128, 128), np.float32)
    return np.concatenate([ident, Tf, Uf, Tb, Ub, ones], axis=1)


def host_layout(C, inp, b):
    L, KT = C.L, C.KT
    f = lambda a: np.ascontiguousarray(a, dtype=np.float32)
    gl = []
    for l in range(L):
        for n in ("norm_mix_g", "norm_xattn_g", "norm_mem_g", "norm_moe_g"):
            gl.append(inp[n][l].reshape(KT, 128).T)
    gl.append(inp["final_norm_g"].reshape(KT, 128).T)
    m = {
        "x": f(inp["x"][b]),
        "gk": f(np.concatenate(gl, axis=1)),
        "consts": host_consts(),
        "gfin_d": f(inp["final_norm_g"].reshape(1, -1)),
        "w_in": f(inp["w_in"]), "w_out": f(inp["w_out"]),
        "convp": f(np.concatenate([np.concatenate([inp["conv_w"][l].T, inp["conv_b"][l][:, None]], axis=1)
                                   .reshape(C.CC // 128, 128, 6).transpose(1, 0, 2).reshape(128, -1) for l in range(L)], axis=1)),
        "rowp": f(np.stack([np.concatenate([inp["dt_bias"][l].reshape(-1), inp["a_log"][l].reshape(-1), np.repeat(inp["d_skip"][l], 64),
                                            inp["ssd_norm_g"][l], inp["gmlp_norm_g"][l]]) for l in range(L)], axis=0)),
        "gws": f(np.stack([inp["gmlp_ws"][l].transpose(2, 0, 1).reshape(128, -1) for l in range(L)], axis=0)),
        "gbs": f(inp["gmlp_bs"].reshape(L, -1)),
        "gmoe_row": f(inp["norm_moe_g"]), "w_router": f(inp["w_router"]), "w_gate_up": f(inp["w_gate_up"]), "w_down": f(inp["w_down"]),
        "iota_in": np.ascontiguousarray(np.broadcast_to(np.arange(C.CAP, dtype=np.float32)[None, :], (128, C.CAP))),
        "pidx_in": np.arange(128, dtype=np.float32).reshape(128, 1),
        "mem": f(inp["mem"][b]), "w_q": f(inp["w_q"]), "w_kv": f(inp["w_kv"]), "w_o": f(inp["w_o"]),
        "w_in": f(inp["w_in"]),
        "w_out": f(inp["w_out"]),
    }
    return m


_NC_CACHE = {}


def run(C, inputs, n_cores=2, stop_after=None):
    key = (id(C), stop_after)
    if key not in _NC_CACHE:
        _NC_CACHE[key] = build(C, stop_after)
    nc = _NC_CACHE[key]
    used = set()
    for alloc in nc.allocations:
        try:
            if alloc.kind == "ExternalInput":
                used.add(alloc.memorylocations[0].name)
        except Exception:
            pass
    maps = []
    for b in range(n_cores):
        m = host_layout(C, inputs, b)
        maps.append({k: v for k, v in m.items() if (not used) or k in used})
    res = run_bass_kernel_spmd(nc, maps, core_ids=list(range(n_cores)))
    LAST["res"] = res.results
    return np.stack([res.results[b]["out"] for b in range(n_cores)], axis=0)


def kernel(**inputs):
    inputs = {k: np.asarray(v) for k, v in inputs.items()}
    return run(FULL, inputs, n_cores=2).astype(np.float32)
```

```python
import numpy as np
from contextlib import ExitStack
import concourse.bass as bass
import concourse.mybir as mybir
from concourse.bass_utils import run_bass_kernel_spmd

F32 = mybir.dt.float32
BF16 = mybir.dt.bfloat16
FP8 = mybir.dt.float8e4
I32 = mybir.dt.int32
AF = mybir.ActivationFunctionType
ALU = mybir.AluOpType
AX = mybir.AxisListType
ENGS = ("pe", "act", "dve", "pool", "sp")
EPS = 1e-6


class Res:
    __slots__ = ("name", "w", "r")

    def __init__(self, name):
        self.name = name
        self.w = None
        self.r = []


class Prog:
    def __init__(self, nc):
        self.nc = nc
        self.q = {e: [] for e in ENGS}
        self.cnt = {}
        self.sems = {}
        self.known = {e: {} for e in ENGS}
        self._ctx = []
        self.res = {}
        self.rr = {}
        for e in ("pe", "act", "dve", "pool"):
            self.sems[e] = self._sem("s_" + e)
            self.cnt[e] = 0

    def _sem(self, name):
        cm = self.nc.semaphore(name)
        s = cm.__enter__()
        self._ctx.append(cm)
        return s

    def R(self, *key):
        r = self.res.get(key)
        if r is None:
            r = self.res[key] = Res(str(key))
        return r

    def _waits_for(self, eng, reads, writes):
        need = {}
        for r in reads:
            if r.w is not None and need.get(r.w[0], 0) < r.w[1]:
                need[r.w[0]] = r.w[1]
        for r in writes:
            for t in ([r.w] if r.w is not None else []) + r.r:
                if need.get(t[0], 0) < t[1]:
                    need[t[0]] = t[1]
        out = []
        kn = self.known[eng]
        for k, v in need.items():
            if kn.get(k, 0) >= v:
                continue
            kn[k] = v
            out.append((self.sems[k], v))
        return out

    def op(self, eng, fn, reads=(), writes=(), stream=None):
        waits = self._waits_for(eng, reads, writes)
        if stream is not None:
            npool = {"sp": 40, "pool": 12, "act": 8}[eng]
            idx = self.rr.get(eng, 0) % npool
            self.rr[eng] = self.rr.get(eng, 0) + 1
            key = "d_%s_%d" % (eng, idx)
            if key not in self.sems:
                self.sems[key] = self._sem(key)
                self.cnt[key] = 0
            prev = self.cnt[key]
            if prev > self.known[eng].get(key, 0):
                self.known[eng][key] = prev
                waits.append((self.sems[key], prev))
            inc = 16
        else:
            key = eng
            inc = 1
        self.cnt[key] += inc
        tok = (key, self.cnt[key])
        for r in reads:
            r.r.append(tok)
        for r in writes:
            r.w = tok
            r.r = []
        self.q[eng].append((waits, fn, self.sems[key], inc))

    def barrier(self):
        for e in ENGS:
            wl = []
            kn = self.known[e]
            for k, v in self.cnt.items():
                if v > kn.get(k, 0):
                    kn[k] = v
                    wl.append((self.sems[k], v))
            self.q[e].append((wl, None, None, 0))

    def mark(self, name):
        self.q["pe"].append(([], ("mark", name), None, 0))

    def wait_all(self, eng, resources):
        self.q[eng].append((self._waits_for(eng, resources, []), None, None, 0))

    def run(self):
        nc = self.nc
        with nc.Block() as block:
            def mk(name):
                def body(e):
                    for wl, fn, semh, inc in self.q[name]:
                        for s, v in wl:
                            e.wait_ge(s, v)
                        if isinstance(fn, tuple):
                            MARKS.append((fn[1], PECOUNT[0]))
                        elif fn is not None:
                            fn().then_inc(semh, inc)
                return body
            block.tensor(mk("pe"))
            block.scalar(mk("act"))
            block.vector(mk("dve"))
            block.gpsimd(mk("pool"))
            block.sync(mk("sp"))

    def close(self):
        for cm in reversed(self._ctx):
            cm.__exit__(None, None, None)


class Cfg:
    def __init__(self, D=4096, S=4096, ML=256, XH=4, NE=16, DE=1536, L=2, NG=8, NGG=16):
        self.D, self.S, self.ML, self.XH, self.NE, self.DE, self.L = D, S, ML, XH, NE, DE, L
        self.NG, self.NGG = NG, NGG
        self.NH = 4 * NG
        self.DS = self.NH * 64
        self.DG = NGG * 128
        assert self.DS + self.DG == D
        self.CC = self.DS + 2 * NG * 128
        self.DIN = self.DS + self.CC + 2 * self.NH + 2 * self.DG
        self.KT = D // 128
        self.NT = S // 128
        self.CAP = 2 * S // NE
        self.XD = D // XH


FULL = Cfg()
TAPS = set()
MARKS = []
PECOUNT = [0]
DBG = {}
LAST = {}


def build(C, stop_after=None):
    nc = bass.Bass("TRN2", target_bir_lowering=False)
    P = Prog(nc)
    R = P.R
    D, S, KT, NT, L = C.D, C.S, C.KT, C.NT, C.L
    DS, DG, NH, NG, NGG, CC = C.DS, C.DG, C.NH, C.NG, C.NGG, C.CC
    NB = NG * 128
    TB = min(1024, S)
    NTB = S // TB
    TPB = TB // 128
    SW = 512
    SWA = 256
    TBA = min(512, S)
    NTBA = S // TBA
    TPBA = TBA // 128
    NROW = 4 * NH + 2 * DS + DG
    CTN = CC // 128
    stages = ["mix", "xattn", "moe"]
    nstage = {None: 3, "none": 0, "mix": 1, "xattn": 2, "moe": 3}[stop_after]
    nlayers = L if stop_after is None else (0 if stop_after == "none" else 1)

    def din(name, shape, dt=F32):
        return nc.dram_tensor(name, list(shape), dt, kind="ExternalInput").ap()

    def dscr(name, shape, dt=F32):
        if name in TAPS:
            return nc.dram_tensor(name, list(shape), dt, kind="ExternalOutput").ap()
        return nc.dram_tensor(name, list(shape), dt).ap()

    x_in = din("x", [S, D])
    out = nc.dram_tensor("out", [S, D], F32, kind="ExternalOutput").ap()
    gk = din("gk", [128, (4 * L + 1) * KT])
    consts = din("consts", [128, 6 * 128])
    gfin_d = din("gfin_d", [1, D])
    xres = dscr("xres", [S, D])
    if nlayers > 0:
        w_in = din("w_in", [L, D, C.DIN])
        w_out = din("w_out", [L, D, D])
        convp = din("convp", [128, L * CTN * 6])
        rowp = din("rowp", [L, NROW])
        gws = din("gws", [L, 128, NGG * 128])
        gbs = din("gbs", [L, NGG * 128])
        ZS = dscr("ZS", [S, DS]); XBCT = dscr("XBCT", [CC, S]); DTR = dscr("DTR", [S, 2 * NH])
        UT = dscr("UT", [DG, S]); VG = dscr("VG", [S, DG]); XST = dscr("XST", [S, DS])
        BTOK = dscr("BTOK", [S, NB], BF16); BTd = dscr("BTd", [NB, S], BF16); CTd = dscr("CTd", [NB, S], BF16)
        CSd = dscr("CSd", [2, NT, 128, DS]); DECd = dscr("DECd", [NT, 128, 2 * NH])
        SPd = dscr("SPd", [2, NT, 128, DS], BF16)
        DTd = dscr("DTd", [S, 2 * NH]); DTAd = dscr("DTAd", [S, 2 * NH])
        YCT = dscr("YCT", [D, S], BF16)
    if nlayers > 0 and nstage >= 3:
        NE_, CAP_ = C.NE, C.CAP
        gmoe_row = din("gmoe_row", [L, D]); w_router = din("w_router", [L, D, NE_])
        w_gate_up = din("w_gate_up", [L, NE_, D, 2 * C.DE]); w_down = din("w_down", [L, NE_, C.DE, D])
        iota_in = din("iota_in", [128, CAP_])
        pidx_in = din("pidx_in", [128, 1])
        H3 = dscr("H3", [S, D], BF16); AFF = dscr("AFF", [S, NE_]); AFFT = dscr("AFFT", [NE_, S])
        SELd = dscr("SELd", [128, NT * NE_]); POSd = dscr("POSd", [128, NT * NE_]); AHLd = dscr("AHLd", [128, NT * NE_ * 4], BF16)
        YG = dscr("YG", [NE_ * CAP_, D], BF16); OHT = dscr("OHT", [NT, 128, NE_ * (CAP_ // 128) * 128], FP8)
    if nlayers > 0 and nstage >= 2:
        mem_in = din("mem", [C.ML, D])
        w_q = din("w_q", [L, D, D]); w_kv = din("w_kv", [L, D, 2 * D]); w_o = din("w_o", [L, D, D])
        KTd = dscr("KTd", [D, C.ML], BF16); Vd = dscr("Vd", [C.ML, D], BF16); QT = dscr("QT", [D, S], BF16)
        DBGY = dscr("DBGY", [S, DS]) if "DBGY" in TAPS else None
        DBG2 = dscr("DBG2", [128, 2048]) if "DBG2" in TAPS else None

    es = ExitStack()

    def sb(name, shape, dt=F32):
        return es.enter_context(nc.sbuf_tensor(name, list(shape), dt))

    NBIG = 43000
    BIG = sb("big", [128, NBIG])
    gk_sb = sb("gk_sb", [128, (4 * L + 1) * KT])
    c_sb = sb("c_sb", [128, 6 * 128])
    c_bf = sb("c_bf", [128, 6 * 128], BF16)
    st = sb("stat", [128, 64])
    ident_f, Tf, Uf, Tb, Ub, ones_f = [c_sb[:, i * 128:(i + 1) * 128] for i in range(6)]
    ident_b = c_bf[:, 0:128]
    ones_b = c_bf[:, 640:768]
    ps = [es.enter_context(nc.psum_tensor(f"ps{i}", [128, 512], F32)) for i in range(8)]
    cnt = {}
    arena = {"off": 0}

    def nxt(k, n):
        v = cnt.get(k, 0)
        cnt[k] = v + 1
        return v % n

    def phase():
        import inspect
        P.mark(inspect.stack()[1].function + ":" + str(inspect.stack()[1].lineno))
        P.barrier()
        arena["off"] = 0

    def al(shape, dt=F32):
        n = int(np.prod(shape))
        words = n if dt == F32 else ((n + 3) // 4 if dt == FP8 else (n + 1) // 2)
        words = (words + 1) // 2 * 2
        o = arena["off"]
        assert o + words <= NBIG, (o, words)
        arena["off"] = o + words
        v = BIG[:, o:o + words]
        if dt != F32:
            v = v.bitcast(dt)[:, 0:n]
        else:
            v = v[:, 0:n]
        if len(shape) == 2:
            v = v.rearrange("p (a b) -> p a b", a=shape[0])
        elif len(shape) == 3:
            v = v.rearrange("p (a b c) -> p a b c", a=shape[0], b=shape[1])
        return v

    def DMA(q, out_, in_, reads, writes, stream):
        eng = {"sp": nc.sync, "pool": nc.gpsimd, "act": nc.scalar}[q]
        P.op(q, lambda: eng.dma_start(out=out_, in_=in_), reads=reads, writes=writes, stream=stream)

    def V(fn, reads, writes):
        P.op("dve", fn, reads=reads, writes=writes)

    def A(fn, reads, writes):
        P.op("act", fn, reads=reads, writes=writes)

    def T(fn, reads, writes):
        P.op("pe", fn, reads=reads, writes=writes)

    DMA("sp", gk_sb[:], gk[:, :], [], [R("gk")], "c")
    DMA("sp", c_sb[:], consts[:, :], [], [R("c")], "c")
    V(lambda: nc.vector.tensor_copy(out=c_bf[:], in_=c_sb[:]), [R("c")], [R("c")])

    def rstd_from_ss(col_in, col_out, n, width):
        V(lambda: nc.vector.tensor_scalar(out=st[:, col_out:col_out + n], in0=st[:, col_in:col_in + n], scalar1=1.0 / width,
                                          scalar2=EPS, op0=ALU.mult, op1=ALU.add), [R("st")], [R("st")])
        A(lambda: nc.scalar.activation(out=st[:, col_out:col_out + n], in_=st[:, col_out:col_out + n], func=AF.Sqrt), [R("st")], [R("st")])
        V(lambda: nc.vector.reciprocal(out=st[:, col_out:col_out + n], in_=st[:, col_out:col_out + n]), [R("st")], [R("st")])

    def sumsq(x_ap, junk_ap, col, rx, rjunk):
        A(lambda: nc.scalar.activation(out=junk_ap, in_=x_ap, func=AF.Square, accum_out=st[:, col:col + 1]), [rx], [rjunk, R("st")])

    def norm_T(xt, xs, ri, src_dram, sname, row0, gi, dst, rdst, col0):
        DMA("sp", xt, src_dram[row0:row0 + 128, :], [R(sname, row0 // 128)], [R("xt", ri)], "ld")
        sumsq(xt, xs, 0, R("xt", ri), R("xs", ri))
        rstd_from_ss(0, 1, 1, D)
        V(lambda: nc.vector.tensor_scalar(out=xs, in0=xt, scalar1=st[:, 1:2], scalar2=None, op0=ALU.mult),
          [R("xt", ri), R("st")], [R("xs", ri)])
        for k0 in range(0, KT, 8):
            b = 6 + nxt("psb", 2)
            nk = min(8, KT - k0)
            pb = ps[b][:, :].bitcast(BF16)

            def tr(pb=pb, k0=k0, nk=nk):
                ins = None
                for k in range(nk):
                    ins = nc.tensor.transpose(pb[:, k * 128:(k + 1) * 128], xs[:, (k0 + k) * 128:(k0 + k + 1) * 128], ident_b)
                return ins
            T(tr, [R("xs", ri), R("c")], [R("ps", b)])
            for k in range(nk):
                kt = k0 + k
                V(lambda pb=pb, k=k, kt=kt: nc.vector.tensor_scalar(
                    out=dst[:, kt, col0:col0 + 128], in0=pb[:, k * 128:(k + 1) * 128],
                    scalar1=gk_sb[:, gi * KT + kt:gi * KT + kt + 1], scalar2=None, op0=ALU.mult),
                    [R("ps", b), R("gk")], [rdst])

    def load_w(wsl, wdram_l, c0, width, kts):
        i = nxt("w", 2)
        src = wdram_l[:, c0:c0 + width].rearrange("(kt p) c -> p kt c", p=128)
        DMA("pool", wsl[i][:, 0:kts, 0:width], src, [], [R("wsl", i)], "w")
        return i

    def mm_tok(psum_ap, act, t0, w_ap, c0, width, kts):
        def f():
            ins = None
            for kt in range(kts):
                ins = nc.tensor.matmul(psum_ap, lhsT=act[:, kt, t0:t0 + 128], rhs=w_ap[:, kt, c0:c0 + width],
                                       start=(kt == 0), stop=(kt == kts - 1))
            return ins
        return f

    def mm_feat(psum_ap, act, t0, tw, w_ap, c0, kts):
        def f():
            ins = None
            for kt in range(kts):
                ins = nc.tensor.matmul(psum_ap, lhsT=w_ap[:, kt, c0:c0 + 128], rhs=act[:, kt, t0:t0 + tw],
                                       start=(kt == 0), stop=(kt == kts - 1))
            return ins
        return f

    def proj_residual(act, ract, wsl, ev, tb, wdram_l):
        for c0 in range(0, D, SW):
            wi = load_w(wsl, wdram_l, c0, SW, KT)
            for t in range(TPB):
                tt = tb * TPB + t
                p = nxt("ps", 6)
                T(mm_tok(ps[p][:, 0:SW], act, t * 128, wsl[wi], 0, SW, KT), [ract, R("wsl", wi)], [R("ps", p)])
                e = nxt("ev", 3)
                DMA("sp", ev[e], xres[tt * 128:(tt + 1) * 128, c0:c0 + SW], [R("xres", tt)], [R("ev", e)], "ld")
                V(lambda p=p, e=e: nc.vector.tensor_tensor(out=ev[e], in0=ps[p][:, 0:SW], in1=ev[e], op=ALU.add),
                  [R("ps", p), R("ev", e)], [R("ev", e)])
                DMA("act", xres[tt * 128:(tt + 1) * 128, c0:c0 + SW], ev[e], [R("ev", e)], [R("xres", tt)], "st")

    def mixer(l):
        o1 = DS; o2 = o1 + CC; o3 = o2 + 2 * NH; o4 = o3 + DG
        def _ph0():
            phase()
            xt = [al([D]) for _ in range(2)]
            xs = [al([D], BF16) for _ in range(2)]
            hT = al([KT, TB], BF16)
            wsl = [al([KT, SWA], BF16) for _ in range(2)]
            ev = [al([SW]) for _ in range(3)]
            wl = w_in[l]
            for tb in range(NTB):
                for t in range(TPB):
                    i = nxt("xt", 2)
                    norm_T(xt[i], xs[i], i, xres, "xres", (tb * TPB + t) * 128, 4 * l + 0, hT, R("hT"), t * 128)
                segs = [("z", c, min(SWA, o1 - c)) for c in range(0, o1, SWA)]
                segs += [("xbc", c, min(SWA, o2 - c)) for c in range(o1, o2, SWA)]
                segs += [("dt", o2, 2 * NH)]
                segs += [("u", c, min(SWA, o4 - c)) for c in range(o3, o4, SWA)]
                segs += [("v", c, min(SWA, C.DIN - c)) for c in range(o4, C.DIN, SWA)]
                for kind, c0, w in segs:
                    wi = load_w(wsl, wl, c0, w, KT)
                    if kind in ("z", "dt", "v"):
                        for t in range(TPB):
                            tt = tb * TPB + t
                            p = nxt("ps", 6)
                            T(mm_tok(ps[p][:, 0:w], hT, t * 128, wsl[wi], 0, w, KT), [R("hT"), R("wsl", wi)], [R("ps", p)])
                            e = nxt("ev", 3)
                            if kind == "dt":
                                V(lambda p=p, e=e, w=w: nc.vector.tensor_copy(out=ev[e][:, 0:w], in_=ps[p][:, 0:w]), [R("ps", p)], [R("ev", e)])
                                dst = DTR[tt * 128:(tt + 1) * 128, :]
                            else:
                                fn = AF.Silu if kind == "z" else AF.Gelu
                                A(lambda p=p, e=e, w=w, fn=fn: nc.scalar.activation(out=ev[e][:, 0:w], in_=ps[p][:, 0:w], func=fn),
                                  [R("ps", p)], [R("ev", e)])
                                if kind == "z":
                                    dst = ZS[tt * 128:(tt + 1) * 128, c0:c0 + w]
                                else:
                                    dst = VG[tt * 128:(tt + 1) * 128, c0 - o4:c0 - o4 + w]
                            DMA("sp", dst, ev[e][:, 0:w], [R("ev", e)], [R(kind + "d", tt)], "st")
                    else:
                        for s0 in range(0, w, 128):
                            for h0 in range(0, TB, 512):
                                hw_ = min(512, TB - h0)
                                p = nxt("ps", 6)
                                T(mm_feat(ps[p][:, 0:hw_], hT, h0, hw_, wsl[wi], s0, KT), [R("hT"), R("wsl", wi)], [R("ps", p)])
                                e = nxt("ev", 3)
                                if kind == "xbc":
                                    V(lambda p=p, e=e, hw_=hw_: nc.vector.tensor_copy(out=ev[e][:, 0:hw_], in_=ps[p][:, 0:hw_]), [R("ps", p)], [R("ev", e)])
                                    ch = c0 - o1 + s0
                                    dst = XBCT[ch:ch + 128, tb * TB + h0:tb * TB + h0 + hw_]
                                    rr = R("xbcd", ch // 128)
                                else:
                                    A(lambda p=p, e=e, hw_=hw_: nc.scalar.activation(out=ev[e][:, 0:hw_], in_=ps[p][:, 0:hw_], func=AF.Gelu), [R("ps", p)], [R("ev", e)])
                                    ch = c0 - o3 + s0
                                    dst = UT[ch:ch + 128, tb * TB + h0:tb * TB + h0 + hw_]
                                    rr = R("ud")
                                DMA("sp", dst, ev[e][:, 0:hw_], [R("ev", e)], [rr], "st")
        _ph0()
        if DBG.get('ph', 99) <= 0:
            return
        def _ph1():
            phase()
            cp = al([L * CTN * 6])
            DMA("sp", cp, convp[:, :], [], [R("cp")], "c")
            xpad = [al([S + 4]) for _ in range(2)]
            acc = [al([S]) for _ in range(2)]
            silb = [al([S], BF16) for _ in range(2)]
            trf = al([NT, 128])
            trb = al([NT, 128], BF16)
            for i in range(2):
                V(lambda i=i: nc.vector.memset(xpad[i][:, 0:2], 0.0), [], [R("xpad", i)])
                V(lambda i=i: nc.vector.memset(xpad[i][:, S + 2:S + 4], 0.0), [], [R("xpad", i)])
            for ct in range(CTN):
                i = nxt("xpad", 2)
                DMA("sp", xpad[i][:, 2:S + 2], XBCT[ct * 128:(ct + 1) * 128, :], [R("xbcd", ct)], [R("xpad", i)], "ld")
                cb0 = (l * CTN + ct) * 6
                V(lambda i=i, cb0=cb0: nc.vector.tensor_scalar(out=acc[i], in0=xpad[i][:, 0:S], scalar1=cp[:, cb0:cb0 + 1],
                                                             scalar2=cp[:, cb0 + 5:cb0 + 6], op0=ALU.mult, op1=ALU.add),
                  [R("xpad", i), R("cp")], [R("acc", i)])
                for k in range(1, 5):
                    V(lambda i=i, cb0=cb0, k=k: nc.vector.scalar_tensor_tensor(out=acc[i], in0=xpad[i][:, k:k + S], scalar=cp[:, cb0 + k:cb0 + k + 1],
                                                                            in1=acc[i], op0=ALU.mult, op1=ALU.add),
                      [R("xpad", i), R("cp"), R("acc", i)], [R("acc", i)])
                A(lambda i=i: nc.scalar.activation(out=acc[i], in_=acc[i], func=AF.Silu), [R("acc", i)], [R("acc", i)])
                isx = ct < DS // 128
                isb = (not isx) and ct < (DS + NB) // 128
                if not isx:
                    V(lambda i=i: nc.vector.tensor_copy(out=silb[i], in_=acc[i]), [R("acc", i)], [R("silb", i)])
                    chb = ct * 128 - DS - (0 if isb else NB)
                    DMA("sp", (BTd if isb else CTd)[chb:chb + 128, :], silb[i], [R("silb", i)], [R("btd" if isb else "ctd")], "st")
                if isx or isb:
                    stg = trf if isx else trb
                    rs = R("trf") if isx else R("trb")
                    for t0 in range(0, NT, 4):
                        p = nxt("ps", 6)

                        def tr(p=p, t0=t0, i=i):
                            ins = None
                            for k in range(min(4, NT - t0)):
                                ins = nc.tensor.transpose(ps[p][:, k * 128:(k + 1) * 128], acc[i][:, (t0 + k) * 128:(t0 + k + 1) * 128], ident_f)
                            return ins
                        T(tr, [R("acc", i), R("c")], [R("ps", p)])
                        nk = min(4, NT - t0)
                        V(lambda p=p, t0=t0, nk=nk, stg=stg: nc.vector.tensor_copy(out=stg[:, t0:t0 + nk, :],
                                                                                  in_=ps[p][:, 0:nk * 128].rearrange("p (a b) -> p a b", a=nk)),
                          [R("ps", p)], [rs])
                    if isx:
                        DMA("sp", XST[:, ct * 128:(ct + 1) * 128].rearrange("(t p) c -> p t c", p=128), trf, [rs], [R("xst")], "st")
                    else:
                        cb_ = ct * 128 - DS
                        DMA("sp", BTOK[:, cb_:cb_ + 128].rearrange("(t p) c -> p t c", p=128), trb, [rs], [R("btok")], "st")

        _ph1()
        if DBG.get('ph', 99) <= 1:
            return
        def _ph2():
            phase()
            rp = al([NROW])
            DMA("sp", rp, rowp[l:l + 1, :].partition_broadcast(128), [], [R("rp")], "c")
            dtb_bc = rp[:, 0:2 * NH]
            A_bc = al([2 * NH])
            A(lambda: nc.scalar.activation(out=A_bc, in_=rp[:, 2 * NH:4 * NH], func=AF.Exp), [R("rp")], [R("Abc")])
            V(lambda: nc.vector.tensor_scalar(out=A_bc, in0=A_bc, scalar1=-1.0, scalar2=None, op0=ALU.mult), [R("Abc")], [R("Abc")])
            dsk_bc = rp[:, 4 * NH:4 * NH + DS]
            sng_bc = rp[:, 4 * NH + DS:4 * NH + 2 * DS]
            gng_bc = rp[:, 4 * NH + 2 * DS:NROW]
            dtr = [al([2 * NH]) for _ in range(2)]
            dtt = [al([2 * NH]) for _ in range(2)]
            dta = [al([2 * NH]) for _ in range(2)]
            dte = [al([2 * NH]) for _ in range(2)]
            decb = [al([2 * NH]) for _ in range(2)]
            xsc = [al([NH, 64]) for _ in range(2)]
            bc = [al([NB], BF16) for _ in range(2)]
            xdts = [al([NH, 64], BF16) for _ in range(2)]
            cse = [al([DS]) for _ in range(2)]
            H2 = 2 * NH
            for c in range(NT):
                i = nxt("c1", 2)
                rows = slice(c * 128, (c + 1) * 128)
                DMA("sp", dtr[i], DTR[rows, :], [R("dtd", c)], [R("dtr", i)], "ld")
                DMA("sp", xsc[i], XST[rows, :].rearrange("p (h d) -> p h d", d=64), [R("xst")], [R("xsc", i)], "ld")
                DMA("sp", bc[i], BTOK[rows, :], [R("btok")], [R("bc", i)], "ld")
                V(lambda i=i: nc.vector.tensor_tensor(out=dtr[i], in0=dtr[i], in1=dtb_bc, op=ALU.add), [R("dtr", i), R("rp")], [R("dtr", i)])
                A(lambda i=i: nc.scalar.activation(out=dtr[i], in_=dtr[i], func=AF.Exp), [R("dtr", i)], [R("dtr", i)])
                A(lambda i=i: nc.scalar.activation(out=dtt[i], in_=dtr[i], func=AF.Ln, bias=1.0), [R("dtr", i)], [R("dtt", i)])
                V(lambda i=i: nc.vector.tensor_tensor(out=dta[i], in0=dtt[i], in1=A_bc, op=ALU.mult), [R("dtt", i), R("Abc")], [R("dta", i)])
                DMA("sp", DTd[rows, :], dtt[i], [R("dtt", i)], [R("DTd", c)], "st")
                DMA("sp", DTAd[rows, :], dta[i], [R("dta", i)], [R("DTAd", c)], "st")
                p = nxt("ps", 6)

                def mmx(p=p, i=i):
                    nc.tensor.matmul(ps[p][:, 0:NH], lhsT=Uf, rhs=dta[i][:, 0:NH], start=True, stop=True)
                    nc.tensor.matmul(ps[p][:, NH:H2], lhsT=Ub, rhs=dta[i][:, NH:H2], start=True, stop=True)
                    return nc.tensor.matmul(ps[p][:, 64:64 + H2], lhsT=ones_f, rhs=dta[i], start=True, stop=True)
                T(mmx, [R("dta", i), R("c")], [R("ps", p)])
                A(lambda p=p, i=i: nc.scalar.activation(out=dte[i], in_=ps[p][:, 0:H2], func=AF.Exp), [R("ps", p)], [R("dte", i)])
                A(lambda p=p, i=i: nc.scalar.activation(out=decb[i], in_=ps[p][:, 64:64 + H2], func=AF.Exp), [R("ps", p)], [R("decb", i)])
                DMA("sp", DECd[c], decb[i], [R("decb", i)], [R("DECd", c)], "st")
                V(lambda i=i: nc.vector.tensor_tensor(out=dte[i], in0=dte[i], in1=dtt[i], op=ALU.mult), [R("dte", i), R("dtt", i)], [R("dte", i)])
                for d_ in range(2):
                    j = nxt("xdts", 2)
                    V(lambda i=i, j=j, d_=d_: nc.vector.tensor_tensor(out=xdts[j], in0=xsc[i], in1=dte[i][:, d_ * NH:(d_ + 1) * NH].to_broadcast([128, NH, 64]),
                                                                     op=ALU.mult), [R("xsc", i), R("dte", i)], [R("xdts", j)])
                    for g0 in range(0, NG, 2):
                        p = nxt("ps", 6)

                        def mms(p=p, i=i, j=j, g0=g0):
                            ins = None
                            for g in range(g0, min(g0 + 2, NG)):
                                ins = nc.tensor.matmul(ps[p][:, (g - g0) * 256:(g - g0 + 1) * 256], lhsT=bc[i][:, g * 128:(g + 1) * 128],
                                                       rhs=xdts[j][:, g * 4:(g + 1) * 4, :].rearrange("p h d -> p (h d)"), start=True, stop=True)
                            return ins
                        T(mms, [R("bc", i), R("xdts", j)], [R("ps", p)])
                        ng = min(2, NG - g0)
                        V(lambda p=p, j=j, g0=g0, ng=ng: nc.vector.tensor_copy(out=cse[j][:, g0 * 256:(g0 + ng) * 256], in_=ps[p][:, 0:ng * 256]),
                          [R("ps", p)], [R("cse", j)])
                    DMA("sp", CSd[d_, c], cse[j], [R("cse", j)], [R("CSd", d_, c)], "st")
            state = al([NH, 64])
            csl = [al([NH, 64]) for _ in range(2)]
            decl = [al([2 * NH]) for _ in range(2)]
            spb = [al([DS], BF16) for _ in range(2)]
            for d_ in range(2):
                V(lambda: nc.vector.memset(state, 0.0), [R("state")], [R("state")])
                order = range(NT) if d_ == 0 else range(NT - 1, -1, -1)
                for c in order:
                    i = nxt("rec", 2)
                    DMA("sp", csl[i], CSd[d_, c].rearrange("p (h d) -> p h d", d=64), [R("CSd", d_, c)], [R("csl", i)], "ld")
                    DMA("sp", decl[i], DECd[c], [R("DECd", c)], [R("decl", i)], "ld")
                    V(lambda i=i: nc.vector.tensor_copy(out=spb[i], in_=state.rearrange("p h d -> p (h d)")), [R("state")], [R("spb", i)])
                    DMA("sp", SPd[d_, c], spb[i], [R("spb", i)], [R("SPd", d_, c)], "st")
                    V(lambda i=i, d_=d_: nc.vector.tensor_tensor(out=state, in0=state, in1=decl[i][:, d_ * NH:(d_ + 1) * NH].to_broadcast([128, NH, 64]),
                                                               op=ALU.mult), [R("state"), R("decl", i)], [R("state")])
                    V(lambda i=i: nc.vector.tensor_tensor(out=state, in0=state, in1=csl[i], op=ALU.add), [R("state"), R("csl", i)], [R("state")])

        _ph2()
        if DBG.get('ph', 99) <= 2:
            return
        def _ph3():
            phase()
            rp2 = al([2 * DS])
            DMA("sp", rp2, rowp[l:l + 1, 4 * NH:4 * NH + 2 * DS].partition_broadcast(128), [], [R("rp")], "c")
            dsk_bc = rp2[:, 0:DS]
            sng_bc = rp2[:, DS:2 * DS]
            dtt = [al([2 * NH]) for _ in range(2)]
            dta = [al([2 * NH]) for _ in range(2)]
            xsc = [al([NH, 64]) for _ in range(2)]
            zsc = [al([DS]) for _ in range(2)]
            btc = [al([NG, 128], BF16) for _ in range(2)]
            ctc = [al([NG, 128], BF16) for _ in range(2)]
            spc = [[al([DS], BF16) for _ in range(2)] for _ in range(2)]
            xdt = [[al([NH, 64], BF16) for _ in range(2)] for _ in range(2)]
            xd = [al([DS]) for _ in range(2)]
            ysb = [al([DS]) for _ in range(2)]
            ybf = al([DS], BF16)
            ytr = al([DS // 128, 128], BF16)
            Rt = [[al([4, 128]) for _ in range(2)] for _ in range(2)]
            dcy = [[al([4, 128]) for _ in range(2)] for _ in range(2)]
            scl = [[al([4, 128]) for _ in range(2)] for _ in range(2)]
            Mb = [[al([4, 128], BF16) for _ in range(2)] for _ in range(2)]
            Cs = [[al([4, 128], BF16) for _ in range(2)] for _ in range(2)]
            cbm = [al([2, 128]) for _ in range(2)]
            Tm = [Tf, Tb]
            Um = [Uf, Ub]

            def s1(i, g, q):
                for d_ in range(2):
                    for r in range(4):
                        hcol = d_ * NH + g * 4 + r
                        if (r + d_) % 3 == 0:
                            P.op("pool", lambda i=i, q=q, r=r, hcol=hcol, d_=d_: nc.gpsimd.tensor_scalar(out=Rt[q][d_][:, r, :], in0=Tm[d_], scalar1=dta[i][:, hcol:hcol + 1],
                                                                                                      scalar2=0.0, op0=ALU.mult, op1=ALU.add),
                                 reads=[R("dta", i), R("c")], writes=[R("Rt", q, d_, r)])
                        else:
                            V(lambda i=i, q=q, r=r, hcol=hcol, d_=d_: nc.vector.tensor_scalar(out=Rt[q][d_][:, r, :], in0=Tm[d_], scalar1=dta[i][:, hcol:hcol + 1],
                                                                                           scalar2=None, op0=ALU.mult),
                              [R("dta", i), R("c")], [R("Rt", q, d_, r)])
                pp = []
                for d_ in range(2):
                    p1 = nxt("ps", 6)
                    p2 = nxt("ps", 6)
                    pp.append((p1, p2))

                    def mseg(p1=p1, p2=p2, q=q, d_=d_):
                        ins = None
                        for r in range(4):
                            nc.tensor.matmul(ps[p1][:, r * 128:(r + 1) * 128], lhsT=Um[d_], rhs=Rt[q][d_][:, r, :], start=True, stop=True)
                            ins = nc.tensor.matmul(ps[p2][:, r * 128:(r + 1) * 128], lhsT=ones_f, rhs=Rt[q][d_][:, r, :], start=True, stop=True)
                        return ins
                    T(mseg, [R("Rt", q, d_, 0), R("Rt", q, d_, 1), R("Rt", q, d_, 2), R("Rt", q, d_, 3), R("c")], [R("ps", p1), R("ps", p2)])
                for d_ in range(2):
                    p1, p2 = pp[d_]
                    A(lambda p1=p1, q=q, d_=d_: nc.scalar.activation(out=dcy[q][d_].rearrange("p a b -> p (a b)"), in_=ps[p1][:, :], func=AF.Exp),
                      [R("ps", p1)], [R("dcy", q, d_)])
                    A(lambda p2=p2, q=q, d_=d_: nc.scalar.activation(out=scl[q][d_].rearrange("p a b -> p (a b)"), in_=ps[p2][:, :], func=AF.Exp),
                      [R("ps", p2)], [R("scl", q, d_)])

            def s2(i, g, q):
                pcb = nxt("ps", 6)
                T(lambda pcb=pcb, i=i, g=g: nc.tensor.matmul(ps[pcb][:, 0:128], lhsT=btc[i][:, g, :], rhs=ctc[i][:, g, :], start=True, stop=True),
                  [R("btc", i), R("ctc", i)], [R("ps", pcb)])
                for d_ in range(2):
                    V(lambda pcb=pcb, q=q, d_=d_: nc.vector.tensor_tensor(out=cbm[q][:, d_, :], in0=ps[pcb][:, 0:128], in1=Tm[d_], op=ALU.mult),
                      [R("ps", pcb), R("c")], [R("cbm", q)])
                for d_ in range(2):
                    for r in range(4):
                        V(lambda q=q, r=r, d_=d_: nc.vector.tensor_tensor(out=Mb[q][d_][:, r, :], in0=dcy[q][d_][:, r, :], in1=cbm[q][:, d_, :], op=ALU.mult),
                          [R("dcy", q, d_), R("cbm", q)], [R("Mb", q, d_)])
                        V(lambda q=q, r=r, i=i, g=g, d_=d_: nc.vector.tensor_tensor(out=Cs[q][d_][:, r, :], in0=scl[q][d_][:, r, :], in1=ctc[i][:, g, :], op=ALU.mult),
                          [R("scl", q, d_), R("ctc", i)], [R("Cs", q, d_)])
                py = 6 + nxt("py", 2)

                def mmy(py=py, q=q, i=i, g=g):
                    ins = None
                    for r in range(4):
                        h = g * 4 + r
                        for d_ in range(2):
                            nc.tensor.matmul(ps[py][:, r * 64:(r + 1) * 64], lhsT=Mb[q][d_][:, r, :], rhs=xdt[d_][i][:, h, :],
                                             start=(d_ == 0), stop=False)
                            ins = nc.tensor.matmul(ps[py][:, r * 64:(r + 1) * 64], lhsT=Cs[q][d_][:, r, :], rhs=spc[d_][i][:, h * 64:(h + 1) * 64],
                                                   start=False, stop=(d_ == 1))
                    return ins
                T(mmy, [R("Mb", q, 0), R("Cs", q, 0), R("Mb", q, 1), R("Cs", q, 1), R("xdt", 0, i), R("xdt", 1, i), R("spc", 0, i), R("spc", 1, i)],
                  [R("ps", py)])
                V(lambda py=py, i=i, g=g: nc.vector.tensor_tensor(out=ysb[i][:, g * 256:(g + 1) * 256], in0=ps[py][:, 0:256],
                                                                 in1=xd[i][:, g * 256:(g + 1) * 256], op=ALU.add),
                  [R("ps", py), R("xd", i)], [R("ysb", i)])

            for c in range(NT):
                i = nxt("c2", 2)
                rows = slice(c * 128, (c + 1) * 128)
                cols = slice(c * 128, (c + 1) * 128)
                DMA("sp", dtt[i], DTd[rows, :], [R("DTd", c)], [R("dtt", i)], "ld")
                DMA("sp", dta[i], DTAd[rows, :], [R("DTAd", c)], [R("dta", i)], "ld")
                DMA("sp", xsc[i], XST[rows, :].rearrange("p (h d) -> p h d", d=64), [R("xst")], [R("xsc", i)], "ld")
                DMA("sp", zsc[i], ZS[rows, :], [R("zd", c)], [R("zsc", i)], "ld")
                DMA("sp", btc[i], BTd[:, cols].rearrange("(g n) s -> n g s", n=128), [R("btd")], [R("btc", i)], "ld")
                DMA("sp", ctc[i], CTd[:, cols].rearrange("(g n) s -> n g s", n=128), [R("ctd")], [R("ctc", i)], "ld")
                for d_ in range(2):
                    DMA("sp", spc[d_][i], SPd[d_, c], [R("SPd", d_, c)], [R("spc", d_, i)], "ld")
                s1(i, 0, 0)
                for d_ in range(2):
                    V(lambda i=i, d_=d_: nc.vector.tensor_tensor(out=xdt[d_][i], in0=xsc[i], in1=dtt[i][:, d_ * NH:(d_ + 1) * NH].to_broadcast([128, NH, 64]),
                                                               op=ALU.mult), [R("xsc", i), R("dtt", i)], [R("xdt", d_, i)])
                V(lambda i=i: nc.vector.tensor_tensor(out=xd[i], in0=xsc[i].rearrange("p h d -> p (h d)"), in1=dsk_bc, op=ALU.mult),
                  [R("xsc", i), R("rp")], [R("xd", i)])
                for g in range(NG):
                    if g + 1 < NG:
                        s1(i, g + 1, (g + 1) % 2)
                    s2(i, g, g % 2)
                if "DBGY" in TAPS:
                    DMA("sp", DBGY[rows, :], ysb[i], [R("ysb", i)], [R("dbgy")], "st")
                V(lambda i=i: nc.vector.tensor_tensor(out=ysb[i], in0=ysb[i], in1=zsc[i], op=ALU.mult), [R("ysb", i), R("zsc", i)], [R("ysb", i)])
                for g in range(NG):
                    sumsq(ysb[i][:, g * 256:(g + 1) * 256], xd[i][:, g * 256:(g + 1) * 256], 8 + g, R("ysb", i), R("xd", i))
                rstd_from_ss(8, 8 + NG, NG, 256)
                for g in range(NG):
                    V(lambda i=i, g=g: nc.vector.scalar_tensor_tensor(out=ybf[:, g * 256:(g + 1) * 256], in0=ysb[i][:, g * 256:(g + 1) * 256],
                                                                     scalar=st[:, 8 + NG + g:8 + NG + g + 1], in1=sng_bc[:, g * 256:(g + 1) * 256],
                                                                     op0=ALU.mult, op1=ALU.mult),
                      [R("ysb", i), R("st"), R("rp")], [R("ybf")])
                for k0 in range(0, DS // 128, 8):
                    b = nxt("ps", 6)
                    pb = ps[b][:, :].bitcast(BF16)
                    nk = min(8, DS // 128 - k0)

                    def tr2(pb=pb, k0=k0, nk=nk):
                        ins = None
                        for k in range(nk):
                            ins = nc.tensor.transpose(pb[:, k * 128:(k + 1) * 128], ybf[:, (k0 + k) * 128:(k0 + k + 1) * 128], ident_b)
                        return ins
                    T(tr2, [R("ybf"), R("c")], [R("ps", b)])
                    V(lambda pb=pb, k0=k0, nk=nk: nc.vector.tensor_copy(out=ytr[:, k0:k0 + nk, :], in_=pb[:, 0:nk * 128].rearrange("p (a b) -> p a b", a=nk)),
                      [R("ps", b)], [R("ytr")])
                DMA("sp", YCT[0:DS, cols].rearrange("(k p) t -> p k t", p=128), ytr, [R("ytr")], [R("yct")], "st")
        _ph3()
        if DBG.get('ph', 99) <= 3:
            return
        def _ph4():
            phase()
            rp = al([NROW])
            DMA("sp", rp, rowp[l:l + 1, :].partition_broadcast(128), [], [R("rp")], "c")
            gng_bc = rp[:, 4 * NH + 2 * DS:NROW]
            wsT = al([NGG, 128], BF16)
            bsr = al([NGG * 128], BF16)
            DMA("pool", wsT, gws[l].rearrange("s (g t) -> s g t", g=NGG), [], [R("wsT")], "w")
            DMA("pool", bsr[0:1, :], gbs[l:l + 1, :], [], [R("bsr")], "w")
            vg = [al([DG]) for _ in range(2)]
            vj = [al([DG]) for _ in range(2)]
            vb = [al([DG], BF16) for _ in range(2)]
            utc = [al([NGG, 128]) for _ in range(2)]
            ygt = [al([NGG, 128], BF16) for _ in range(2)]
            for c in range(NT):
                i = nxt("gm", 2)
                rows = slice(c * 128, (c + 1) * 128)
                DMA("sp", vg[i], VG[rows, :], [R("vd", c)], [R("vg", i)], "ld")
                DMA("sp", utc[i], UT[:, rows].rearrange("(g d) t -> d g t", d=128), [R("ud")], [R("utc", i)], "ld")
                sumsq(vg[i], vj[i], 0, R("vg", i), R("vj", i))
                rstd_from_ss(0, 1, 1, DG)
                V(lambda i=i: nc.vector.scalar_tensor_tensor(out=vb[i], in0=vg[i], scalar=st[:, 1:2], in1=gng_bc, op0=ALU.mult, op1=ALU.mult),
                  [R("vg", i), R("st"), R("rp")], [R("vb", i)])
                for g0 in range(0, NGG, 4):
                    p = nxt("ps", 6)

                    def mmg(p=p, i=i, g0=g0):
                        ins = None
                        for gg in range(g0, min(g0 + 4, NGG)):
                            o = (gg - g0) * 128
                            nc.tensor.matmul(ps[p][:, o:o + 128], lhsT=vb[i][:, gg * 128:(gg + 1) * 128], rhs=wsT[:, gg, :], start=(gg == g0), stop=False)
                            ins = nc.tensor.matmul(ps[p][:, o:o + 128], lhsT=ones_b[0:1, :], rhs=bsr[0:1, gg * 128:(gg + 1) * 128], start=False,
                                                   stop=(gg == min(g0 + 4, NGG) - 1))
                        return ins
                    T(mmg, [R("vb", i), R("wsT"), R("bsr"), R("c")], [R("ps", p)])
                    ng = min(4, NGG - g0)
                    V(lambda p=p, i=i, g0=g0, ng=ng: nc.vector.tensor_tensor(out=ygt[i][:, g0:g0 + ng, :], in0=ps[p][:, 0:ng * 128].rearrange("p (a b) -> p a b", a=ng),
                                                                            in1=utc[i][:, g0:g0 + ng, :], op=ALU.mult),
                      [R("ps", p), R("utc", i)], [R("ygt", i)])
                DMA("sp", YCT[DS:D, rows].rearrange("(g d) t -> d g t", d=128), ygt[i], [R("ygt", i)], [R("yct")], "st")

        _ph4()
        if DBG.get('ph', 99) <= 4:
            return
        def _ph5():
            phase()
            act = al([KT, TB], BF16)
            wsl = [al([KT, SW], BF16) for _ in range(2)]
            ev = [al([SW]) for _ in range(3)]
            for tb in range(NTB):
                DMA("sp", act, YCT[:, tb * TB:(tb + 1) * TB].rearrange("(k p) t -> p k t", p=128), [R("yct")], [R("act")], "ld")
                proj_residual(act, R("act"), wsl, ev, tb, w_out[l])


        _ph5()
    def xattn(l):
        ML, XH = C.ML, C.XH
        MT = ML // 128
        ET = KT // XH
        scale = float(C.XD) ** -0.5

        def _x1():
            phase()
            xt = [al([D]) for _ in range(2)]
            xs = [al([D], BF16) for _ in range(2)]
            memT = al([KT, ML], BF16)
            wsl = [al([KT, SW], BF16) for _ in range(2)]
            evb = [al([SW], BF16) for _ in range(3)]
            for t in range(MT):
                i = nxt("xt", 2)
                norm_T(xt[i], xs[i], i, mem_in, "mem", t * 128, 4 * l + 2, memT, R("memT"), t * 128)
            for c0 in range(0, D, SW):
                wi = load_w(wsl, w_kv[l], c0, SW, KT)
                for s0 in range(0, SW, 128):
                    p = nxt("ps", 6)
                    T(mm_feat(ps[p][:, 0:ML], memT, 0, ML, wsl[wi], s0, KT), [R("memT"), R("wsl", wi)], [R("ps", p)])
                    e = nxt("evb", 3)
                    V(lambda p=p, e=e: nc.vector.tensor_copy(out=evb[e][:, 0:ML], in_=ps[p][:, 0:ML]), [R("ps", p)], [R("evb", e)])
                    DMA("sp", KTd[c0 + s0:c0 + s0 + 128, :], evb[e][:, 0:ML], [R("evb", e)], [R("ktd")], "st")
            for c0 in range(0, D, SW):
                wi = load_w(wsl, w_kv[l], D + c0, SW, KT)
                for t in range(MT):
                    p = nxt("ps", 6)
                    T(mm_tok(ps[p][:, 0:SW], memT, t * 128, wsl[wi], 0, SW, KT), [R("memT"), R("wsl", wi)], [R("ps", p)])
                    e = nxt("evb", 3)
                    V(lambda p=p, e=e: nc.vector.tensor_copy(out=evb[e], in_=ps[p][:, 0:SW]), [R("ps", p)], [R("evb", e)])
                    DMA("sp", Vd[t * 128:(t + 1) * 128, c0:c0 + SW], evb[e], [R("evb", e)], [R("vdd")], "st")
        _x1()
        if DBG.get('xph', 99) <= 0:
            return

        def _x2a():
            phase()
            xt = [al([D]) for _ in range(2)]
            xs = [al([D], BF16) for _ in range(2)]
            hT = al([KT, TB], BF16)
            wsl = [al([KT, SWA], BF16) for _ in range(2)]
            evb = [al([512], BF16) for _ in range(3)]
            for tb in range(NTB):
                for t in range(TPB):
                    i = nxt("xt", 2)
                    norm_T(xt[i], xs[i], i, xres, "xres", (tb * TPB + t) * 128, 4 * l + 1, hT, R("hT"), t * 128)
                for c0 in range(0, D, SWA):
                    wi = load_w(wsl, w_q[l], c0, SWA, KT)
                    for s0 in range(0, SWA, 128):
                        for h0 in range(0, TB, 512):
                            hw_ = min(512, TB - h0)
                            p = nxt("ps", 6)
                            T(mm_feat(ps[p][:, 0:hw_], hT, h0, hw_, wsl[wi], s0, KT), [R("hT"), R("wsl", wi)], [R("ps", p)])
                            e = nxt("evb", 3)
                            V(lambda p=p, e=e, hw_=hw_: nc.vector.tensor_copy(out=evb[e][:, 0:hw_], in_=ps[p][:, 0:hw_]), [R("ps", p)], [R("evb", e)])
                            DMA("sp", QT[c0 + s0:c0 + s0 + 128, tb * TB + h0:tb * TB + h0 + hw_], evb[e][:, 0:hw_], [R("evb", e)], [R("qtd")], "st")
        _x2a()
        if DBG.get('xph', 99) <= 1:
            return

        def _x2b():
            phase()
            KTs = al([KT, ML], BF16)
            Vs = al([MT, D], BF16)
            DMA("sp", KTs, KTd.rearrange("(k p) m -> p k m", p=128), [R("ktd")], [R("KTs")], "ld")
            DMA("sp", Vs, Vd.rearrange("(m p) d -> p m d", p=128), [R("vdd")], [R("Vs")], "ld")
            qT = [al([KT, TBA], BF16) for _ in range(2)]
            oTb = [al([KT, TBA], BF16) for _ in range(2)]
            pT = [al([MT, TBA], BF16) for _ in range(2)]
            pr = [al([ML]) for _ in range(2)]
            pb = [al([ML], BF16) for _ in range(2)]
            for tb in range(NTBA):
                qi = nxt("qT", 2)
                DMA("sp", qT[qi], QT[:, tb * TBA:(tb + 1) * TBA].rearrange("(k p) t -> p k t", p=128), [R("qtd")], [R("qT", qi)], "ld")
                for hd in range(XH):
                    pi = nxt("pT", 2)
                    for t in range(TPBA):
                        p = nxt("ps", 6)

                        def mms(p=p, qi=qi, hd=hd, t=t):
                            ins = None
                            for e in range(ET):
                                k = hd * ET + e
                                ins = nc.tensor.matmul(ps[p][:, 0:ML], lhsT=qT[qi][:, k, t * 128:(t + 1) * 128], rhs=KTs[:, k, :],
                                                       start=(e == 0), stop=(e == ET - 1))
                            return ins
                        T(mms, [R("qT", qi), R("KTs")], [R("ps", p)])
                        k2 = nxt("pr", 2)
                        V(lambda p=p: nc.vector.tensor_reduce(out=st[:, 32:33], in_=ps[p][:, 0:ML], axis=AX.X, op=ALU.max), [R("ps", p)], [R("st")])
                        V(lambda: nc.vector.tensor_scalar(out=st[:, 33:34], in0=st[:, 32:33], scalar1=-scale, scalar2=None, op0=ALU.mult), [R("st")], [R("st")])
                        A(lambda p=p, k2=k2: nc.scalar.activation(out=pr[k2], in_=ps[p][:, 0:ML], func=AF.Exp, bias=st[:, 33:34], scale=scale,
                                                                 accum_out=st[:, 34:35]), [R("ps", p), R("st")], [R("pr", k2), R("st")])
                        V(lambda: nc.vector.reciprocal(out=st[:, 35:36], in_=st[:, 34:35]), [R("st")], [R("st")])
                        V(lambda k2=k2: nc.vector.tensor_scalar(out=pb[k2], in0=pr[k2], scalar1=st[:, 35:36], scalar2=None, op0=ALU.mult),
                          [R("pr", k2), R("st")], [R("pb", k2)])
                        b = 6 + nxt("psb", 2)
                        pbv = ps[b][:, :].bitcast(BF16)

                        def trp(pbv=pbv, k2=k2):
                            ins = None
                            for m in range(MT):
                                ins = nc.tensor.transpose(pbv[:, m * 128:(m + 1) * 128], pb[k2][:, m * 128:(m + 1) * 128], ident_b)
                            return ins
                        T(trp, [R("pb", k2), R("c")], [R("ps", b)])
                        V(lambda pbv=pbv, pi=pi, t=t: nc.vector.tensor_copy(out=pT[pi][:, :, t * 128:(t + 1) * 128],
                                                                            in_=pbv[:, 0:MT * 128].rearrange("p (a b) -> p a b", a=MT)),
                          [R("ps", b)], [R("pT", pi)])
                    for dv in range(ET):
                        p = nxt("ps", 6)
                        k = hd * ET + dv

                        def mmo(p=p, pi=pi, k=k):
                            ins = None
                            for m in range(MT):
                                ins = nc.tensor.matmul(ps[p][:, 0:TBA], lhsT=Vs[:, m, k * 128:(k + 1) * 128], rhs=pT[pi][:, m, :],
                                                       start=(m == 0), stop=(m == MT - 1))
                            return ins
                        T(mmo, [R("Vs"), R("pT", pi)], [R("ps", p)])
                        V(lambda p=p, qi=qi, k=k: nc.vector.tensor_copy(out=oTb[qi][:, k, :], in_=ps[p][:, 0:TBA]), [R("ps", p)], [R("oTb", qi)])
                DMA("sp", YCT[:, tb * TBA:(tb + 1) * TBA].rearrange("(k p) t -> p k t", p=128), oTb[qi], [R("oTb", qi)], [R("yct")], "st")
        _x2b()
        if DBG.get('xph', 99) <= 2:
            return

        def _x2c():
            phase()
            act = al([KT, TB], BF16)
            wsl = [al([KT, SW], BF16) for _ in range(2)]
            ev = [al([SW]) for _ in range(3)]
            for tb in range(NTB):
                DMA("sp", act, YCT[:, tb * TB:(tb + 1) * TB].rearrange("(k p) t -> p k t", p=128), [R("yct")], [R("act")], "ld")
                proj_residual(act, R("act"), wsl, ev, tb, w_o[l])
        _x2c()


    def moe(l):
        NE, DE, CAP = C.NE, C.DE, C.CAP
        JT = CAP // 128
        FT = DE // 128
        NQ = NE * JT
        Ub_b = c_bf[:, 512:640]

        def _m1():
            phase()
            gbc = al([D])
            DMA("sp", gbc, gmoe_row[l:l + 1, :].partition_broadcast(128), [], [R("gbc")], "c")
            wr = al([KT, NE])
            DMA("sp", wr, w_router[l].rearrange("(k p) e -> p k e", p=128), [], [R("wr")], "c")
            xt = [al([D]) for _ in range(2)]
            h32 = [al([D]) for _ in range(2)]
            hb = [al([D], BF16) for _ in range(2)]
            hT32 = al([KT, 128])
            lg = [al([NE]) for _ in range(2)]
            aft = [al([NE]) for _ in range(2)]
            afs = [al([128]) for _ in range(2)]
            for tt in range(NT):
                i = nxt("xt", 2)
                rows = slice(tt * 128, (tt + 1) * 128)
                DMA("sp", xt[i], xres[rows, :], [R("xres", tt)], [R("xt", i)], "ld")
                sumsq(xt[i], h32[i], 0, R("xt", i), R("h32", i))
                rstd_from_ss(0, 1, 1, D)
                V(lambda i=i: nc.vector.scalar_tensor_tensor(out=h32[i], in0=xt[i], scalar=st[:, 1:2], in1=gbc, op0=ALU.mult, op1=ALU.mult),
                  [R("xt", i), R("st"), R("gbc")], [R("h32", i)])
                A(lambda i=i: nc.scalar.activation(out=hb[i], in_=h32[i], func=AF.Copy), [R("h32", i)], [R("hb", i)])
                DMA("sp", H3[rows, :], hb[i], [R("hb", i)], [R("h3d")], "st")
                for k0 in range(0, KT, 4):
                    p = nxt("ps", 6)
                    nk = min(4, KT - k0)

                    def tr(p=p, k0=k0, nk=nk, i=i):
                        ins = None
                        for k in range(nk):
                            ins = nc.tensor.transpose(ps[p][:, k * 128:(k + 1) * 128], h32[i][:, (k0 + k) * 128:(k0 + k + 1) * 128], ident_f)
                        return ins
                    T(tr, [R("h32", i), R("c")], [R("ps", p)])
                    V(lambda p=p, k0=k0, nk=nk: nc.vector.tensor_copy(out=hT32[:, k0:k0 + nk, :], in_=ps[p][:, 0:nk * 128].rearrange("p (a b) -> p a b", a=nk)),
                      [R("ps", p)], [R("hT32")])
                p = nxt("ps", 6)

                def mml(p=p):
                    ins = None
                    for kt in range(KT):
                        ins = nc.tensor.matmul(ps[p][:, 0:NE], lhsT=hT32[:, kt, :], rhs=wr[:, kt, :], start=(kt == 0), stop=(kt == KT - 1))
                    return ins
                T(mml, [R("hT32"), R("wr")], [R("ps", p)])
                V(lambda p=p: nc.vector.tensor_reduce(out=st[:, 32:33], in_=ps[p][:, 0:NE], axis=AX.X, op=ALU.max), [R("ps", p)], [R("st")])
                V(lambda: nc.vector.tensor_scalar(out=st[:, 33:34], in0=st[:, 32:33], scalar1=-1.0, scalar2=None, op0=ALU.mult), [R("st")], [R("st")])
                A(lambda p=p, i=i: nc.scalar.activation(out=lg[i], in_=ps[p][:, 0:NE], func=AF.Exp, bias=st[:, 33:34], scale=1.0, accum_out=st[:, 34:35]),
                  [R("ps", p), R("st")], [R("lg", i), R("st")])
                V(lambda: nc.vector.reciprocal(out=st[:, 35:36], in_=st[:, 34:35]), [R("st")], [R("st")])
                V(lambda i=i: nc.vector.tensor_scalar(out=aft[i], in0=lg[i], scalar1=st[:, 35:36], scalar2=None, op0=ALU.mult),
                  [R("lg", i), R("st")], [R("aft", i)])
                DMA("sp", AFF[rows, :], aft[i], [R("aft", i)], [R("affd")], "st")
                p2 = nxt("ps", 6)
                T(lambda p2=p2, i=i: nc.tensor.transpose(ps[p2][0:NE, 0:128], aft[i], ident_f), [R("aft", i), R("c")], [R("ps", p2)])
                V(lambda p2=p2, i=i: nc.vector.tensor_copy(out=afs[i][0:NE, :], in_=ps[p2][0:NE, 0:128]), [R("ps", p2)], [R("afs", i)])
                DMA("sp", AFFT[:, rows], afs[i][0:NE, :], [R("afs", i)], [R("afftd")], "st")
        _m1()
        if DBG.get('mph', 99) <= 0:
            return

        def _m2():
            phase()
            affall = al([NT, NE])
            DMA("sp", affall, AFF.rearrange("(t p) e -> p t e", p=128), [R("affd")], [R("affall")], "ld")
            rowbc = [al([S]) for _ in range(2)]
            junk = al([S])
            junk2 = al([S])
            naff = al([NT, NE])
            V(lambda: nc.vector.tensor_scalar(out=naff, in0=affall, scalar1=-1.0, scalar2=None, op0=ALU.mult), [R("affall")], [R("naff")])
            rank = al([NT, NE])
            sel = al([NT, NE])
            pos = al([NT, NE])
            selb = al([NT, NE], BF16)
            ahi = al([NT, NE], BF16)
            hif = al([NT, NE])
            ahl = al([NT * NE, 4], BF16)
            pidx = al([1])
            DMA("sp", pidx, pidx_in[:, :], [], [R("pidx")], "c")
            for e in range(NE):
                i = nxt("rowbc", 2)
                DMA("sp", rowbc[i], AFFT[e:e + 1, :].partition_broadcast(128), [R("afftd")], [R("rowbc", i)], "ld")
                for tt in range(NT):
                    if tt % 9 < 5:
                        V(lambda i=i, tt=tt, e=e: nc.vector.tensor_scalar(out=junk, in0=rowbc[i], scalar1=affall[:, tt, e:e + 1], scalar2=None,
                                                                         op0=ALU.is_gt, op1=ALU.add, accum_out=rank[:, tt, e:e + 1]),
                          [R("rowbc", i), R("affall")], [R("junk"), R("rank")])
                    else:
                        A(lambda i=i, tt=tt, e=e: nc.scalar.activation(out=junk2, in_=rowbc[i], func=AF.Sign, bias=naff[:, tt, e:e + 1], scale=1.0,
                                                                      accum_out=rank[:, tt, e:e + 1]),
                          [R("rowbc", i), R("naff")], [R("junk2"), R("rank2")])
            for tt in range(NT):
                if tt % 9 >= 5:
                    V(lambda tt=tt: nc.vector.tensor_scalar(out=rank[:, tt, :], in0=rank[:, tt, :], scalar1=float(S - 1), scalar2=0.5, op0=ALU.add, op1=ALU.mult),
                      [R("rank"), R("rank2")], [R("rank"), R("rank2")])
            V(lambda: nc.vector.tensor_scalar(out=sel, in0=rank, scalar1=float(CAP), scalar2=None, op0=ALU.is_lt), [R("rank"), R("rank2")], [R("sel")])
            V(lambda: nc.vector.tensor_copy(out=selb, in_=sel), [R("sel")], [R("selb")])
            for tt in range(NT):
                p = nxt("ps", 6)

                def mmp(p=p, tt=tt):
                    ins = nc.tensor.matmul(ps[p][:, 0:NE], lhsT=Ub_b, rhs=selb[:, tt, :], start=True, stop=(tt == 0))
                    for t2 in range(tt):
                        ins = nc.tensor.matmul(ps[p][:, 0:NE], lhsT=ones_b, rhs=selb[:, t2, :], start=False, stop=(t2 == tt - 1))
                    return ins
                T(mmp, [R("selb"), R("c")], [R("ps", p)])
                V(lambda p=p, tt=tt: nc.vector.tensor_copy(out=pos[:, tt, :], in_=ps[p][:, 0:NE]), [R("ps", p)], [R("pos")])
            V(lambda: nc.vector.tensor_copy(out=ahi, in_=affall), [R("affall")], [R("ahi")])
            V(lambda: nc.vector.tensor_copy(out=hif, in_=ahi), [R("ahi")], [R("hif")])
            V(lambda: nc.vector.tensor_tensor(out=hif, in0=affall, in1=hif, op=ALU.subtract), [R("affall"), R("hif")], [R("hif")])
            V(lambda: nc.vector.tensor_copy(out=ahl[:, :, 0], in_=ahi.rearrange("p a b -> p (a b)")), [R("ahi")], [R("ahl")])
            V(lambda: nc.vector.tensor_copy(out=ahl[:, :, 1], in_=hif.rearrange("p a b -> p (a b)")), [R("hif")], [R("ahl")])
            for tt in range(NT):
                V(lambda tt=tt: nc.vector.memset(ahl[:, tt * NE:(tt + 1) * NE, 2], float(tt)), [R("ahl")], [R("ahl")])
            V(lambda: nc.vector.tensor_scalar(out=ahl[:, :, 3], in0=hif.rearrange("p a b -> p (a b)"), scalar1=0.0, scalar2=pidx[:, 0:1],
                                              op0=ALU.mult, op1=ALU.add), [R("hif"), R("pidx"), R("ahl")], [R("ahl")])
            DMA("sp", SELd[:, :], sel.rearrange("p a b -> p (a b)"), [R("sel")], [R("seld")], "st")
            DMA("sp", POSd[:, :], pos.rearrange("p a b -> p (a b)"), [R("pos")], [R("posd")], "st")
            DMA("sp", AHLd[:, :], ahl.rearrange("p a b -> p (a b)"), [R("ahl")], [R("ahld")], "st")
        _m2()
        if DBG.get('mph', 99) <= 1:
            return

        phase()
        sel = al([NT * NE]); pos = al([NT * NE]); ahl = al([NT * NE, 4], BF16); iot = al([CAP])
        DMA("sp", sel, SELd[:, :], [R("seld")], [R("sel3")], "ld")
        DMA("sp", pos, POSd[:, :], [R("posd")], [R("pos3")], "ld")
        DMA("sp", ahl, AHLd.rearrange("p (a b) -> p a b", b=4), [R("ahld")], [R("ahl3")], "ld")
        DMA("sp", iot, iota_in[:, :], [], [R("iot")], "c")
        xsT = al([KT, CAP], BF16)
        actT = al([FT, CAP], BF16)
        gl = al([2, JT])
        gtmp = al([4])
        idxf = al([2])
        GCH = min(1024, D // 2)
        NCH = D // GCH
        H3v = H3.rearrange("s (c g) -> (s c) g", g=GCH)
        idxc = al([NCH])
        idxi = al([2 * JT * NCH]).bitcast(I32)
        xg = al([D], BF16)
        oh = al([NT, CAP], BF16)
        stg = [al([S], FP8) for _ in range(1)]
        bigb = [al([max(NT, KT), SW], BF16) for _ in range(2)]
        h3s = [bigb[i][:, 0:NT, :] for i in range(2)]
        wsl = [bigb[i][:, 0:KT, :] for i in range(2)]
        sg = [al([CAP]) for _ in range(1)]
        ygs = [al([SW], BF16) for _ in range(2)]

        def stA(e):
            for tt in range(NT):
                q = tt * NE + e
                V(lambda tt=tt, q=q: nc.vector.tensor_scalar(out=oh[:, tt, :], in0=iot, scalar1=pos[:, q:q + 1], scalar2=sel[:, q:q + 1],
                                                           op0=ALU.is_equal, op1=ALU.mult),
                  [R("iot"), R("pos3"), R("sel3")], [R("oh")])

        def stB(e):
            ep = e % 2
            for jt in range(JT):
                p = nxt("ps", 6)

                def mmg(p=p, jt=jt):
                    ins = None
                    for tt in range(NT):
                        ins = nc.tensor.matmul(ps[p][:, 0:4], lhsT=oh[:, tt, jt * 128:(jt + 1) * 128], rhs=ahl[:, tt * NE + e, :],
                                               start=(tt == 0), stop=(tt == NT - 1))
                    return ins
                T(mmg, [R("oh"), R("ahl3")], [R("ps", p)])
                V(lambda p=p: nc.vector.tensor_copy(out=gtmp, in_=ps[p][:, 0:4]), [R("ps", p)], [R("gtmp")])
                V(lambda jt=jt, ep=ep: nc.vector.tensor_tensor(out=gl[:, ep, jt:jt + 1], in0=gtmp[:, 0:1], in1=gtmp[:, 1:2], op=ALU.add), [R("gtmp")], [R("gl", ep)])
                V(lambda: nc.vector.scalar_tensor_tensor(out=idxf[:, 0:1], in0=gtmp[:, 2:3], scalar=128.0, in1=gtmp[:, 3:4], op0=ALU.mult, op1=ALU.add),
                  [R("gtmp")], [R("idxf")])
                for c_ in range(NCH):
                    V(lambda c_=c_: nc.vector.tensor_scalar(out=idxc[:, c_:c_ + 1], in0=idxf[:, 0:1], scalar1=float(NCH), scalar2=float(c_),
                                                          op0=ALU.mult, op1=ALU.add), [R("idxf")], [R("idxc")])
                io = (ep * JT + jt) * NCH
                V(lambda io=io: nc.vector.tensor_copy(out=idxi[:, io:io + NCH], in_=idxc), [R("idxc")], [R("idxi", ep, jt)])

        def stC(e):
            for jt in range(JT):
                si = nxt("stg", 1)
                for t0 in range(0, NT, 8):
                    b = 6 + nxt("psb", 2)
                    pbv = ps[b][:, :].bitcast(BF16)
                    nk = min(8, NT - t0)

                    def tro(pbv=pbv, t0=t0, nk=nk, jt=jt):
                        ins = None
                        for k in range(nk):
                            ins = nc.tensor.transpose(pbv[:, k * 128:(k + 1) * 128], oh[:, t0 + k, jt * 128:(jt + 1) * 128], ident_b)
                        return ins
                    T(tro, [R("oh"), R("c")], [R("ps", b)])
                    V(lambda pbv=pbv, t0=t0, nk=nk, si=si: nc.vector.tensor_copy(out=stg[si][:, t0 * 128:(t0 + nk) * 128], in_=pbv[:, 0:nk * 128], saturate=False),
                      [R("ps", b)], [R("stg", si)])
                qq = e * JT + jt
                DMA("sp", OHT[:, :, qq * 128:(qq + 1) * 128].rearrange("t p c -> p t c"), stg[si].rearrange("p (t c) -> p t c", c=128),
                    [R("stg", si)], [R("ohtd")], "st")

        def stD(e):
            ep = e % 2
            for jt in range(JT):
                for c_ in range(NCH):
                    io = (ep * JT + jt) * NCH + c_
                    P.op("pool", lambda io=io, c_=c_: nc.gpsimd.indirect_dma_start(out=xg[:, c_ * GCH:(c_ + 1) * GCH], out_offset=None, in_=H3v[:, :],
                                                                                 in_offset=bass.IndirectOffsetOnAxis(ap=idxi[:, io:io + 1], axis=0),
                                                                                 bounds_check=None, oob_is_err=True),
                         reads=[R("idxi", ep, jt), R("h3d")], writes=[R("xg")], stream="g")
                for k0 in range(0, KT, 8):
                    b2 = 6 + nxt("psb", 2)
                    pbv2 = ps[b2][:, :].bitcast(BF16)
                    nk2 = min(8, KT - k0)

                    def trg(pbv2=pbv2, k0=k0, nk2=nk2):
                        ins = None
                        for k in range(nk2):
                            ins = nc.tensor.transpose(pbv2[:, k * 128:(k + 1) * 128], xg[:, (k0 + k) * 128:(k0 + k + 1) * 128], ident_b)
                        return ins
                    T(trg, [R("xg"), R("c")], [R("ps", b2)])
                    V(lambda pbv2=pbv2, k0=k0, nk2=nk2, jt=jt: nc.vector.tensor_copy(out=xsT[:, k0:k0 + nk2, jt * 128:(jt + 1) * 128],
                                                                                   in_=pbv2[:, 0:nk2 * 128].rearrange("p (a b) -> p a b", a=nk2)),
                      [R("ps", b2)], [R("xsT")])

        def stE(e, mid=None):
            wgu = w_gate_up[l, e]
            for f2 in range(DE // 256):
                if f2 == 1 and mid is not None:
                    mid()
                    mid = None
                wi = nxt("w", 2)
                DMA("pool", wsl[wi][:, :, 0:256], wgu[:, f2 * 256:(f2 + 1) * 256].rearrange("(kt p) c -> p kt c", p=128), [], [R("wsl", wi)], "w")
                DMA("pool", wsl[wi][:, :, 256:512], wgu[:, DE + f2 * 256:DE + (f2 + 1) * 256].rearrange("(kt p) c -> p kt c", p=128), [], [R("wsl", wi)], "w")
                for sub in range(2):
                    pg = nxt("ps", 6)
                    pu = nxt("ps", 6)
                    T(mm_feat(ps[pg][:, 0:CAP], xsT, 0, CAP, wsl[wi], sub * 128, KT), [R("xsT"), R("wsl", wi)], [R("ps", pg)])
                    T(mm_feat(ps[pu][:, 0:CAP], xsT, 0, CAP, wsl[wi], 256 + sub * 128, KT), [R("xsT"), R("wsl", wi)], [R("ps", pu)])
                    gi_ = nxt("sg", 1)
                    A(lambda pg=pg, gi_=gi_: nc.scalar.activation(out=sg[gi_], in_=ps[pg][:, 0:CAP], func=AF.Silu), [R("ps", pg)], [R("sg", gi_)])
                    fi = f2 * 2 + sub
                    V(lambda pu=pu, gi_=gi_, fi=fi: nc.vector.tensor_tensor(out=actT[:, fi, :], in0=sg[gi_], in1=ps[pu][:, 0:CAP], op=ALU.mult),
                      [R("sg", gi_), R("ps", pu)], [R("actT")])
            if mid is not None:
                mid()

        def stF(e):
            ep = e % 2
            for dblk in range(D // SW):
                wi = load_w(wsl, w_down[l, e], dblk * SW, SW, FT)
                for jt in range(JT):
                    p = nxt("ps", 6)
                    T(mm_tok(ps[p][:, 0:SW], actT, jt * 128, wsl[wi], 0, SW, FT), [R("actT"), R("wsl", wi)], [R("ps", p)])
                    yi = nxt("ygs", 2)
                    V(lambda p=p, yi=yi, jt=jt: nc.vector.tensor_scalar(out=ygs[yi], in0=ps[p][:, 0:SW], scalar1=gl[:, ep, jt:jt + 1], scalar2=None, op0=ALU.mult),
                      [R("ps", p), R("gl", ep)], [R("ygs", yi)])
                    DMA("sp", YG[e * CAP + jt * 128:e * CAP + (jt + 1) * 128, dblk * SW:(dblk + 1) * SW], ygs[yi], [R("ygs", yi)], [R("ygd")], "st")

        stA(0); stB(0); stC(0); stD(0)
        for e in range(NE):
            if e + 1 < NE:
                stE(e, mid=lambda e=e: stA(e + 1))
                stB(e + 1)
            else:
                stE(e)
            stF(e)
            if e + 1 < NE:
                stC(e + 1)
                stD(e + 1)
        if DBG.get('mph', 99) <= 2:
            return

        def _m4():
            phase()
            ygs = al([NQ, SW], BF16)
            oht = [al([NQ, 128], FP8) for _ in range(2)]
            ev = [al([SW]) for _ in range(3)]
            for dblk in range(D // SW):
                DMA("sp", ygs, YG[:, dblk * SW:(dblk + 1) * SW].rearrange("(q p) c -> p q c", p=128), [R("ygd")], [R("ygs4")], "ld")
                for tt in range(NT):
                    oi = nxt("oht", 2)
                    DMA("sp", oht[oi], OHT[tt].rearrange("p (q t) -> p q t", t=128), [R("ohtd")], [R("oht", oi)], "ld")
                    p = nxt("ps", 6)

                    def mmc(p=p, oi=oi):
                        ins = None
                        for q in range(NQ):
                            ins = nc.tensor.matmul(ps[p][:, 0:SW], lhsT=oht[oi][:, q, :], rhs=ygs[:, q, :], start=(q == 0), stop=(q == NQ - 1))
                        return ins
                    T(mmc, [R("oht", oi), R("ygs4")], [R("ps", p)])
                    ei = nxt("ev", 3)
                    DMA("sp", ev[ei], xres[tt * 128:(tt + 1) * 128, dblk * SW:(dblk + 1) * SW], [R("xres", tt)], [R("ev", ei)], "ld")
                    V(lambda p=p, ei=ei: nc.vector.tensor_tensor(out=ev[ei], in0=ps[p][:, 0:SW], in1=ev[ei], op=ALU.add),
                      [R("ps", p), R("ev", ei)], [R("ev", ei)])
                    DMA("act", xres[tt * 128:(tt + 1) * 128, dblk * SW:(dblk + 1) * SW], ev[ei], [R("ev", ei)], [R("xres", tt)], "st")
        _m4()


    for tt in range(NT):
        DMA("sp", xres[tt * 128:(tt + 1) * 128, :], x_in[tt * 128:(tt + 1) * 128, :], [], [R("xres", tt)], "cp")

    for l in range(nlayers):
        if nstage >= 1:
            mixer(l)
        if nstage >= 2:
            xattn(l)
        if nstage >= 3:
            moe(l)
    phase()
    gfin = al([D])
    xt2 = [al([D]) for _ in range(2)]
    xs2 = [al([D], BF16) for _ in range(2)]
    DMA("sp", gfin, gfin_d[0:1, :].partition_broadcast(128), [], [R("gfin")], "c")
    for tt in range(NT):
        i = nxt("xt", 2)
        DMA("sp", xt2[i], xres[tt * 128:(tt + 1) * 128, :], [R("xres", tt)], [R("xt", i)], "ld")
        sumsq(xt2[i], xs2[i], 0, R("xt", i), R("xs", i))
        rstd_from_ss(0, 1, 1, D)
        V(lambda i=i: nc.vector.scalar_tensor_tensor(out=xt2[i], in0=xt2[i], scalar=st[:, 1:2], in1=gfin, op0=ALU.mult, op1=ALU.mult),
          [R("xt", i), R("st"), R("gfin")], [R("xt", i)])
        DMA("sp", out[tt * 128:(tt + 1) * 128, :], xt2[i], [R("xt", i)], [R("out")], "out")
    P.wait_all("sp", [R("out")])
    P.run()
    es.close()
    P.close()
    return nc


def host_consts():
    i = np.arange(128)
    ident = np.eye(128, dtype=np.float32)
    Tf = (i[:, None] <= i[None, :]).astype(np.float32)
    Uf = (i[:, None] > i[None, :]).astype(np.float32)
    Tb = (i[:, None] >= i[None, :]).astype(np.float32)
    Ub = (i[:, None] < i[None, :]).astype(np.float32)
    ones = np.ones((128, 128), np.float32)
    return np.concatenate([ident, Tf, Uf, Tb, Ub, ones], axis=1)


def host_layout(C, inp, b):
    L, KT = C.L, C.KT
    f = lambda a: np.ascontiguousarray(a, dtype=np.float32)
    gl = []
    for l in range(L):
        for n in ("norm_mix_g", "norm_xattn_g", "norm_mem_g", "norm_moe_g"):
            gl.append(inp[n][l].reshape(KT, 128).T)
    gl.append(inp["final_norm_g"].reshape(KT, 128).T)
    m = {
        "x": f(inp["x"][b]),
        "gk": f(np.concatenate(gl, axis=1)),
        "consts": host_consts(),
        "gfin_d": f(inp["final_norm_g"].reshape(1, -1)),
        "w_in": f(inp["w_in"]), "w_out": f(inp["w_out"]),
        "convp": f(np.concatenate([np.concatenate([inp["conv_w"][l].T, inp["conv_b"][l][:, None]], axis=1)
                                   .reshape(C.CC // 128, 128, 6).transpose(1, 0, 2).reshape(128, -1) for l in range(L)], axis=1)),
        "rowp": f(np.stack([np.concatenate([inp["dt_bias"][l].reshape(-1), inp["a_log"][l].reshape(-1), np.repeat(inp["d_skip"][l], 64),
                                            inp["ssd_norm_g"][l], inp["gmlp_norm_g"][l]]) for l in range(L)], axis=0)),
        "gws": f(np.stack([inp["gmlp_ws"][l].transpose(2, 0, 1).reshape(128, -1) for l in range(L)], axis=0)),
        "gbs": f(inp["gmlp_bs"].reshape(L, -1)),
        "gmoe_row": f(inp["norm_moe_g"]), "w_router": f(inp["w_router"]), "w_gate_up": f(inp["w_gate_up"]), "w_down": f(inp["w_down"]),
        "iota_in": np.ascontiguousarray(np.broadcast_to(np.arange(C.CAP, dtype=np.float32)[None, :], (128, C.CAP))),
        "pidx_in": np.arange(128, dtype=np.float32).reshape(128, 1),
        "mem": f(inp["mem"][b]), "w_q": f(inp["w_q"]), "w_kv": f(inp["w_kv"]), "w_o": f(inp["w_o"]),
        "w_in": f(inp["w_in"]),
        "w_out": f(inp["w_out"]),
    }
    return m


_NC_CACHE = {}


def run(C, inputs, n_cores=2, stop_after=None):
    key = (id(C), stop_after)
    if key not in _NC_CACHE:
        _NC_CACHE[key] = build(C, stop_after)
    nc = _NC_CACHE[key]
    used = set()
    for alloc in nc.allocations:
        try:
            if alloc.kind == "ExternalInput":
                used.add(alloc.memorylocations[0].name)
        except Exception:
            pass
    maps = []
    for b in range(n_cores):
        m = host_layout(C, inputs, b)
        maps.append({k: v for k, v in m.items() if (not used) or k in used})
    res = run_bass_kernel_spmd(nc, maps, core_ids=list(range(n_cores)))
    LAST["res"] = res.results
    return np.stack([res.results[b]["out"] for b in range(n_cores)], axis=0)


def kernel(**inputs):
    inputs = {k: np.asarray(v) for k, v in inputs.items()}
    return run(FULL, inputs, n_cores=2).astype(np.float32)
```

```python
import numpy as np
from contextlib import ExitStack
import concourse.bass as bass
import concourse.mybir as mybir
from concourse.bass_utils import run_bass_kernel_spmd

F32 = mybir.dt.float32
BF16 = mybir.dt.bfloat16
FP8 = mybir.dt.float8e4
I32 = mybir.dt.int32
AF = mybir.ActivationFunctionType
ALU = mybir.AluOpType
AX = mybir.AxisListType
ENGS = ("pe", "act", "dve", "pool", "sp")
EPS = 1e-6


class Res:
    __slots__ = ("name", "w", "r")

    def __init__(self, name):
        self.name = name
        self.w = None
        self.r = []


class Prog:
    def __init__(self, nc):
        self.nc = nc
        self.q = {e: [] for e in ENGS}
        self.cnt = {}
        self.sems = {}
        self.known = {e: {} for e in ENGS}
        self._ctx = []
        self.res = {}
        self.rr = {}
        for e in ("pe", "act", "dve", "pool"):
            self.sems[e] = self._sem("s_" + e)
            self.cnt[e] = 0

    def _sem(self, name):
        cm = self.nc.semaphore(name)
        s = cm.__enter__()
        self._ctx.append(cm)
        return s

    def R(self, *key):
        r = self.res.get(key)
        if r is None:
            r = self.res[key] = Res(str(key))
        return r

    def _waits_for(self, eng, reads, writes):
        need = {}
        for r in reads:
            if r.w is not None and need.get(r.w[0], 0) < r.w[1]:
                need[r.w[0]] = r.w[1]
        for r in writes:
            for t in ([r.w] if r.w is not None else []) + r.r:
                if need.get(t[0], 0) < t[1]:
                    need[t[0]] = t[1]
        out = []
        kn = self.known[eng]
        for k, v in need.items():
            if kn.get(k, 0) >= v:
                continue
            kn[k] = v
            out.append((self.sems[k], v))
        return out

    def op(self, eng, fn, reads=(), writes=(), stream=None):
        waits = self._waits_for(eng, reads, writes)
        if stream is not None:
            npool = {"sp": 40, "pool": 12, "act": 8}[eng]
            idx = self.rr.get(eng, 0) % npool
            self.rr[eng] = self.rr.get(eng, 0) + 1
            key = "d_%s_%d" % (eng, idx)
            if key not in self.sems:
                self.sems[key] = self._sem(key)
                self.cnt[key] = 0
            prev = self.cnt[key]
            if prev > self.known[eng].get(key, 0):
                self.known[eng][key] = prev
                waits.append((self.sems[key], prev))
            inc = 16
        else:
            key = eng
            inc = 1
        self.cnt[key] += inc
        tok = (key, self.cnt[key])
        for r in reads:
            r.r.append(tok)
        for r in writes:
            r.w = tok
            r.r = []
        self.q[eng].append((waits, fn, self.sems[key], inc))

    def barrier(self):
        for e in ENGS:
            wl = []
            kn = self.known[e]
            for k, v in self.cnt.items():
                if v > kn.get(k, 0):
                    kn[k] = v
                    wl.append((self.sems[k], v))
            self.q[e].append((wl, None, None, 0))

    def mark(self, name):
        self.q["pe"].append(([], ("mark", name), None, 0))

    def wait_all(self, eng, resources):
        self.q[eng].append((self._waits_for(eng, resources, []), None, None, 0))

    def run(self):
        nc = self.nc
        with nc.Block() as block:
            def mk(name):
                def body(e):
                    for wl, fn, semh, inc in self.q[name]:
                        for s, v in wl:
                            e.wait_ge(s, v)
                        if isinstance(fn, tuple):
                            MARKS.append((fn[1], PECOUNT[0]))
                        elif fn is not None:
                            fn().then_inc(semh, inc)
                return body
            block.tensor(mk("pe"))
            block.scalar(mk("act"))
            block.vector(mk("dve"))
            block.gpsimd(mk("pool"))
            block.sync(mk("sp"))

    def close(self):
        for cm in reversed(self._ctx):
            cm.__exit__(None, None, None)


class Cfg:
    def __init__(self, D=4096, S=4096, ML=256, XH=4, NE=16, DE=1536, L=2, NG=8, NGG=16):
        self.D, self.S, self.ML, self.XH, self.NE, self.DE, self.L = D, S, ML, XH, NE, DE, L
        self.NG, self.NGG = NG, NGG
        self.NH = 4 * NG
        self.DS = self.NH * 64
        self.DG = NGG * 128
        assert self.DS + self.DG == D
        self.CC = self.DS + 2 * NG * 128
        self.DIN = self.DS + self.CC + 2 * self.NH + 2 * self.DG
        self.KT = D // 128
        self.NT = S // 128
        self.CAP = 2 * S // NE
        self.XD = D // XH


FULL = Cfg()
TAPS = set()
MARKS = []
PECOUNT = [0]
DBG = {}
LAST = {}


def build(C, stop_after=None):
    nc = bass.Bass("TRN2", target_bir_lowering=False)
    P = Prog(nc)
    R = P.R
    D, S, KT, NT, L = C.D, C.S, C.KT, C.NT, C.L
    DS, DG, NH, NG, NGG, CC = C.DS, C.DG, C.NH, C.NG, C.NGG, C.CC
    NB = NG * 128
    TB = min(1024, S)
    NTB = S // TB
    TPB = TB // 128
    SW = 512
    SWA = 256
    TBA = min(512, S)
    NTBA = S // TBA
    TPBA = TBA // 128
    NROW = 4 * NH + 2 * DS + DG
    CTN = CC // 128
    stages = ["mix", "xattn", "moe"]
    nstage = {None: 3, "none": 0, "mix": 1, "xattn": 2, "moe": 3}[stop_after]
    nlayers = L if stop_after is None else (0 if stop_after == "none" else 1)

    def din(name, shape, dt=F32):
        return nc.dram_tensor(name, list(shape), dt, kind="ExternalInput").ap()

    def dscr(name, shape, dt=F32):
        if name in TAPS:
            return nc.dram_tensor(name, list(shape), dt, kind="ExternalOutput").ap()
        return nc.dram_tensor(name, list(shape), dt).ap()

    x_in = din("x", [S, D])
    out = nc.dram_tensor("out", [S, D], F32, kind="ExternalOutput").ap()
    gk = din("gk", [128, (4 * L + 1) * KT])
    consts = din("consts", [128, 6 * 128])
    gfin_d = din("gfin_d", [1, D])
    xres = dscr("xres", [S, D])
    if nlayers > 0:
        w_in = din("w_in", [L, D, C.DIN])
        w_out = din("w_out", [L, D, D])
        convp = din("convp", [128, L * CTN * 6])
        rowp = din("rowp", [L, NROW])
        gws = din("gws", [L, 128, NGG * 128])
        gbs = din("gbs", [L, NGG * 128])
        ZS = dscr("ZS", [S, DS]); XBCT = dscr("XBCT", [CC, S]); DTR = dscr("DTR", [S, 2 * NH])
        UT = dscr("UT", [DG, S]); VG = dscr("VG", [S, DG]); XST = dscr("XST", [S, DS])
        BTOK = dscr("BTOK", [S, NB], BF16); BTd = dscr("BTd", [NB, S], BF16); CTd = dscr("CTd", [NB, S], BF16)
        CSd = dscr("CSd", [2, NT, 128, DS]); DECd = dscr("DECd", [NT, 128, 2 * NH])
        SPd = dscr("SPd", [2, NT, 128, DS], BF16)
        DTd = dscr("DTd", [S, 2 * NH]); DTAd = dscr("DTAd", [S, 2 * NH])
        YCT = dscr("YCT", [D, S], BF16)
    if nlayers > 0 and nstage >= 3:
        NE_, CAP_ = C.NE, C.CAP
        gmoe_row = din("gmoe_row", [L, D]); w_router = din("w_router", [L, D, NE_])
        w_gate_up = din("w_gate_up", [L, NE_, D, 2 * C.DE]); w_down = din("w_down", [L, NE_, C.DE, D])
        iota_in = din("iota_in", [128, CAP_])
        pidx_in = din("pidx_in", [128, 1])
        H3 = dscr("H3", [S, D], BF16); AFF = dscr("AFF", [S, NE_]); AFFT = dscr("AFFT", [NE_, S])
        SELd = dscr("SELd", [128, NT * NE_]); POSd = dscr("POSd", [128, NT * NE_]); AHLd = dscr("AHLd", [128, NT * NE_ * 4], BF16)
        YG = dscr("YG", [NE_ * CAP_, D], BF16); OHT = dscr("OHT", [NT, 128, NE_ * (CAP_ // 128) * 128], FP8)
    if nlayers > 0 and nstage >= 2:
        mem_in = din("mem", [C.ML, D])
        w_q = din("w_q", [L, D, D]); w_kv = din("w_kv", [L, D, 2 * D]); w_o = din("w_o", [L, D, D])
        KTd = dscr("KTd", [D, C.ML], BF16); Vd = dscr("Vd", [C.ML, D], BF16); QT = dscr("QT", [D, S], BF16)
        DBGY = dscr("DBGY", [S, DS]) if "DBGY" in TAPS else None
        DBG2 = dscr("DBG2", [128, 2048]) if "DBG2" in TAPS else None

    es = ExitStack()

    def sb(name, shape, dt=F32):
        return es.enter_context(nc.sbuf_tensor(name, list(shape), dt))

    NBIG = 43000
    BIG = sb("big", [128, NBIG])
    gk_sb = sb("gk_sb", [128, (4 * L + 1) * KT])
    c_sb = sb("c_sb", [128, 6 * 128])
    c_bf = sb("c_bf", [128, 6 * 128], BF16)
    st = sb("stat", [128, 64])
    ident_f, Tf, Uf, Tb, Ub, ones_f = [c_sb[:, i * 128:(i + 1) * 128] for i in range(6)]
    ident_b = c_bf[:, 0:128]
    ones_b = c_bf[:, 640:768]
    ps = [es.enter_context(nc.psum_tensor(f"ps{i}", [128, 512], F32)) for i in range(8)]
    cnt = {}
    arena = {"off": 0}

    def nxt(k, n):
        v = cnt.get(k, 0)
        cnt[k] = v + 1
        return v % n

    def phase():
        import inspect
        P.mark(inspect.stack()[1].function + ":" + str(inspect.stack()[1].lineno))
        P.barrier()
        arena["off"] = 0

    def al(shape, dt=F32):
        n = int(np.prod(shape))
        words = n if dt == F32 else ((n + 3) // 4 if dt == FP8 else (n + 1) // 2)
        words = (words + 1) // 2 * 2
        o = arena["off"]
        assert o + words <= NBIG, (o, words)
        arena["off"] = o + words
        v = BIG[:, o:o + words]
        if dt != F32:
            v = v.bitcast(dt)[:, 0:n]
        else:
            v = v[:, 0:n]
        if len(shape) == 2:
            v = v.rearrange("p (a b) -> p a b", a=shape[0])
        elif len(shape) == 3:
            v = v.rearrange("p (a b c) -> p a b c", a=shape[0], b=shape[1])
        return v

    def DMA(q, out_, in_, reads, writes, stream):
        eng = {"sp": nc.sync, "pool": nc.gpsimd, "act": nc.scalar}[q]
        P.op(q, lambda: eng.dma_start(out=out_, in_=in_), reads=reads, writes=writes, stream=stream)

    def V(fn, reads, writes):
        P.op("dve", fn, reads=reads, writes=writes)

    def A(fn, reads, writes):
        P.op("act", fn, reads=reads, writes=writes)

    def T(fn, reads, writes):
        P.op("pe", fn, reads=reads, writes=writes)

    DMA("sp", gk_sb[:], gk[:, :], [], [R("gk")], "c")
    DMA("sp", c_sb[:], consts[:, :], [], [R("c")], "c")
    V(lambda: nc.vector.tensor_copy(out=c_bf[:], in_=c_sb[:]), [R("c")], [R("c")])

    def rstd_from_ss(col_in, col_out, n, width):
        V(lambda: nc.vector.tensor_scalar(out=st[:, col_out:col_out + n], in0=st[:, col_in:col_in + n], scalar1=1.0 / width,
                                          scalar2=EPS, op0=ALU.mult, op1=ALU.add), [R("st")], [R("st")])
        A(lambda: nc.scalar.activation(out=st[:, col_out:col_out + n], in_=st[:, col_out:col_out + n], func=AF.Sqrt), [R("st")], [R("st")])
        V(lambda: nc.vector.reciprocal(out=st[:, col_out:col_out + n], in_=st[:, col_out:col_out + n]), [R("st")], [R("st")])

    def sumsq(x_ap, junk_ap, col, rx, rjunk):
        A(lambda: nc.scalar.activation(out=junk_ap, in_=x_ap, func=AF.Square, accum_out=st[:, col:col + 1]), [rx], [rjunk, R("st")])

    def norm_T(xt, xs, ri, src_dram, sname, row0, gi, dst, rdst, col0):
        DMA("sp", xt, src_dram[row0:row0 + 128, :], [R(sname, row0 // 128)], [R("xt", ri)], "ld")
        sumsq(xt, xs, 0, R("xt", ri), R("xs", ri))
        rstd_from_ss(0, 1, 1, D)
        V(lambda: nc.vector.tensor_scalar(out=xs, in0=xt, scalar1=st[:, 1:2], scalar2=None, op0=ALU.mult),
          [R("xt", ri), R("st")], [R("xs", ri)])
        for k0 in range(0, KT, 8):
            b = 6 + nxt("psb", 2)
            nk = min(8, KT - k0)
            pb = ps[b][:, :].bitcast(BF16)

            def tr(pb=pb, k0=k0, nk=nk):
                ins = None
                for k in range(nk):
                    ins = nc.tensor.transpose(pb[:, k * 128:(k + 1) * 128], xs[:, (k0 + k) * 128:(k0 + k + 1) * 128], ident_b)
                return ins
            T(tr, [R("xs", ri), R("c")], [R("ps", b)])
            for k in range(nk):
                kt = k0 + k
                V(lambda pb=pb, k=k, kt=kt: nc.vector.tensor_scalar(
                    out=dst[:, kt, col0:col0 + 128], in0=pb[:, k * 128:(k + 1) * 128],
                    scalar1=gk_sb[:, gi * KT + kt:gi * KT + kt + 1], scalar2=None, op0=ALU.mult),
                    [R("ps", b), R("gk")], [rdst])

    def load_w(wsl, wdram_l, c0, width, kts):
        i = nxt("w", 2)
        src = wdram_l[:, c0:c0 + width].rearrange("(kt p) c -> p kt c", p=128)
        DMA("pool", wsl[i][:, 0:kts, 0:width], src, [], [R("wsl", i)], "w")
        return i

    def mm_tok(psum_ap, act, t0, w_ap, c0, width, kts):
        def f():
            ins = None
            for kt in range(kts):
                ins = nc.tensor.matmul(psum_ap, lhsT=act[:, kt, t0:t0 + 128], rhs=w_ap[:, kt, c0:c0 + width],
                                       start=(kt == 0), stop=(kt == kts - 1))
            return ins
        return f

    def mm_feat(psum_ap, act, t0, tw, w_ap, c0, kts):
        def f():
            ins = None
            for kt in range(kts):
                ins = nc.tensor.matmul(psum_ap, lhsT=w_ap[:, kt, c0:c0 + 128], rhs=act[:, kt, t0:t0 + tw],
                                       start=(kt == 0), stop=(kt == kts - 1))
            return ins
        return f

    def proj_residual(act, ract, wsl, ev, tb, wdram_l):
        for c0 in range(0, D, SW):
            wi = load_w(wsl, wdram_l, c0, SW, KT)
            for t in range(TPB):
                tt = tb * TPB + t
                p = nxt("ps", 6)
                T(mm_tok(ps[p][:, 0:SW], act, t * 128, wsl[wi], 0, SW, KT), [ract, R("wsl", wi)], [R("ps", p)])
                e = nxt("ev", 3)
                DMA("sp", ev[e], xres[tt * 128:(tt + 1) * 128, c0:c0 + SW], [R("xres", tt)], [R("ev", e)], "ld")
                V(lambda p=p, e=e: nc.vector.tensor_tensor(out=ev[e], in0=ps[p][:, 0:SW], in1=ev[e], op=ALU.add),
                  [R("ps", p), R("ev", e)], [R("ev", e)])
                DMA("act", xres[tt * 128:(tt + 1) * 128, c0:c0 + SW], ev[e], [R("ev", e)], [R("xres", tt)], "st")

    def mixer(l):
        o1 = DS; o2 = o1 + CC; o3 = o2 + 2 * NH; o4 = o3 + DG
        def _ph0():
            phase()
            xt = [al([D]) for _ in range(2)]
            xs = [al([D], BF16) for _ in range(2)]
            hT = al([KT, TB], BF16)
            wsl = [al([KT, SWA], BF16) for _ in range(2)]
            ev = [al([SW]) for _ in range(3)]
            wl = w_in[l]
            for tb in range(NTB):
                for t in range(TPB):
                    i = nxt("xt", 2)
                    norm_T(xt[i], xs[i], i, xres, "xres", (tb * TPB + t) * 128, 4 * l + 0, hT, R("hT"), t * 128)
                segs = [("z", c, min(SWA, o1 - c)) for c in range(0, o1, SWA)]
                segs += [("xbc", c, min(SWA, o2 - c)) for c in range(o1, o2, SWA)]
                segs += [("dt", o2, 2 * NH)]
                segs += [("u", c, min(SWA, o4 - c)) for c in range(o3, o4, SWA)]
                segs += [("v", c, min(SWA, C.DIN - c)) for c in range(o4, C.DIN, SWA)]
                for kind, c0, w in segs:
                    wi = load_w(wsl, wl, c0, w, KT)
                    if kind in ("z", "dt", "v"):
                        for t in range(TPB):
                            tt = tb * TPB + t
                            p = nxt("ps", 6)
                            T(mm_tok(ps[p][:, 0:w], hT, t * 128, wsl[wi], 0, w, KT), [R("hT"), R("wsl", wi)], [R("ps", p)])
                            e = nxt("ev", 3)
                            if kind == "dt":
                                V(lambda p=p, e=e, w=w: nc.vector.tensor_copy(out=ev[e][:, 0:w], in_=ps[p][:, 0:w]), [R("ps", p)], [R("ev", e)])
                                dst = DTR[tt * 128:(tt + 1) * 128, :]
                            else:
                                fn = AF.Silu if kind == "z" else AF.Gelu
                                A(lambda p=p, e=e, w=w, fn=fn: nc.scalar.activation(out=ev[e][:, 0:w], in_=ps[p][:, 0:w], func=fn),
                                  [R("ps", p)], [R("ev", e)])
                                if kind == "z":
                                    dst = ZS[tt * 128:(tt + 1) * 128, c0:c0 + w]
                                else:
                                    dst = VG[tt * 128:(tt + 1) * 128, c0 - o4:c0 - o4 + w]
                            DMA("sp", dst, ev[e][:, 0:w], [R("ev", e)], [R(kind + "d", tt)], "st")
                    else:
                        for s0 in range(0, w, 128):
                            for h0 in range(0, TB, 512):
                                hw_ = min(512, TB - h0)
                                p = nxt("ps", 6)
                                T(mm_feat(ps[p][:, 0:hw_], hT, h0, hw_, wsl[wi], s0, KT), [R("hT"), R("wsl", wi)], [R("ps", p)])
                                e = nxt("ev", 3)
                                if kind == "xbc":
                                    V(lambda p=p, e=e, hw_=hw_: nc.vector.tensor_copy(out=ev[e][:, 0:hw_], in_=ps[p][:, 0:hw_]), [R("ps", p)], [R("ev", e)])
                                    ch = c0 - o1 + s0
                                    dst = XBCT[ch:ch + 128, tb * TB + h0:tb * TB + h0 + hw_]
                                    rr = R("xbcd", ch // 128)
                                else:
                                    A(lambda p=p, e=e, hw_=hw_: nc.scalar.activation(out=ev[e][:, 0:hw_], in_=ps[p][:, 0:hw_], func=AF.Gelu), [R("ps", p)], [R("ev", e)])
                                    ch = c0 - o3 + s0
                                    dst = UT[ch:ch + 128, tb * TB + h0:tb * TB + h0 + hw_]
                                    rr = R("ud")
                                DMA("sp", dst, ev[e][:, 0:hw_], [R("ev", e)], [rr], "st")
        _ph0()
        if DBG.get('ph', 99) <= 0:
            return
        def _ph1():
            phase()
            cp = al([L * CTN * 6])
            DMA("sp", cp, convp[:, :], [], [R("cp")], "c")
            xpad = [al([S + 4]) for _ in range(2)]
            acc = [al([S]) for _ in range(2)]
            silb = [al([S], BF16) for _ in range(2)]
            trf = al([NT, 128])
            trb = al([NT, 128], BF16)
            for i in range(2):
                V(lambda i=i: nc.vector.memset(xpad[i][:, 0:2], 0.0), [], [R("xpad", i)])
                V(lambda i=i: nc.vector.memset(xpad[i][:, S + 2:S + 4], 0.0), [], [R("xpad", i)])
            for ct in range(CTN):
                i = nxt("xpad", 2)
                DMA("sp", xpad[i][:, 2:S + 2], XBCT[ct * 128:(ct + 1) * 128, :], [R("xbcd", ct)], [R("xpad", i)], "ld")
                cb0 = (l * CTN + ct) * 6
                A(lambda i=i, cb0=cb0: nc.scalar.activation(out=acc[i], in_=xpad[i][:, 0:S], func=AF.Identity, scale=cp[:, cb0:cb0 + 1],
                                                          bias=cp[:, cb0 + 5:cb0 + 6]),
                  [R("xpad", i), R("cp")], [R("acc", i)])
                for k in range(1, 5):
                    V(lambda i=i, cb0=cb0, k=k: nc.vector.scalar_tensor_tensor(out=acc[i], in0=xpad[i][:, k:k + S], scalar=cp[:, cb0 + k:cb0 + k + 1],
                                                                            in1=acc[i], op0=ALU.mult, op1=ALU.add),
                      [R("xpad", i), R("cp"), R("acc", i)], [R("acc", i)])
                A(lambda i=i: nc.scalar.activation(out=acc[i], in_=acc[i], func=AF.Silu), [R("acc", i)], [R("acc", i)])
                isx = ct < DS // 128
                isb = (not isx) and ct < (DS + NB) // 128
                if not isx:
                    V(lambda i=i: nc.vector.tensor_copy(out=silb[i], in_=acc[i]), [R("acc", i)], [R("silb", i)])
                    chb = ct * 128 - DS - (0 if isb else NB)
                    DMA("sp", (BTd if isb else CTd)[chb:chb + 128, :], silb[i], [R("silb", i)], [R("btd" if isb else "ctd")], "st")
                if isx or isb:
                    stg = trf if isx else trb
                    rs = R("trf") if isx else R("trb")
                    for t0 in range(0, NT, 4):
                        p = nxt("ps", 6)

                        def tr(p=p, t0=t0, i=i):
                            ins = None
                            for k in range(min(4, NT - t0)):
                                ins = nc.tensor.transpose(ps[p][:, k * 128:(k + 1) * 128], acc[i][:, (t0 + k) * 128:(t0 + k + 1) * 128], ident_f)
                            return ins
                        T(tr, [R("acc", i), R("c")], [R("ps", p)])
                        nk = min(4, NT - t0)
                        V(lambda p=p, t0=t0, nk=nk, stg=stg: nc.vector.tensor_copy(out=stg[:, t0:t0 + nk, :],
                                                                                  in_=ps[p][:, 0:nk * 128].rearrange("p (a b) -> p a b", a=nk)),
                          [R("ps", p)], [rs])
                    if isx:
                        DMA("sp", XST[:, ct * 128:(ct + 1) * 128].rearrange("(t p) c -> p t c", p=128), trf, [rs], [R("xst")], "st")
                    else:
                        cb_ = ct * 128 - DS
                        DMA("sp", BTOK[:, cb_:cb_ + 128].rearrange("(t p) c -> p t c", p=128), trb, [rs], [R("btok")], "st")

        _ph1()
        if DBG.get('ph', 99) <= 1:
            return
        def _ph2():
            phase()
            rp = al([NROW])
            DMA("sp", rp, rowp[l:l + 1, :].partition_broadcast(128), [], [R("rp")], "c")
            dtb_bc = rp[:, 0:2 * NH]
            A_bc = al([2 * NH])
            A(lambda: nc.scalar.activation(out=A_bc, in_=rp[:, 2 * NH:4 * NH], func=AF.Exp), [R("rp")], [R("Abc")])
            V(lambda: nc.vector.tensor_scalar(out=A_bc, in0=A_bc, scalar1=-1.0, scalar2=None, op0=ALU.mult), [R("Abc")], [R("Abc")])
            dsk_bc = rp[:, 4 * NH:4 * NH + DS]
            sng_bc = rp[:, 4 * NH + DS:4 * NH + 2 * DS]
            gng_bc = rp[:, 4 * NH + 2 * DS:NROW]
            dtr = [al([2 * NH]) for _ in range(2)]
            dtt = [al([2 * NH]) for _ in range(2)]
            dta = [al([2 * NH]) for _ in range(2)]
            dte = [al([2 * NH]) for _ in range(2)]
            decb = [al([2 * NH]) for _ in range(2)]
            xsc = [al([NH, 64]) for _ in range(2)]
            bc = [al([NB], BF16) for _ in range(2)]
            xdts = [al([NH, 64], BF16) for _ in range(2)]
            cse = [al([DS]) for _ in range(2)]
            H2 = 2 * NH
            for c in range(NT):
                i = nxt("c1", 2)
                rows = slice(c * 128, (c + 1) * 128)
                DMA("sp", dtr[i], DTR[rows, :], [R("dtd", c)], [R("dtr", i)], "ld")
                DMA("sp", xsc[i], XST[rows, :].rearrange("p (h d) -> p h d", d=64), [R("xst")], [R("xsc", i)], "ld")
                DMA("sp", bc[i], BTOK[rows, :], [R("btok")], [R("bc", i)], "ld")
                V(lambda i=i: nc.vector.tensor_tensor(out=dtr[i], in0=dtr[i], in1=dtb_bc, op=ALU.add), [R("dtr", i), R("rp")], [R("dtr", i)])
                A(lambda i=i: nc.scalar.activation(out=dtr[i], in_=dtr[i], func=AF.Exp), [R("dtr", i)], [R("dtr", i)])
                A(lambda i=i: nc.scalar.activation(out=dtt[i], in_=dtr[i], func=AF.Ln, bias=1.0), [R("dtr", i)], [R("dtt", i)])
                V(lambda i=i: nc.vector.tensor_tensor(out=dta[i], in0=dtt[i], in1=A_bc, op=ALU.mult), [R("dtt", i), R("Abc")], [R("dta", i)])
                DMA("sp", DTd[rows, :], dtt[i], [R("dtt", i)], [R("DTd", c)], "st")
                DMA("sp", DTAd[rows, :], dta[i], [R("dta", i)], [R("DTAd", c)], "st")
                p = nxt("ps", 6)

                def mmx(p=p, i=i):
                    nc.tensor.matmul(ps[p][:, 0:NH], lhsT=Uf, rhs=dta[i][:, 0:NH], start=True, stop=True)
                    nc.tensor.matmul(ps[p][:, NH:H2], lhsT=Ub, rhs=dta[i][:, NH:H2], start=True, stop=True)
                    return nc.tensor.matmul(ps[p][:, 64:64 + H2], lhsT=ones_f, rhs=dta[i], start=True, stop=True)
                T(mmx, [R("dta", i), R("c")], [R("ps", p)])
                A(lambda p=p, i=i: nc.scalar.activation(out=dte[i], in_=ps[p][:, 0:H2], func=AF.Exp), [R("ps", p)], [R("dte", i)])
                A(lambda p=p, i=i: nc.scalar.activation(out=decb[i], in_=ps[p][:, 64:64 + H2], func=AF.Exp), [R("ps", p)], [R("decb", i)])
                DMA("sp", DECd[c], decb[i], [R("decb", i)], [R("DECd", c)], "st")
                V(lambda i=i: nc.vector.tensor_tensor(out=dte[i], in0=dte[i], in1=dtt[i], op=ALU.mult), [R("dte", i), R("dtt", i)], [R("dte", i)])
                for d_ in range(2):
                    j = nxt("xdts", 2)
                    V(lambda i=i, j=j, d_=d_: nc.vector.tensor_tensor(out=xdts[j], in0=xsc[i], in1=dte[i][:, d_ * NH:(d_ + 1) * NH].to_broadcast([128, NH, 64]),
                                                                     op=ALU.mult), [R("xsc", i), R("dte", i)], [R("xdts", j)])
                    for g0 in range(0, NG, 2):
                        p = nxt("ps", 6)

                        def mms(p=p, i=i, j=j, g0=g0):
                            ins = None
                            for g in range(g0, min(g0 + 2, NG)):
                                ins = nc.tensor.matmul(ps[p][:, (g - g0) * 256:(g - g0 + 1) * 256], lhsT=bc[i][:, g * 128:(g + 1) * 128],
                                                       rhs=xdts[j][:, g * 4:(g + 1) * 4, :].rearrange("p h d -> p (h d)"), start=True, stop=True)
                            return ins
                        T(mms, [R("bc", i), R("xdts", j)], [R("ps", p)])
                        ng = min(2, NG - g0)
                        V(lambda p=p, j=j, g0=g0, ng=ng: nc.vector.tensor_copy(out=cse[j][:, g0 * 256:(g0 + ng) * 256], in_=ps[p][:, 0:ng * 256]),
                          [R("ps", p)], [R("cse", j)])
                    DMA("sp", CSd[d_, c], cse[j], [R("cse", j)], [R("CSd", d_, c)], "st")
            state = al([NH, 64])
            csl = [al([NH, 64]) for _ in range(2)]
            decl = [al([2 * NH]) for _ in range(2)]
            spb = [al([DS], BF16) for _ in range(2)]
            for d_ in range(2):
                V(lambda: nc.vector.memset(state, 0.0), [R("state")], [R("state")])
                order = range(NT) if d_ == 0 else range(NT - 1, -1, -1)
                for c in order:
                    i = nxt("rec", 2)
                    DMA("sp", csl[i], CSd[d_, c].rearrange("p (h d) -> p h d", d=64), [R("CSd", d_, c)], [R("csl", i)], "ld")
                    DMA("sp", decl[i], DECd[c], [R("DECd", c)], [R("decl", i)], "ld")
                    V(lambda i=i: nc.vector.tensor_copy(out=spb[i], in_=state.rearrange("p h d -> p (h d)")), [R("state")], [R("spb", i)])
                    DMA("sp", SPd[d_, c], spb[i], [R("spb", i)], [R("SPd", d_, c)], "st")
                    V(lambda i=i, d_=d_: nc.vector.tensor_tensor(out=state, in0=state, in1=decl[i][:, d_ * NH:(d_ + 1) * NH].to_broadcast([128, NH, 64]),
                                                               op=ALU.mult), [R("state"), R("decl", i)], [R("state")])
                    V(lambda i=i: nc.vector.tensor_tensor(out=state, in0=state, in1=csl[i], op=ALU.add), [R("state"), R("csl", i)], [R("state")])

        _ph2()
        if DBG.get('ph', 99) <= 2:
            return
        def _ph3():
            phase()
            rp2 = al([2 * DS])
            DMA("sp", rp2, rowp[l:l + 1, 4 * NH:4 * NH + 2 * DS].partition_broadcast(128), [], [R("rp")], "c")
            dsk_bc = rp2[:, 0:DS]
            sng_bc = rp2[:, DS:2 * DS]
            dtt = [al([2 * NH]) for _ in range(2)]
            dta = [al([2 * NH]) for _ in range(2)]
            xsc = [al([NH, 64]) for _ in range(2)]
            zsc = [al([DS]) for _ in range(2)]
            btc = [al([NG, 128], BF16) for _ in range(2)]
            ctc = [al([NG, 128], BF16) for _ in range(2)]
            spc = [[al([DS], BF16) for _ in range(2)] for _ in range(2)]
            xdt = [[al([NH, 64], BF16) for _ in range(2)] for _ in range(2)]
            xd = [al([DS]) for _ in range(2)]
            ysb = [al([DS]) for _ in range(2)]
            ybf = al([DS], BF16)
            ytr = al([DS // 128, 128], BF16)
            Rt = [[al([4, 128]) for _ in range(2)] for _ in range(2)]
            dcy = [[al([4, 128]) for _ in range(2)] for _ in range(2)]
            scl = [[al([4, 128]) for _ in range(2)] for _ in range(2)]
            Mb = [[al([4, 128], BF16) for _ in range(2)] for _ in range(2)]
            Cs = [[al([4, 128], BF16) for _ in range(2)] for _ in range(2)]
            cbm = [al([2, 128]) for _ in range(2)]
            Tm = [Tf, Tb]
            Um = [Uf, Ub]

            def s1(i, g, q):
                for d_ in range(2):
                    for r in range(4):
                        hcol = d_ * NH + g * 4 + r
                        if (r + d_) % 3 == 0:
                            P.op("pool", lambda i=i, q=q, r=r, hcol=hcol, d_=d_: nc.gpsimd.tensor_scalar(out=Rt[q][d_][:, r, :], in0=Tm[d_], scalar1=dta[i][:, hcol:hcol + 1],
                                                                                                      scalar2=0.0, op0=ALU.mult, op1=ALU.add),
                                 reads=[R("dta", i), R("c")], writes=[R("Rt", q, d_, r)])
                        else:
                            V(lambda i=i, q=q, r=r, hcol=hcol, d_=d_: nc.vector.tensor_scalar(out=Rt[q][d_][:, r, :], in0=Tm[d_], scalar1=dta[i][:, hcol:hcol + 1],
                                                                                           scalar2=None, op0=ALU.mult),
                              [R("dta", i), R("c")], [R("Rt", q, d_, r)])
                pp = []
                for d_ in range(2):
                    p1 = nxt("ps", 6)
                    p2 = nxt("ps", 6)
                    pp.append((p1, p2))

                    def mseg(p1=p1, p2=p2, q=q, d_=d_):
                        ins = None
                        for r in range(4):
                            nc.tensor.matmul(ps[p1][:, r * 128:(r + 1) * 128], lhsT=Um[d_], rhs=Rt[q][d_][:, r, :], start=True, stop=True)
                            ins = nc.tensor.matmul(ps[p2][:, r * 128:(r + 1) * 128], lhsT=ones_f, rhs=Rt[q][d_][:, r, :], start=True, stop=True)
                        return ins
                    T(mseg, [R("Rt", q, d_, 0), R("Rt", q, d_, 1), R("Rt", q, d_, 2), R("Rt", q, d_, 3), R("c")], [R("ps", p1), R("ps", p2)])
                for d_ in range(2):
                    p1, p2 = pp[d_]
                    A(lambda p1=p1, q=q, d_=d_: nc.scalar.activation(out=dcy[q][d_].rearrange("p a b -> p (a b)"), in_=ps[p1][:, :], func=AF.Exp),
                      [R("ps", p1)], [R("dcy", q, d_)])
                    A(lambda p2=p2, q=q, d_=d_: nc.scalar.activation(out=scl[q][d_].rearrange("p a b -> p (a b)"), in_=ps[p2][:, :], func=AF.Exp),
                      [R("ps", p2)], [R("scl", q, d_)])

            def s2(i, g, q):
                pcb = nxt("ps", 6)
                T(lambda pcb=pcb, i=i, g=g: nc.tensor.matmul(ps[pcb][:, 0:128], lhsT=btc[i][:, g, :], rhs=ctc[i][:, g, :], start=True, stop=True),
                  [R("btc", i), R("ctc", i)], [R("ps", pcb)])
                for d_ in range(2):
                    V(lambda pcb=pcb, q=q, d_=d_: nc.vector.tensor_tensor(out=cbm[q][:, d_, :], in0=ps[pcb][:, 0:128], in1=Tm[d_], op=ALU.mult),
                      [R("ps", pcb), R("c")], [R("cbm", q)])
                for d_ in range(2):
                    for r in range(4):
                        V(lambda q=q, r=r, d_=d_: nc.vector.tensor_tensor(out=Mb[q][d_][:, r, :], in0=dcy[q][d_][:, r, :], in1=cbm[q][:, d_, :], op=ALU.mult),
                          [R("dcy", q, d_), R("cbm", q)], [R("Mb", q, d_)])
                        V(lambda q=q, r=r, i=i, g=g, d_=d_: nc.vector.tensor_tensor(out=Cs[q][d_][:, r, :], in0=scl[q][d_][:, r, :], in1=ctc[i][:, g, :], op=ALU.mult),
                          [R("scl", q, d_), R("ctc", i)], [R("Cs", q, d_)])
                py = 6 + nxt("py", 2)

                def mmy(py=py, q=q, i=i, g=g):
                    ins = None
                    for r in range(4):
                        h = g * 4 + r
                        for d_ in range(2):
                            nc.tensor.matmul(ps[py][:, r * 64:(r + 1) * 64], lhsT=Mb[q][d_][:, r, :], rhs=xdt[d_][i][:, h, :],
                                             start=(d_ == 0), stop=False)
                            ins = nc.tensor.matmul(ps[py][:, r * 64:(r + 1) * 64], lhsT=Cs[q][d_][:, r, :], rhs=spc[d_][i][:, h * 64:(h + 1) * 64],
                                                   start=False, stop=(d_ == 1))
                    return ins
                T(mmy, [R("Mb", q, 0), R("Cs", q, 0), R("Mb", q, 1), R("Cs", q, 1), R("xdt", 0, i), R("xdt", 1, i), R("spc", 0, i), R("spc", 1, i)],
                  [R("ps", py)])
                V(lambda py=py, i=i, g=g: nc.vector.tensor_tensor(out=ysb[i][:, g * 256:(g + 1) * 256], in0=ps[py][:, 0:256],
                                                                 in1=xd[i][:, g * 256:(g + 1) * 256], op=ALU.add),
                  [R("ps", py), R("xd", i)], [R("ysb", i)])

            for c in range(NT):
                i = nxt("c2", 2)
                rows = slice(c * 128, (c + 1) * 128)
                cols = slice(c * 128, (c + 1) * 128)
                DMA("sp", dtt[i], DTd[rows, :], [R("DTd", c)], [R("dtt", i)], "ld")
                DMA("sp", dta[i], DTAd[rows, :], [R("DTAd", c)], [R("dta", i)], "ld")
                DMA("sp", xsc[i], XST[rows, :].rearrange("p (h d) -> p h d", d=64), [R("xst")], [R("xsc", i)], "ld")
                DMA("sp", zsc[i], ZS[rows, :], [R("zd", c)], [R("zsc", i)], "ld")
                DMA("sp", btc[i], BTd[:, cols].rearrange("(g n) s -> n g s", n=128), [R("btd")], [R("btc", i)], "ld")
                DMA("sp", ctc[i], CTd[:, cols].rearrange("(g n) s -> n g s", n=128), [R("ctd")], [R("ctc", i)], "ld")
                for d_ in range(2):
                    DMA("sp", spc[d_][i], SPd[d_, c], [R("SPd", d_, c)], [R("spc", d_, i)], "ld")
                s1(i, 0, 0)
                for d_ in range(2):
                    V(lambda i=i, d_=d_: nc.vector.tensor_tensor(out=xdt[d_][i], in0=xsc[i], in1=dtt[i][:, d_ * NH:(d_ + 1) * NH].to_broadcast([128, NH, 64]),
                                                               op=ALU.mult), [R("xsc", i), R("dtt", i)], [R("xdt", d_, i)])
                V(lambda i=i: nc.vector.tensor_tensor(out=xd[i], in0=xsc[i].rearrange("p h d -> p (h d)"), in1=dsk_bc, op=ALU.mult),
                  [R("xsc", i), R("rp")], [R("xd", i)])
                for g in range(NG):
                    if g + 1 < NG:
                        s1(i, g + 1, (g + 1) % 2)
                    s2(i, g, g % 2)
                if "DBGY" in TAPS:
                    DMA("sp", DBGY[rows, :], ysb[i], [R("ysb", i)], [R("dbgy")], "st")
                V(lambda i=i: nc.vector.tensor_tensor(out=ysb[i], in0=ysb[i], in1=zsc[i], op=ALU.mult), [R("ysb", i), R("zsc", i)], [R("ysb", i)])
                for g in range(NG):
                    sumsq(ysb[i][:, g * 256:(g + 1) * 256], xd[i][:, g * 256:(g + 1) * 256], 8 + g, R("ysb", i), R("xd", i))
                rstd_from_ss(8, 8 + NG, NG, 256)
                for g in range(NG):
                    V(lambda i=i, g=g: nc.vector.scalar_tensor_tensor(out=ybf[:, g * 256:(g + 1) * 256], in0=ysb[i][:, g * 256:(g + 1) * 256],
                                                                     scalar=st[:, 8 + NG + g:8 + NG + g + 1], in1=sng_bc[:, g * 256:(g + 1) * 256],
                                                                     op0=ALU.mult, op1=ALU.mult),
                      [R("ysb", i), R("st"), R("rp")], [R("ybf")])
                for k0 in range(0, DS // 128, 8):
                    b = nxt("ps", 6)
                    pb = ps[b][:, :].bitcast(BF16)
                    nk = min(8, DS // 128 - k0)

                    def tr2(pb=pb, k0=k0, nk=nk):
                        ins = None
                        for k in range(nk):
                            ins = nc.tensor.transpose(pb[:, k * 128:(k + 1) * 128], ybf[:, (k0 + k) * 128:(k0 + k + 1) * 128], ident_b)
                        return ins
                    T(tr2, [R("ybf"), R("c")], [R("ps", b)])
                    V(lambda pb=pb, k0=k0, nk=nk: nc.vector.tensor_copy(out=ytr[:, k0:k0 + nk, :], in_=pb[:, 0:nk * 128].rearrange("p (a b) -> p a b", a=nk)),
                      [R("ps", b)], [R("ytr")])
                DMA("sp", YCT[0:DS, cols].rearrange("(k p) t -> p k t", p=128), ytr, [R("ytr")], [R("yct")], "st")
        _ph3()
        if DBG.get('ph', 99) <= 3:
            return
        def _ph4():
            phase()
            rp = al([NROW])
            DMA("sp", rp, rowp[l:l + 1, :].partition_broadcast(128), [], [R("rp")], "c")
            gng_bc = rp[:, 4 * NH + 2 * DS:NROW]
            wsT = al([NGG, 128], BF16)
            bsr = al([NGG * 128], BF16)
            DMA("pool", wsT, gws[l].rearrange("s (g t) -> s g t", g=NGG), [], [R("wsT")], "w")
            DMA("pool", bsr[0:1, :], gbs[l:l + 1, :], [], [R("bsr")], "w")
            vg = [al([DG]) for _ in range(2)]
            vj = [al([DG]) for _ in range(2)]
            vb = [al([DG], BF16) for _ in range(2)]
            utc = [al([NGG, 128]) for _ in range(2)]
            ygt = [al([NGG, 128], BF16) for _ in range(2)]
            for c in range(NT):
                i = nxt("gm", 2)
                rows = slice(c * 128, (c + 1) * 128)
                DMA("sp", vg[i], VG[rows, :], [R("vd", c)], [R("vg", i)], "ld")
                DMA("sp", utc[i], UT[:, rows].rearrange("(g d) t -> d g t", d=128), [R("ud")], [R("utc", i)], "ld")
                sumsq(vg[i], vj[i], 0, R("vg", i), R("vj", i))
                rstd_from_ss(0, 1, 1, DG)
                V(lambda i=i: nc.vector.scalar_tensor_tensor(out=vb[i], in0=vg[i], scalar=st[:, 1:2], in1=gng_bc, op0=ALU.mult, op1=ALU.mult),
                  [R("vg", i), R("st"), R("rp")], [R("vb", i)])
                for g0 in range(0, NGG, 4):
                    p = nxt("ps", 6)

                    def mmg(p=p, i=i, g0=g0):
                        ins = None
                        for gg in range(g0, min(g0 + 4, NGG)):
                            o = (gg - g0) * 128
                            nc.tensor.matmul(ps[p][:, o:o + 128], lhsT=vb[i][:, gg * 128:(gg + 1) * 128], rhs=wsT[:, gg, :], start=(gg == g0), stop=False)
                            ins = nc.tensor.matmul(ps[p][:, o:o + 128], lhsT=ones_b[0:1, :], rhs=bsr[0:1, gg * 128:(gg + 1) * 128], start=False,
                                                   stop=(gg == min(g0 + 4, NGG) - 1))
                        return ins
                    T(mmg, [R("vb", i), R("wsT"), R("bsr"), R("c")], [R("ps", p)])
                    ng = min(4, NGG - g0)
                    V(lambda p=p, i=i, g0=g0, ng=ng: nc.vector.tensor_tensor(out=ygt[i][:, g0:g0 + ng, :], in0=ps[p][:, 0:ng * 128].rearrange("p (a b) -> p a b", a=ng),
                                                                            in1=utc[i][:, g0:g0 + ng, :], op=ALU.mult),
                      [R("ps", p), R("utc", i)], [R("ygt", i)])
                DMA("sp", YCT[DS:D, rows].rearrange("(g d) t -> d g t", d=128), ygt[i], [R("ygt", i)], [R("yct")], "st")

        _ph4()
        if DBG.get('ph', 99) <= 4:
            return
        def _ph5():
            phase()
            act = al([KT, TB], BF16)
            wsl = [al([KT, SW], BF16) for _ in range(2)]
            ev = [al([SW]) for _ in range(3)]
            for tb in range(NTB):
                DMA("sp", act, YCT[:, tb * TB:(tb + 1) * TB].rearrange("(k p) t -> p k t", p=128), [R("yct")], [R("act")], "ld")
                proj_residual(act, R("act"), wsl, ev, tb, w_out[l])


        _ph5()
    def xattn(l):
        ML, XH = C.ML, C.XH
        MT = ML // 128
        ET = KT // XH
        scale = float(C.XD) ** -0.5

        def _x1():
            phase()
            xt = [al([D]) for _ in range(2)]
            xs = [al([D], BF16) for _ in range(2)]
            memT = al([KT, ML], BF16)
            wsl = [al([KT, SW], BF16) for _ in range(2)]
            evb = [al([SW], BF16) for _ in range(3)]
            for t in range(MT):
                i = nxt("xt", 2)
                norm_T(xt[i], xs[i], i, mem_in, "mem", t * 128, 4 * l + 2, memT, R("memT"), t * 128)
            for c0 in range(0, D, SW):
                wi = load_w(wsl, w_kv[l], c0, SW, KT)
                for s0 in range(0, SW, 128):
                    p = nxt("ps", 6)
                    T(mm_feat(ps[p][:, 0:ML], memT, 0, ML, wsl[wi], s0, KT), [R("memT"), R("wsl", wi)], [R("ps", p)])
                    e = nxt("evb", 3)
                    V(lambda p=p, e=e: nc.vector.tensor_copy(out=evb[e][:, 0:ML], in_=ps[p][:, 0:ML]), [R("ps", p)], [R("evb", e)])
                    DMA("sp", KTd[c0 + s0:c0 + s0 + 128, :], evb[e][:, 0:ML], [R("evb", e)], [R("ktd")], "st")
            for c0 in range(0, D, SW):
                wi = load_w(wsl, w_kv[l], D + c0, SW, KT)
                for t in range(MT):
                    p = nxt("ps", 6)
                    T(mm_tok(ps[p][:, 0:SW], memT, t * 128, wsl[wi], 0, SW, KT), [R("memT"), R("wsl", wi)], [R("ps", p)])
                    e = nxt("evb", 3)
                    V(lambda p=p, e=e: nc.vector.tensor_copy(out=evb[e], in_=ps[p][:, 0:SW]), [R("ps", p)], [R("evb", e)])
                    DMA("sp", Vd[t * 128:(t + 1) * 128, c0:c0 + SW], evb[e], [R("evb", e)], [R("vdd")], "st")
        _x1()
        if DBG.get('xph', 99) <= 0:
            return

        def _x2a():
            phase()
            xt = [al([D]) for _ in range(2)]
            xs = [al([D], BF16) for _ in range(2)]
            hT = al([KT, TB], BF16)
            wsl = [al([KT, SWA], BF16) for _ in range(2)]
            evb = [al([512], BF16) for _ in range(3)]
            for tb in range(NTB):
                for t in range(TPB):
                    i = nxt("xt", 2)
                    norm_T(xt[i], xs[i], i, xres, "xres", (tb * TPB + t) * 128, 4 * l + 1, hT, R("hT"), t * 128)
                for c0 in range(0, D, SWA):
                    wi = load_w(wsl, w_q[l], c0, SWA, KT)
                    for s0 in range(0, SWA, 128):
                        for h0 in range(0, TB, 512):
                            hw_ = min(512, TB - h0)
                            p = nxt("ps", 6)
                            T(mm_feat(ps[p][:, 0:hw_], hT, h0, hw_, wsl[wi], s0, KT), [R("hT"), R("wsl", wi)], [R("ps", p)])
                            e = nxt("evb", 3)
                            V(lambda p=p, e=e, hw_=hw_: nc.vector.tensor_copy(out=evb[e][:, 0:hw_], in_=ps[p][:, 0:hw_]), [R("ps", p)], [R("evb", e)])
                            DMA("sp", QT[c0 + s0:c0 + s0 + 128, tb * TB + h0:tb * TB + h0 + hw_], evb[e][:, 0:hw_], [R("evb", e)], [R("qtd")], "st")
        _x2a()
        if DBG.get('xph', 99) <= 1:
            return

        def _x2b():
            phase()
            KTs = al([KT, ML], BF16)
            Vs = al([MT, D], BF16)
            DMA("sp", KTs, KTd.rearrange("(k p) m -> p k m", p=128), [R("ktd")], [R("KTs")], "ld")
            DMA("sp", Vs, Vd.rearrange("(m p) d -> p m d", p=128), [R("vdd")], [R("Vs")], "ld")
            qT = [al([KT, TBA], BF16) for _ in range(2)]
            oTb = [al([KT, TBA], BF16) for _ in range(2)]
            pT = [al([MT, TBA], BF16) for _ in range(2)]
            pr = [al([ML]) for _ in range(2)]
            pb = [al([ML], BF16) for _ in range(2)]
            for tb in range(NTBA):
                qi = nxt("qT", 2)
                DMA("sp", qT[qi], QT[:, tb * TBA:(tb + 1) * TBA].rearrange("(k p) t -> p k t", p=128), [R("qtd")], [R("qT", qi)], "ld")
                for hd in range(XH):
                    pi = nxt("pT", 2)
                    for t in range(TPBA):
                        p = nxt("ps", 6)

                        def mms(p=p, qi=qi, hd=hd, t=t):
                            ins = None
                            for e in range(ET):
                                k = hd * ET + e
                                ins = nc.tensor.matmul(ps[p][:, 0:ML], lhsT=qT[qi][:, k, t * 128:(t + 1) * 128], rhs=KTs[:, k, :],
                                                       start=(e == 0), stop=(e == ET - 1))
                            return ins
                        T(mms, [R("qT", qi), R("KTs")], [R("ps", p)])
                        k2 = nxt("pr", 2)
                        V(lambda p=p: nc.vector.tensor_reduce(out=st[:, 32:33], in_=ps[p][:, 0:ML], axis=AX.X, op=ALU.max), [R("ps", p)], [R("st")])
                        V(lambda: nc.vector.tensor_scalar(out=st[:, 33:34], in0=st[:, 32:33], scalar1=-scale, scalar2=None, op0=ALU.mult), [R("st")], [R("st")])
                        A(lambda p=p, k2=k2: nc.scalar.activation(out=pr[k2], in_=ps[p][:, 0:ML], func=AF.Exp, bias=st[:, 33:34], scale=scale,
                                                                 accum_out=st[:, 34:35]), [R("ps", p), R("st")], [R("pr", k2), R("st")])
                        V(lambda: nc.vector.reciprocal(out=st[:, 35:36], in_=st[:, 34:35]), [R("st")], [R("st")])
                        V(lambda k2=k2: nc.vector.tensor_scalar(out=pb[k2], in0=pr[k2], scalar1=st[:, 35:36], scalar2=None, op0=ALU.mult),
                          [R("pr", k2), R("st")], [R("pb", k2)])
                        b = 6 + nxt("psb", 2)
                        pbv = ps[b][:, :].bitcast(BF16)

                        def trp(pbv=pbv, k2=k2):
                            ins = None
                            for m in range(MT):
                                ins = nc.tensor.transpose(pbv[:, m * 128:(m + 1) * 128], pb[k2][:, m * 128:(m + 1) * 128], ident_b)
                            return ins
                        T(trp, [R("pb", k2), R("c")], [R("ps", b)])
                        V(lambda pbv=pbv, pi=pi, t=t: nc.vector.tensor_copy(out=pT[pi][:, :, t * 128:(t + 1) * 128],
                                                                            in_=pbv[:, 0:MT * 128].rearrange("p (a b) -> p a b", a=MT)),
                          [R("ps", b)], [R("pT", pi)])
                    for dv in range(ET):
                        p = nxt("ps", 6)
                        k = hd * ET + dv

                        def mmo(p=p, pi=pi, k=k):
                            ins = None
                            for m in range(MT):
                                ins = nc.tensor.matmul(ps[p][:, 0:TBA], lhsT=Vs[:, m, k * 128:(k + 1) * 128], rhs=pT[pi][:, m, :],
                                                       start=(m == 0), stop=(m == MT - 1))
                            return ins
                        T(mmo, [R("Vs"), R("pT", pi)], [R("ps", p)])
                        V(lambda p=p, qi=qi, k=k: nc.vector.tensor_copy(out=oTb[qi][:, k, :], in_=ps[p][:, 0:TBA]), [R("ps", p)], [R("oTb", qi)])
                DMA("sp", YCT[:, tb * TBA:(tb + 1) * TBA].rearrange("(k p) t -> p k t", p=128), oTb[qi], [R("oTb", qi)], [R("yct")], "st")
        _x2b()
        if DBG.get('xph', 99) <= 2:
            return

        def _x2c():
            phase()
            act = al([KT, TB], BF16)
            wsl = [al([KT, SW], BF16) for _ in range(2)]
            ev = [al([SW]) for _ in range(3)]
            for tb in range(NTB):
                DMA("sp", act, YCT[:, tb * TB:(tb + 1) * TB].rearrange("(k p) t -> p k t", p=128), [R("yct")], [R("act")], "ld")
                proj_residual(act, R("act"), wsl, ev, tb, w_o[l])
        _x2c()


    def moe(l):
        NE, DE, CAP = C.NE, C.DE, C.CAP
        JT = CAP // 128
        FT = DE // 128
        NQ = NE * JT
        Ub_b = c_bf[:, 512:640]

        def _m1():
            phase()
            gbc = al([D])
            DMA("sp", gbc, gmoe_row[l:l + 1, :].partition_broadcast(128), [], [R("gbc")], "c")
            wr = al([KT, NE])
            DMA("sp", wr, w_router[l].rearrange("(k p) e -> p k e", p=128), [], [R("wr")], "c")
            xt = [al([D]) for _ in range(2)]
            h32 = [al([D]) for _ in range(2)]
            hb = [al([D], BF16) for _ in range(2)]
            hT32 = al([KT, 128])
            lg = [al([NE]) for _ in range(2)]
            aft = [al([NE]) for _ in range(2)]
            afs = [al([128]) for _ in range(2)]
            for tt in range(NT):
                i = nxt("xt", 2)
                rows = slice(tt * 128, (tt + 1) * 128)
                DMA("sp", xt[i], xres[rows, :], [R("xres", tt)], [R("xt", i)], "ld")
                sumsq(xt[i], h32[i], 0, R("xt", i), R("h32", i))
                rstd_from_ss(0, 1, 1, D)
                V(lambda i=i: nc.vector.scalar_tensor_tensor(out=h32[i], in0=xt[i], scalar=st[:, 1:2], in1=gbc, op0=ALU.mult, op1=ALU.mult),
                  [R("xt", i), R("st"), R("gbc")], [R("h32", i)])
                A(lambda i=i: nc.scalar.activation(out=hb[i], in_=h32[i], func=AF.Copy), [R("h32", i)], [R("hb", i)])
                DMA("sp", H3[rows, :], hb[i], [R("hb", i)], [R("h3d")], "st")
                for k0 in range(0, KT, 4):
                    p = nxt("ps", 6)
                    nk = min(4, KT - k0)

                    def tr(p=p, k0=k0, nk=nk, i=i):
                        ins = None
                        for k in range(nk):
                            ins = nc.tensor.transpose(ps[p][:, k * 128:(k + 1) * 128], h32[i][:, (k0 + k) * 128:(k0 + k + 1) * 128], ident_f)
                        return ins
                    T(tr, [R("h32", i), R("c")], [R("ps", p)])
                    V(lambda p=p, k0=k0, nk=nk: nc.vector.tensor_copy(out=hT32[:, k0:k0 + nk, :], in_=ps[p][:, 0:nk * 128].rearrange("p (a b) -> p a b", a=nk)),
                      [R("ps", p)], [R("hT32")])
                p = nxt("ps", 6)

                def mml(p=p):
                    ins = None
                    for kt in range(KT):
                        ins = nc.tensor.matmul(ps[p][:, 0:NE], lhsT=hT32[:, kt, :], rhs=wr[:, kt, :], start=(kt == 0), stop=(kt == KT - 1))
                    return ins
                T(mml, [R("hT32"), R("wr")], [R("ps", p)])
                V(lambda p=p: nc.vector.tensor_reduce(out=st[:, 32:33], in_=ps[p][:, 0:NE], axis=AX.X, op=ALU.max), [R("ps", p)], [R("st")])
                V(lambda: nc.vector.tensor_scalar(out=st[:, 33:34], in0=st[:, 32:33], scalar1=-1.0, scalar2=None, op0=ALU.mult), [R("st")], [R("st")])
                A(lambda p=p, i=i: nc.scalar.activation(out=lg[i], in_=ps[p][:, 0:NE], func=AF.Exp, bias=st[:, 33:34], scale=1.0, accum_out=st[:, 34:35]),
                  [R("ps", p), R("st")], [R("lg", i), R("st")])
                V(lambda: nc.vector.reciprocal(out=st[:, 35:36], in_=st[:, 34:35]), [R("st")], [R("st")])
                V(lambda i=i: nc.vector.tensor_scalar(out=aft[i], in0=lg[i], scalar1=st[:, 35:36], scalar2=None, op0=ALU.mult),
                  [R("lg", i), R("st")], [R("aft", i)])
                DMA("sp", AFF[rows, :], aft[i], [R("aft", i)], [R("affd")], "st")
                p2 = nxt("ps", 6)
                T(lambda p2=p2, i=i: nc.tensor.transpose(ps[p2][0:NE, 0:128], aft[i], ident_f), [R("aft", i), R("c")], [R("ps", p2)])
                V(lambda p2=p2, i=i: nc.vector.tensor_copy(out=afs[i][0:NE, :], in_=ps[p2][0:NE, 0:128]), [R("ps", p2)], [R("afs", i)])
                DMA("sp", AFFT[:, rows], afs[i][0:NE, :], [R("afs", i)], [R("afftd")], "st")
        _m1()
        if DBG.get('mph', 99) <= 0:
            return

        def _m2():
            phase()
            affall = al([NT, NE])
            DMA("sp", affall, AFF.rearrange("(t p) e -> p t e", p=128), [R("affd")], [R("affall")], "ld")
            rowbc = [al([S]) for _ in range(2)]
            junk = al([S])
            junk2 = al([S])
            naff = al([NT, NE])
            V(lambda: nc.vector.tensor_scalar(out=naff, in0=affall, scalar1=-1.0, scalar2=None, op0=ALU.mult), [R("affall")], [R("naff")])
            rank = al([NT, NE])
            sel = al([NT, NE])
            pos = al([NT, NE])
            selb = al([NT, NE], BF16)
            ahi = al([NT, NE], BF16)
            hif = al([NT, NE])
            ahl = al([NT * NE, 4], BF16)
            pidx = al([1])
            DMA("sp", pidx, pidx_in[:, :], [], [R("pidx")], "c")
            for e in range(NE):
                i = nxt("rowbc", 2)
                DMA("sp", rowbc[i], AFFT[e:e + 1, :].partition_broadcast(128), [R("afftd")], [R("rowbc", i)], "ld")
                for tt in range(NT):
                    if tt % 5 < 2:
                        V(lambda i=i, tt=tt, e=e: nc.vector.tensor_scalar(out=junk, in0=rowbc[i], scalar1=affall[:, tt, e:e + 1], scalar2=None,
                                                                         op0=ALU.is_gt, op1=ALU.add, accum_out=rank[:, tt, e:e + 1]),
                          [R("rowbc", i), R("affall")], [R("junk"), R("rank")])
                    else:
                        A(lambda i=i, tt=tt, e=e: nc.scalar.activation(out=junk2, in_=rowbc[i], func=AF.Sign, bias=naff[:, tt, e:e + 1], scale=1.0,
                                                                      accum_out=rank[:, tt, e:e + 1]),
                          [R("rowbc", i), R("naff")], [R("junk2"), R("rank2")])
            for tt in range(NT):
                if tt % 5 >= 2:
                    V(lambda tt=tt: nc.vector.tensor_scalar(out=rank[:, tt, :], in0=rank[:, tt, :], scalar1=float(S - 1), scalar2=0.5, op0=ALU.add, op1=ALU.mult),
                      [R("rank"), R("rank2")], [R("rank"), R("rank2")])
            V(lambda: nc.vector.tensor_scalar(out=sel, in0=rank, scalar1=float(CAP), scalar2=None, op0=ALU.is_lt), [R("rank"), R("rank2")], [R("sel")])
            V(lambda: nc.vector.tensor_copy(out=selb, in_=sel), [R("sel")], [R("selb")])
            for tt in range(NT):
                p = nxt("ps", 6)

                def mmp(p=p, tt=tt):
                    ins = nc.tensor.matmul(ps[p][:, 0:NE], lhsT=Ub_b, rhs=selb[:, tt, :], start=True, stop=(tt == 0))
                    for t2 in range(tt):
                        ins = nc.tensor.matmul(ps[p][:, 0:NE], lhsT=ones_b, rhs=selb[:, t2, :], start=False, stop=(t2 == tt - 1))
                    return ins
                T(mmp, [R("selb"), R("c")], [R("ps", p)])
                V(lambda p=p, tt=tt: nc.vector.tensor_copy(out=pos[:, tt, :], in_=ps[p][:, 0:NE]), [R("ps", p)], [R("pos")])
            V(lambda: nc.vector.tensor_copy(out=ahi, in_=affall), [R("affall")], [R("ahi")])
            V(lambda: nc.vector.tensor_copy(out=hif, in_=ahi), [R("ahi")], [R("hif")])
            V(lambda: nc.vector.tensor_tensor(out=hif, in0=affall, in1=hif, op=ALU.subtract), [R("affall"), R("hif")], [R("hif")])
            V(lambda: nc.vector.tensor_copy(out=ahl[:, :, 0], in_=ahi.rearrange("p a b -> p (a b)")), [R("ahi")], [R("ahl")])
            V(lambda: nc.vector.tensor_copy(out=ahl[:, :, 1], in_=hif.rearrange("p a b -> p (a b)")), [R("hif")], [R("ahl")])
            for tt in range(NT):
                V(lambda tt=tt: nc.vector.memset(ahl[:, tt * NE:(tt + 1) * NE, 2], float(tt)), [R("ahl")], [R("ahl")])
            V(lambda: nc.vector.tensor_scalar(out=ahl[:, :, 3], in0=hif.rearrange("p a b -> p (a b)"), scalar1=0.0, scalar2=pidx[:, 0:1],
                                              op0=ALU.mult, op1=ALU.add), [R("hif"), R("pidx"), R("ahl")], [R("ahl")])
            DMA("sp", SELd[:, :], sel.rearrange("p a b -> p (a b)"), [R("sel")], [R("seld")], "st")
            DMA("sp", POSd[:, :], pos.rearrange("p a b -> p (a b)"), [R("pos")], [R("posd")], "st")
            DMA("sp", AHLd[:, :], ahl.rearrange("p a b -> p (a b)"), [R("ahl")], [R("ahld")], "st")
        _m2()
        if DBG.get('mph', 99) <= 1:
            return

        phase()
        sel = al([NT * NE]); pos = al([NT * NE]); ahl = al([NT * NE, 4], BF16); iot = al([CAP])
        DMA("sp", sel, SELd[:, :], [R("seld")], [R("sel3")], "ld")
        DMA("sp", pos, POSd[:, :], [R("posd")], [R("pos3")], "ld")
        DMA("sp", ahl, AHLd.rearrange("p (a b) -> p a b", b=4), [R("ahld")], [R("ahl3")], "ld")
        DMA("sp", iot, iota_in[:, :], [], [R("iot")], "c")
        xsT = al([KT, CAP], BF16)
        actT = al([FT, CAP], BF16)
        gl = al([2, JT])
        gtmp = al([4])
        idxf = al([2])
        GCH = min(1024, D // 2)
        NCH = D // GCH
        H3v = H3.rearrange("s (c g) -> (s c) g", g=GCH)
        idxc = al([NCH])
        idxi = al([2 * JT * NCH]).bitcast(I32)
        xg = al([D], BF16)
        oh = al([NT, CAP], BF16)
        stg = [al([S], FP8) for _ in range(1)]
        bigb = [al([max(NT, KT), SW], BF16) for _ in range(2)]
        h3s = [bigb[i][:, 0:NT, :] for i in range(2)]
        wsl = [bigb[i][:, 0:KT, :] for i in range(2)]
        sg = [al([CAP]) for _ in range(1)]
        ygs = [al([SW], BF16) for _ in range(2)]

        def stA(e):
            for tt in range(NT):
                q = tt * NE + e
                V(lambda tt=tt, q=q: nc.vector.tensor_scalar(out=oh[:, tt, :], in0=iot, scalar1=pos[:, q:q + 1], scalar2=sel[:, q:q + 1],
                                                           op0=ALU.is_equal, op1=ALU.mult),
                  [R("iot"), R("pos3"), R("sel3")], [R("oh")])

        def stB(e):
            ep = e % 2
            for jt in range(JT):
                p = nxt("ps", 6)

                def mmg(p=p, jt=jt):
                    ins = None
                    for tt in range(NT):
                        ins = nc.tensor.matmul(ps[p][:, 0:4], lhsT=oh[:, tt, jt * 128:(jt + 1) * 128], rhs=ahl[:, tt * NE + e, :],
                                               start=(tt == 0), stop=(tt == NT - 1))
                    return ins
                T(mmg, [R("oh"), R("ahl3")], [R("ps", p)])
                V(lambda p=p: nc.vector.tensor_copy(out=gtmp, in_=ps[p][:, 0:4]), [R("ps", p)], [R("gtmp")])
                V(lambda jt=jt, ep=ep: nc.vector.tensor_tensor(out=gl[:, ep, jt:jt + 1], in0=gtmp[:, 0:1], in1=gtmp[:, 1:2], op=ALU.add), [R("gtmp")], [R("gl", ep)])
                V(lambda: nc.vector.scalar_tensor_tensor(out=idxf[:, 0:1], in0=gtmp[:, 2:3], scalar=128.0, in1=gtmp[:, 3:4], op0=ALU.mult, op1=ALU.add),
                  [R("gtmp")], [R("idxf")])
                for c_ in range(NCH):
                    V(lambda c_=c_: nc.vector.tensor_scalar(out=idxc[:, c_:c_ + 1], in0=idxf[:, 0:1], scalar1=float(NCH), scalar2=float(c_),
                                                          op0=ALU.mult, op1=ALU.add), [R("idxf")], [R("idxc")])
                io = (ep * JT + jt) * NCH
                V(lambda io=io: nc.vector.tensor_copy(out=idxi[:, io:io + NCH], in_=idxc), [R("idxc")], [R("idxi", ep, jt)])

        def stC(e):
            for jt in range(JT):
                si = nxt("stg", 1)
                for t0 in range(0, NT, 8):
                    b = 6 + nxt("psb", 2)
                    pbv = ps[b][:, :].bitcast(BF16)
                    nk = min(8, NT - t0)

                    def tro(pbv=pbv, t0=t0, nk=nk, jt=jt):
                        ins = None
                        for k in range(nk):
                            ins = nc.tensor.transpose(pbv[:, k * 128:(k + 1) * 128], oh[:, t0 + k, jt * 128:(jt + 1) * 128], ident_b)
                        return ins
                    T(tro, [R("oh"), R("c")], [R("ps", b)])
                    V(lambda pbv=pbv, t0=t0, nk=nk, si=si: nc.vector.tensor_copy(out=stg[si][:, t0 * 128:(t0 + nk) * 128], in_=pbv[:, 0:nk * 128], saturate=False),
                      [R("ps", b)], [R("stg", si)])
                qq = e * JT + jt
                DMA("sp", OHT[:, :, qq * 128:(qq + 1) * 128].rearrange("t p c -> p t c"), stg[si].rearrange("p (t c) -> p t c", c=128),
                    [R("stg", si)], [R("ohtd")], "st")

        def stD(e):
            ep = e % 2
            for jt in range(JT):
                for c_ in range(NCH):
                    io = (ep * JT + jt) * NCH + c_
                    P.op("pool", lambda io=io, c_=c_: nc.gpsimd.indirect_dma_start(out=xg[:, c_ * GCH:(c_ + 1) * GCH], out_offset=None, in_=H3v[:, :],
                                                                                 in_offset=bass.IndirectOffsetOnAxis(ap=idxi[:, io:io + 1], axis=0),
                                                                                 bounds_check=None, oob_is_err=True),
                         reads=[R("idxi", ep, jt), R("h3d")], writes=[R("xg")], stream="g")
                for k0 in range(0, KT, 8):
                    b2 = 6 + nxt("psb", 2)
                    pbv2 = ps[b2][:, :].bitcast(BF16)
                    nk2 = min(8, KT - k0)

                    def trg(pbv2=pbv2, k0=k0, nk2=nk2):
                        ins = None
                        for k in range(nk2):
                            ins = nc.tensor.transpose(pbv2[:, k * 128:(k + 1) * 128], xg[:, (k0 + k) * 128:(k0 + k + 1) * 128], ident_b)
                        return ins
                    T(trg, [R("xg"), R("c")], [R("ps", b2)])
                    V(lambda pbv2=pbv2, k0=k0, nk2=nk2, jt=jt: nc.vector.tensor_copy(out=xsT[:, k0:k0 + nk2, jt * 128:(jt + 1) * 128],
                                                                                   in_=pbv2[:, 0:nk2 * 128].rearrange("p (a b) -> p a b", a=nk2)),
                      [R("ps", b2)], [R("xsT")])

        def stE(e, mid=None):
            wgu = w_gate_up[l, e]
            for f2 in range(DE // 256):
                if f2 == 1 and mid is not None:
                    mid()
                    mid = None
                wi = nxt("w", 2)
                DMA("pool", wsl[wi][:, :, 0:256], wgu[:, f2 * 256:(f2 + 1) * 256].rearrange("(kt p) c -> p kt c", p=128), [], [R("wsl", wi)], "w")
                DMA("pool", wsl[wi][:, :, 256:512], wgu[:, DE + f2 * 256:DE + (f2 + 1) * 256].rearrange("(kt p) c -> p kt c", p=128), [], [R("wsl", wi)], "w")
                for sub in range(2):
                    pg = nxt("ps", 6)
                    pu = nxt("ps", 6)
                    T(mm_feat(ps[pg][:, 0:CAP], xsT, 0, CAP, wsl[wi], sub * 128, KT), [R("xsT"), R("wsl", wi)], [R("ps", pg)])
                    T(mm_feat(ps[pu][:, 0:CAP], xsT, 0, CAP, wsl[wi], 256 + sub * 128, KT), [R("xsT"), R("wsl", wi)], [R("ps", pu)])
                    gi_ = nxt("sg", 1)
                    A(lambda pg=pg, gi_=gi_: nc.scalar.activation(out=sg[gi_], in_=ps[pg][:, 0:CAP], func=AF.Silu), [R("ps", pg)], [R("sg", gi_)])
                    fi = f2 * 2 + sub
                    V(lambda pu=pu, gi_=gi_, fi=fi: nc.vector.tensor_tensor(out=actT[:, fi, :], in0=sg[gi_], in1=ps[pu][:, 0:CAP], op=ALU.mult),
                      [R("sg", gi_), R("ps", pu)], [R("actT")])
            if mid is not None:
                mid()

        def stF(e):
            ep = e % 2
            for dblk in range(D // SW):
                wi = load_w(wsl, w_down[l, e], dblk * SW, SW, FT)
                for jt in range(JT):
                    p = nxt("ps", 6)
                    T(mm_tok(ps[p][:, 0:SW], actT, jt * 128, wsl[wi], 0, SW, FT), [R("actT"), R("wsl", wi)], [R("ps", p)])
                    yi = nxt("ygs", 2)
                    V(lambda p=p, yi=yi, jt=jt: nc.vector.tensor_scalar(out=ygs[yi], in0=ps[p][:, 0:SW], scalar1=gl[:, ep, jt:jt + 1], scalar2=None, op0=ALU.mult),
                      [R("ps", p), R("gl", ep)], [R("ygs", yi)])
                    DMA("sp", YG[e * CAP + jt * 128:e * CAP + (jt + 1) * 128, dblk * SW:(dblk + 1) * SW], ygs[yi], [R("ygs", yi)], [R("ygd")], "st")

        stA(0); stB(0); stC(0); stD(0)
        for e in range(NE):
            if e + 1 < NE:
                stE(e, mid=lambda e=e: stA(e + 1))
                stB(e + 1)
            else:
                stE(e)
            stF(e)
            if e + 1 < NE:
                stC(e + 1)
                stD(e + 1)
        if DBG.get('mph', 99) <= 2:
            return

        def _m4():
            phase()
            ygs = al([NQ, SW], BF16)
            oht = [al([NQ, 128], FP8) for _ in range(2)]
            ev = [al([SW]) for _ in range(3)]
            for dblk in range(D // SW):
                DMA("sp", ygs, YG[:, dblk * SW:(dblk + 1) * SW].rearrange("(q p) c -> p q c", p=128), [R("ygd")], [R("ygs4")], "ld")
                for tt in range(NT):
                    oi = nxt("oht", 2)
                    DMA("sp", oht[oi], OHT[tt].rearrange("p (q t) -> p q t", t=128), [R("ohtd")], [R("oht", oi)], "ld")
                    p = nxt("ps", 6)

                    def mmc(p=p, oi=oi):
                        ins = None
                        for q in range(NQ):
                            ins = nc.tensor.matmul(ps[p][:, 0:SW], lhsT=oht[oi][:, q, :], rhs=ygs[:, q, :], start=(q == 0), stop=(q == NQ - 1))
                        return ins
                    T(mmc, [R("oht", oi), R("ygs4")], [R("ps", p)])
                    ei = nxt("ev", 3)
                    DMA("sp", ev[ei], xres[tt * 128:(tt + 1) * 128, dblk * SW:(dblk + 1) * SW], [R("xres", tt)], [R("ev", ei)], "ld")
                    V(lambda p=p, ei=ei: nc.vector.tensor_tensor(out=ev[ei], in0=ps[p][:, 0:SW], in1=ev[ei], op=ALU.add),
                      [R("ps", p), R("ev", ei)], [R("ev", ei)])
                    DMA("act", xres[tt * 128:(tt + 1) * 128, dblk * SW:(dblk + 1) * SW], ev[ei], [R("ev", ei)], [R("xres", tt)], "st")
        _m4()


    for tt in range(NT):
        DMA("sp", xres[tt * 128:(tt + 1) * 128, :], x_in[tt * 128:(tt + 1) * 128, :], [], [R("xres", tt)], "cp")

    for l in range(nlayers):
        if nstage >= 1:
            mixer(l)
        if nstage >= 2:
            xattn(l)
        if nstage >= 3:
            moe(l)
    phase()
    gfin = al([D])
    xt2 = [al([D]) for _ in range(2)]
    xs2 = [al([D], BF16) for _ in range(2)]
    DMA("sp", gfin, gfin_d[0:1, :].partition_broadcast(128), [], [R("gfin")], "c")
    for tt in range(NT):
        i = nxt("xt", 2)
        DMA("sp", xt2[i], xres[tt * 128:(tt + 1) * 128, :], [R("xres", tt)], [R("xt", i)], "ld")
        sumsq(xt2[i], xs2[i], 0, R("xt", i), R("xs", i))
        rstd_from_ss(0, 1, 1, D)
        V(lambda i=i: nc.vector.scalar_tensor_tensor(out=xt2[i], in0=xt2[i], scalar=st[:, 1:2], in1=gfin, op0=ALU.mult, op1=ALU.mult),
          [R("xt", i), R("st"), R("gfin")], [R("xt", i)])
        DMA("sp", out[tt * 128:(tt + 1) * 128, :], xt2[i], [R("xt", i)], [R("out")], "out")
    P.wait_all("sp", [R("out")])
    P.run()
    es.close()
    P.close()
    return nc


def host_consts():
    i = np.arange(128)
    ident = np.eye(128, dtype=np.float32)
    Tf = (i[:, None] <= i[None, :]).astype(np.float32)
    Uf = (i[:, None] > i[None, :]).astype(np.float32)
    Tb = (i[:, None] >= i[None, :]).astype(np.float32)
    Ub = (i[:, None] < i[None, :]).astype(np.float32)
    ones = np.ones((128, 128), np.float32)
    return np.concatenate([ident, Tf, Uf, Tb, Ub, ones], axis=1)


def host_layout(C, inp, b):
    L, KT = C.L, C.KT
    f = lambda a: np.ascontiguousarray(a, dtype=np.float32)
    gl = []
    for l in range(L):
        for n in ("norm_mix_g", "norm_xattn_g", "norm_mem_g", "norm_moe_g"):
            gl.append(inp[n][l].reshape(KT, 128).T)
    gl.append(inp["final_norm_g"].reshape(KT, 128).T)
    m = {
        "x": f(inp["x"][b]),
        "gk": f(np.concatenate(gl, axis=1)),
        "consts": host_consts(),
        "gfin_d": f(inp["final_norm_g"].reshape(1, -1)),
        "w_in": f(inp["w_in"]), "w_out": f(inp["w_out"]),
        "convp": f(np.concatenate([np.concatenate([inp["conv_w"][l].T, inp["conv_b"][l][:, None]], axis=1)
                                   .reshape(C.CC // 128, 128, 6).transpose(1, 0, 2).reshape(128, -1) for l in range(L)], axis=1)),
        "rowp": f(np.stack([np.concatenate([inp["dt_bias"][l].reshape(-1), inp["a_log"][l].reshape(-1), np.repeat(inp["d_skip"][l], 64),
                                            inp["ssd_norm_g"][l], inp["gmlp_norm_g"][l]]) for l in range(L)], axis=0)),
        "gws": f(np.stack([inp["gmlp_ws"][l].transpose(2, 0, 1).reshape(128, -1) for l in range(L)], axis=0)),
        "gbs": f(inp["gmlp_bs"].reshape(L, -1)),
        "gmoe_row": f(inp["norm_moe_g"]), "w_router": f(inp["w_router"]), "w_gate_up": f(inp["w_gate_up"]), "w_down": f(inp["w_down"]),
        "iota_in": np.ascontiguousarray(np.broadcast_to(np.arange(C.CAP, dtype=np.float32)[None, :], (128, C.CAP))),
        "pidx_in": np.arange(128, dtype=np.float32).reshape(128, 1),
        "mem": f(inp["mem"][b]), "w_q": f(inp["w_q"]), "w_kv": f(inp["w_kv"]), "w_o": f(inp["w_o"]),
        "w_in": f(inp["w_in"]),
        "w_out": f(inp["w_out"]),
    }
    return m


_NC_CACHE = {}


def run(C, inputs, n_cores=2, stop_after=None):
    key = (id(C), stop_after)
    if key not in _NC_CACHE:
        _NC_CACHE[key] = build(C, stop_after)
    nc = _NC_CACHE[key]
    used = set()
    for alloc in nc.allocations:
        try:
            if alloc.kind == "ExternalInput":
                used.add(alloc.memorylocations[0].name)
        except Exception:
            pass
    maps = []
    for b in range(n_cores):
        m = host_layout(C, inputs, b)
        maps.append({k: v for k, v in m.items() if (not used) or k in used})
    res = run_bass_kernel_spmd(nc, maps, core_ids=list(range(n_cores)))
    LAST["res"] = res.results
    return np.stack([res.results[b]["out"] for b in range(n_cores)], axis=0)


def kernel(**inputs):
    inputs = {k: np.asarray(v) for k, v in inputs.items()}
    return run(FULL, inputs, n_cores=2).astype(np.float32)
```

```python
import numpy as np
from contextlib import ExitStack
import concourse.bass as bass
import concourse.mybir as mybir
from concourse.bass_utils import run_bass_kernel_spmd

F32 = mybir.dt.float32
BF16 = mybir.dt.bfloat16
FP8 = mybir.dt.float8e4
I32 = mybir.dt.int32
AF = mybir.ActivationFunctionType
ALU = mybir.AluOpType
AX = mybir.AxisListType
ENGS = ("pe", "act", "dve", "pool", "sp")
EPS = 1e-6


class Res:
    __slots__ = ("name", "w", "r")

    def __init__(self, name):
        self.name = name
        self.w = None
        self.r = []


class Prog:
    def __init__(self, nc):
        self.nc = nc
        self.q = {e: [] for e in ENGS}
        self.cnt = {}
        self.sems = {}
        self.known = {e: {} for e in ENGS}
        self._ctx = []
        self.res = {}
        self.rr = {}
        for e in ("pe", "act", "dve", "pool"):
            self.sems[e] = self._sem("s_" + e)
            self.cnt[e] = 0

    def _sem(self, name):
        cm = self.nc.semaphore(name)
        s = cm.__enter__()
        self._ctx.append(cm)
        return s

    def R(self, *key):
        r = self.res.get(key)
        if r is None:
            r = self.res[key] = Res(str(key))
        return r

    def _waits_for(self, eng, reads, writes):
        need = {}
        for r in reads:
            if r.w is not None and need.get(r.w[0], 0) < r.w[1]:
                need[r.w[0]] = r.w[1]
        for r in writes:
            for t in ([r.w] if r.w is not None else []) + r.r:
                if need.get(t[0], 0) < t[1]:
                    need[t[0]] = t[1]
        out = []
        kn = self.known[eng]
        for k, v in need.items():
            if kn.get(k, 0) >= v:
                continue
            kn[k] = v
            out.append((self.sems[k], v))
        return out

    def op(self, eng, fn, reads=(), writes=(), stream=None):
        waits = self._waits_for(eng, reads, writes)
        if stream is not None:
            npool = {"sp": 40, "pool": 12, "act": 8}[eng]
            idx = self.rr.get(eng, 0) % npool
            self.rr[eng] = self.rr.get(eng, 0) + 1
            key = "d_%s_%d" % (eng, idx)
            if key not in self.sems:
                self.sems[key] = self._sem(key)
                self.cnt[key] = 0
            prev = self.cnt[key]
            if prev > self.known[eng].get(key, 0):
                self.known[eng][key] = prev
                waits.append((self.sems[key], prev))
            inc = 16
        else:
            key = eng
            inc = 1
        self.cnt[key] += inc
        tok = (key, self.cnt[key])
        for r in reads:
            r.r.append(tok)
        for r in writes:
            r.w = tok
            r.r = []
        self.q[eng].append((waits, fn, self.sems[key], inc))

    def barrier(self):
        for e in ENGS:
            wl = []
            kn = self.known[e]
            for k, v in self.cnt.items():
                if v > kn.get(k, 0):
                    kn[k] = v
                    wl.append((self.sems[k], v))
            self.q[e].append((wl, None, None, 0))

    def mark(self, name):
        self.q["pe"].append(([], ("mark", name), None, 0))

    def wait_all(self, eng, resources):
        self.q[eng].append((self._waits_for(eng, resources, []), None, None, 0))

    def run(self):
        nc = self.nc
        with nc.Block() as block:
            def mk(name):
                def body(e):
                    for wl, fn, semh, inc in self.q[name]:
                        for s, v in wl:
                            e.wait_ge(s, v)
                        if isinstance(fn, tuple):
                            MARKS.append((fn[1], PECOUNT[0]))
                        elif fn is not None:
                            fn().then_inc(semh, inc)
                return body
            block.tensor(mk("pe"))
            block.scalar(mk("act"))
            block.vector(mk("dve"))
            block.gpsimd(mk("pool"))
            block.sync(mk("sp"))

    def close(self):
        for cm in reversed(self._ctx):
            cm.__exit__(None, None, None)


class Cfg:
    def __init__(self, D=4096, S=4096, ML=256, XH=4, NE=16, DE=1536, L=2, NG=8, NGG=16):
        self.D, self.S, self.ML, self.XH, self.NE, self.DE, self.L = D, S, ML, XH, NE, DE, L
        self.NG, self.NGG = NG, NGG
        self.NH = 4 * NG
        self.DS = self.NH * 64
        self.DG = NGG * 128
        assert self.DS + self.DG == D
        self.CC = self.DS + 2 * NG * 128
        self.DIN = self.DS + self.CC + 2 * self.NH + 2 * self.DG
        self.KT = D // 128
        self.NT = S // 128
        self.CAP = 2 * S // NE
        self.XD = D // XH


FULL = Cfg()
TAPS = set()
MARKS = []
PECOUNT = [0]
DBG = {}
LAST = {}


def build(C, stop_after=None):
    nc = bass.Bass("TRN2", target_bir_lowering=False)
    P = Prog(nc)
    R = P.R
    D, S, KT, NT, L = C.D, C.S, C.KT, C.NT, C.L
    DS, DG, NH, NG, NGG, CC = C.DS, C.DG, C.NH, C.NG, C.NGG, C.CC
    NB = NG * 128
    TB = min(1024, S)
    NTB = S // TB
    TPB = TB // 128
    SW = 512
    SWA = 256
    TBA = min(512, S)
    NTBA = S // TBA
    TPBA = TBA // 128
    NROW = 4 * NH + 2 * DS + DG
    CTN = CC // 128
    stages = ["mix", "xattn", "moe"]
    nstage = {None: 3, "none": 0, "mix": 1, "xattn": 2, "moe": 3}[stop_after]
    nlayers = L if stop_after is None else (0 if stop_after == "none" else 1)

    def din(name, shape, dt=F32):
        return nc.dram_tensor(name, list(shape), dt, kind="ExternalInput").ap()

    def dscr(name, shape, dt=F32):
        if name in TAPS:
            return nc.dram_tensor(name, list(shape), dt, kind="ExternalOutput").ap()
        return nc.dram_tensor(name, list(shape), dt).ap()

    x_in = din("x", [S, D])
    out = nc.dram_tensor("out", [S, D], F32, kind="ExternalOutput").ap()
    gk = din("gk", [128, (4 * L + 1) * KT])
    consts = din("consts", [128, 6 * 128])
    gfin_d = din("gfin_d", [1, D])
    xres = dscr("xres", [S, D])
    if nlayers > 0:
        w_in = din("w_in", [L, D, C.DIN])
        w_out = din("w_out", [L, D, D])
        convp = din("convp", [128, L * CTN * 6])
        rowp = din("rowp", [L, NROW])
        gws = din("gws", [L, 128, NGG * 128])
        gbs = din("gbs", [L, NGG * 128])
        ZS = dscr("ZS", [S, DS]); XBCT = dscr("XBCT", [CC, S]); DTR = dscr("DTR", [S, 2 * NH])
        UT = dscr("UT", [DG, S]); VG = dscr("VG", [S, DG]); XST = dscr("XST", [S, DS])
        BTOK = dscr("BTOK", [S, NB], BF16); BTd = dscr("BTd", [NB, S], BF16); CTd = dscr("CTd", [NB, S], BF16)
        CSd = dscr("CSd", [2, NT, 128, DS]); DECd = dscr("DECd", [NT, 128, 2 * NH])
        SPd = dscr("SPd", [2, NT, 128, DS], BF16)
        DTd = dscr("DTd", [S, 2 * NH]); DTAd = dscr("DTAd", [S, 2 * NH])
        YCT = dscr("YCT", [D, S], BF16)
    if nlayers > 0 and nstage >= 3:
        NE_, CAP_ = C.NE, C.CAP
        gmoe_row = din("gmoe_row", [L, D]); w_router = din("w_router", [L, D, NE_])
        w_gate_up = din("w_gate_up", [L, NE_, D, 2 * C.DE]); w_down = din("w_down", [L, NE_, C.DE, D])
        iota_in = din("iota_in", [128, CAP_])
        pidx_in = din("pidx_in", [128, 1])
        H3 = dscr("H3", [S, D], BF16); AFF = dscr("AFF", [S, NE_]); AFFT = dscr("AFFT", [NE_, S])
        SELd = dscr("SELd", [128, NT * NE_]); POSd = dscr("POSd", [128, NT * NE_]); AHLd = dscr("AHLd", [128, NT * NE_ * 4], BF16)
        YG = dscr("YG", [NE_ * CAP_, D], BF16); OHT = dscr("OHT", [NT, 128, NE_ * (CAP_ // 128) * 128], FP8)
    if nlayers > 0 and nstage >= 2:
        mem_in = din("mem", [C.ML, D])
        w_q = din("w_q", [L, D, D]); w_kv = din("w_kv", [L, D, 2 * D]); w_o = din("w_o", [L, D, D])
        KTd = dscr("KTd", [D, C.ML], BF16); Vd = dscr("Vd", [C.ML, D], BF16); QT = dscr("QT", [D, S], BF16)
        DBGY = dscr("DBGY", [S, DS]) if "DBGY" in TAPS else None
        DBG2 = dscr("DBG2", [128, 2048]) if "DBG2" in TAPS else None

    es = ExitStack()

    def sb(name, shape, dt=F32):
        return es.enter_context(nc.sbuf_tensor(name, list(shape), dt))

    NBIG = 43000
    BIG = sb("big", [128, NBIG])
    gk_sb = sb("gk_sb", [128, (4 * L + 1) * KT])
    c_sb = sb("c_sb", [128, 6 * 128])
    c_bf = sb("c_bf", [128, 6 * 128], BF16)
    st = sb("stat", [128, 64])
    ident_f, Tf, Uf, Tb, Ub, ones_f = [c_sb[:, i * 128:(i + 1) * 128] for i in range(6)]
    ident_b = c_bf[:, 0:128]
    ones_b = c_bf[:, 640:768]
    ps = [es.enter_context(nc.psum_tensor(f"ps{i}", [128, 512], F32)) for i in range(8)]
    cnt = {}
    arena = {"off": 0}

    def nxt(k, n):
        v = cnt.get(k, 0)
        cnt[k] = v + 1
        return v % n

    def phase():
        import inspect
        P.mark(inspect.stack()[1].function + ":" + str(inspect.stack()[1].lineno))
        P.barrier()
        arena["off"] = 0

    def al(shape, dt=F32):
        n = int(np.prod(shape))
        words = n if dt == F32 else ((n + 3) // 4 if dt == FP8 else (n + 1) // 2)
        words = (words + 1) // 2 * 2
        o = arena["off"]
        assert o + words <= NBIG, (o, words)
        arena["off"] = o + words
        v = BIG[:, o:o + words]
        if dt != F32:
            v = v.bitcast(dt)[:, 0:n]
        else:
            v = v[:, 0:n]
        if len(shape) == 2:
            v = v.rearrange("p (a b) -> p a b", a=shape[0])
        elif len(shape) == 3:
            v = v.rearrange("p (a b c) -> p a b c", a=shape[0], b=shape[1])
        return v

    def DMA(q, out_, in_, reads, writes, stream):
        eng = {"sp": nc.sync, "pool": nc.gpsimd, "act": nc.scalar}[q]
        P.op(q, lambda: eng.dma_start(out=out_, in_=in_), reads=reads, writes=writes, stream=stream)

    def V(fn, reads, writes):
        P.op("dve", fn, reads=reads, writes=writes)

    def A(fn, reads, writes):
        P.op("act", fn, reads=reads, writes=writes)

    def T(fn, reads, writes):
        P.op("pe", fn, reads=reads, writes=writes)

    DMA("sp", gk_sb[:], gk[:, :], [], [R("gk")], "c")
    DMA("sp", c_sb[:], consts[:, :], [], [R("c")], "c")
    V(lambda: nc.vector.tensor_copy(out=c_bf[:], in_=c_sb[:]), [R("c")], [R("c")])

    def rstd_from_ss(col_in, col_out, n, width):
        V(lambda: nc.vector.tensor_scalar(out=st[:, col_out:col_out + n], in0=st[:, col_in:col_in + n], scalar1=1.0 / width,
                                          scalar2=EPS, op0=ALU.mult, op1=ALU.add), [R("st")], [R("st")])
        A(lambda: nc.scalar.activation(out=st[:, col_out:col_out + n], in_=st[:, col_out:col_out + n], func=AF.Sqrt), [R("st")], [R("st")])
        V(lambda: nc.vector.reciprocal(out=st[:, col_out:col_out + n], in_=st[:, col_out:col_out + n]), [R("st")], [R("st")])

    def sumsq(x_ap, junk_ap, col, rx, rjunk):
        A(lambda: nc.scalar.activation(out=junk_ap, in_=x_ap, func=AF.Square, accum_out=st[:, col:col + 1]), [rx], [rjunk, R("st")])

    def norm_T(xt, xs, ri, src_dram, sname, row0, gi, dst, rdst, col0):
        DMA("sp", xt, src_dram[row0:row0 + 128, :], [R(sname, row0 // 128)], [R("xt", ri)], "ld")
        sumsq(xt, xs, 0, R("xt", ri), R("xs", ri))
        rstd_from_ss(0, 1, 1, D)
        V(lambda: nc.vector.tensor_scalar(out=xs, in0=xt, scalar1=st[:, 1:2], scalar2=None, op0=ALU.mult),
          [R("xt", ri), R("st")], [R("xs", ri)])
        for k0 in range(0, KT, 8):
            b = 6 + nxt("psb", 2)
            nk = min(8, KT - k0)
            pb = ps[b][:, :].bitcast(BF16)

            def tr(pb=pb, k0=k0, nk=nk):
                ins = None
                for k in range(nk):
                    ins = nc.tensor.transpose(pb[:, k * 128:(k + 1) * 128], xs[:, (k0 + k) * 128:(k0 + k + 1) * 128], ident_b)
                return ins
            T(tr, [R("xs", ri), R("c")], [R("ps", b)])
            for k in range(nk):
                kt = k0 + k
                V(lambda pb=pb, k=k, kt=kt: nc.vector.tensor_scalar(
                    out=dst[:, kt, col0:col0 + 128], in0=pb[:, k * 128:(k + 1) * 128],
                    scalar1=gk_sb[:, gi * KT + kt:gi * KT + kt + 1], scalar2=None, op0=ALU.mult),
                    [R("ps", b), R("gk")], [rdst])

    def load_w(wsl, wdram_l, c0, width, kts):
        i = nxt("w", 2)
        src = wdram_l[:, c0:c0 + width].rearrange("(kt p) c -> p kt c", p=128)
        DMA("pool", wsl[i][:, 0:kts, 0:width], src, [], [R("wsl", i)], "w")
        return i

    def mm_tok(psum_ap, act, t0, w_ap, c0, width, kts):
        def f():
            ins = None
            for kt in range(kts):
                ins = nc.tensor.matmul(psum_ap, lhsT=act[:, kt, t0:t0 + 128], rhs=w_ap[:, kt, c0:c0 + width],
                                       start=(kt == 0), stop=(kt == kts - 1))
            return ins
        return f

    def mm_feat(psum_ap, act, t0, tw, w_ap, c0, kts):
        def f():
            ins = None
            for kt in range(kts):
                ins = nc.tensor.matmul(psum_ap, lhsT=w_ap[:, kt, c0:c0 + 128], rhs=act[:, kt, t0:t0 + tw],
                                       start=(kt == 0), stop=(kt == kts - 1))
            return ins
        return f

    def proj_residual(act, ract, wsl, ev, tb, wdram_l):
        for c0 in range(0, D, SW):
            wi = load_w(wsl, wdram_l, c0, SW, KT)
            for t in range(TPB):
                tt = tb * TPB + t
                p = nxt("ps", 6)
                T(mm_tok(ps[p][:, 0:SW], act, t * 128, wsl[wi], 0, SW, KT), [ract, R("wsl", wi)], [R("ps", p)])
                e = nxt("ev", 3)
                DMA("sp", ev[e], xres[tt * 128:(tt + 1) * 128, c0:c0 + SW], [R("xres", tt)], [R("ev", e)], "ld")
                V(lambda p=p, e=e: nc.vector.tensor_tensor(out=ev[e], in0=ps[p][:, 0:SW], in1=ev[e], op=ALU.add),
                  [R("ps", p), R("ev", e)], [R("ev", e)])
                DMA("act", xres[tt * 128:(tt + 1) * 128, c0:c0 + SW], ev[e], [R("ev", e)], [R("xres", tt)], "st")

    def mixer(l):
        o1 = DS; o2 = o1 + CC; o3 = o2 + 2 * NH; o4 = o3 + DG
        def _ph0():
            phase()
            xt = [al([D]) for _ in range(2)]
            xs = [al([D], BF16) for _ in range(2)]
            hT = al([KT, TB], BF16)
            wsl = [al([KT, SWA], BF16) for _ in range(2)]
            ev = [al([SW]) for _ in range(3)]
            wl = w_in[l]
            for tb in range(NTB):
                for t in range(TPB):
                    i = nxt("xt", 2)
                    norm_T(xt[i], xs[i], i, xres, "xres", (tb * TPB + t) * 128, 4 * l + 0, hT, R("hT"), t * 128)
                segs = [("z", c, min(SWA, o1 - c)) for c in range(0, o1, SWA)]
                segs += [("xbc", c, min(SWA, o2 - c)) for c in range(o1, o2, SWA)]
                segs += [("dt", o2, 2 * NH)]
                segs += [("u", c, min(SWA, o4 - c)) for c in range(o3, o4, SWA)]
                segs += [("v", c, min(SWA, C.DIN - c)) for c in range(o4, C.DIN, SWA)]
                for kind, c0, w in segs:
                    wi = load_w(wsl, wl, c0, w, KT)
                    if kind in ("z", "dt", "v"):
                        for t in range(TPB):
                            tt = tb * TPB + t
                            p = nxt("ps", 6)
                            T(mm_tok(ps[p][:, 0:w], hT, t * 128, wsl[wi], 0, w, KT), [R("hT"), R("wsl", wi)], [R("ps", p)])
                            e = nxt("ev", 3)
                            if kind == "dt":
                                V(lambda p=p, e=e, w=w: nc.vector.tensor_copy(out=ev[e][:, 0:w], in_=ps[p][:, 0:w]), [R("ps", p)], [R("ev", e)])
                                dst = DTR[tt * 128:(tt + 1) * 128, :]
                            else:
                                fn = AF.Silu if kind == "z" else AF.Gelu
                                A(lambda p=p, e=e, w=w, fn=fn: nc.scalar.activation(out=ev[e][:, 0:w], in_=ps[p][:, 0:w], func=fn),
                                  [R("ps", p)], [R("ev", e)])
                                if kind == "z":
                                    dst = ZS[tt * 128:(tt + 1) * 128, c0:c0 + w]
                                else:
                                    dst = VG[tt * 128:(tt + 1) * 128, c0 - o4:c0 - o4 + w]
                            DMA("sp" if kind == "dt" else "act", dst, ev[e][:, 0:w], [R("ev", e)], [R(kind + "d", tt)], "st")
                    else:
                        for s0 in range(0, w, 128):
                            for h0 in range(0, TB, 512):
                                hw_ = min(512, TB - h0)
                                p = nxt("ps", 6)
                                T(mm_feat(ps[p][:, 0:hw_], hT, h0, hw_, wsl[wi], s0, KT), [R("hT"), R("wsl", wi)], [R("ps", p)])
                                e = nxt("ev", 3)
                                if kind == "xbc":
                                    V(lambda p=p, e=e, hw_=hw_: nc.vector.tensor_copy(out=ev[e][:, 0:hw_], in_=ps[p][:, 0:hw_]), [R("ps", p)], [R("ev", e)])
                                    ch = c0 - o1 + s0
                                    dst = XBCT[ch:ch + 128, tb * TB + h0:tb * TB + h0 + hw_]
                                    rr = R("xbcd", ch // 128)
                                else:
                                    A(lambda p=p, e=e, hw_=hw_: nc.scalar.activation(out=ev[e][:, 0:hw_], in_=ps[p][:, 0:hw_], func=AF.Gelu), [R("ps", p)], [R("ev", e)])
                                    ch = c0 - o3 + s0
                                    dst = UT[ch:ch + 128, tb * TB + h0:tb * TB + h0 + hw_]
                                    rr = R("ud")
                                DMA("sp" if kind == "xbc" else "act", dst, ev[e][:, 0:hw_], [R("ev", e)], [rr], "st")
        _ph0()
        if DBG.get('ph', 99) <= 0:
            return
        def _ph1():
            phase()
            cp = al([L * CTN * 6])
            DMA("sp", cp, convp[:, :], [], [R("cp")], "c")
            xpad = [al([S + 4]) for _ in range(2)]
            acc = [al([S]) for _ in range(2)]
            silb = [al([S], BF16) for _ in range(2)]
            trf = al([NT, 128])
            trb = al([NT, 128], BF16)
            for i in range(2):
                V(lambda i=i: nc.vector.memset(xpad[i][:, 0:2], 0.0), [], [R("xpad", i)])
                V(lambda i=i: nc.vector.memset(xpad[i][:, S + 2:S + 4], 0.0), [], [R("xpad", i)])
            for ct in range(CTN):
                i = nxt("xpad", 2)
                DMA("sp", xpad[i][:, 2:S + 2], XBCT[ct * 128:(ct + 1) * 128, :], [R("xbcd", ct)], [R("xpad", i)], "ld")
                cb0 = (l * CTN + ct) * 6
                A(lambda i=i, cb0=cb0: nc.scalar.activation(out=acc[i], in_=xpad[i][:, 0:S], func=AF.Identity, scale=cp[:, cb0:cb0 + 1],
                                                          bias=cp[:, cb0 + 5:cb0 + 6]),
                  [R("xpad", i), R("cp")], [R("acc", i)])
                for k in range(1, 5):
                    V(lambda i=i, cb0=cb0, k=k: nc.vector.scalar_tensor_tensor(out=acc[i], in0=xpad[i][:, k:k + S], scalar=cp[:, cb0 + k:cb0 + k + 1],
                                                                            in1=acc[i], op0=ALU.mult, op1=ALU.add),
                      [R("xpad", i), R("cp"), R("acc", i)], [R("acc", i)])
                A(lambda i=i: nc.scalar.activation(out=acc[i], in_=acc[i], func=AF.Silu), [R("acc", i)], [R("acc", i)])
                isx = ct < DS // 128
                isb = (not isx) and ct < (DS + NB) // 128
                if not isx:
                    V(lambda i=i: nc.vector.tensor_copy(out=silb[i], in_=acc[i]), [R("acc", i)], [R("silb", i)])
                    chb = ct * 128 - DS - (0 if isb else NB)
                    DMA("sp", (BTd if isb else CTd)[chb:chb + 128, :], silb[i], [R("silb", i)], [R("btd" if isb else "ctd")], "st")
                if isx or isb:
                    stg = trf if isx else trb
                    rs = R("trf") if isx else R("trb")
                    for t0 in range(0, NT, 4):
                        p = nxt("ps", 6)

                        def tr(p=p, t0=t0, i=i):
                            ins = None
                            for k in range(min(4, NT - t0)):
                                ins = nc.tensor.transpose(ps[p][:, k * 128:(k + 1) * 128], acc[i][:, (t0 + k) * 128:(t0 + k + 1) * 128], ident_f)
                            return ins
                        T(tr, [R("acc", i), R("c")], [R("ps", p)])
                        nk = min(4, NT - t0)
                        V(lambda p=p, t0=t0, nk=nk, stg=stg: nc.vector.tensor_copy(out=stg[:, t0:t0 + nk, :],
                                                                                  in_=ps[p][:, 0:nk * 128].rearrange("p (a b) -> p a b", a=nk)),
                          [R("ps", p)], [rs])
                    if isx:
                        DMA("sp", XST[:, ct * 128:(ct + 1) * 128].rearrange("(t p) c -> p t c", p=128), trf, [rs], [R("xst")], "st")
                    else:
                        cb_ = ct * 128 - DS
                        DMA("sp", BTOK[:, cb_:cb_ + 128].rearrange("(t p) c -> p t c", p=128), trb, [rs], [R("btok")], "st")

        _ph1()
        if DBG.get('ph', 99) <= 1:
            return
        def _ph2():
            phase()
            rp = al([NROW])
            DMA("sp", rp, rowp[l:l + 1, :].partition_broadcast(128), [], [R("rp")], "c")
            dtb_bc = rp[:, 0:2 * NH]
            A_bc = al([2 * NH])
            A(lambda: nc.scalar.activation(out=A_bc, in_=rp[:, 2 * NH:4 * NH], func=AF.Exp), [R("rp")], [R("Abc")])
            V(lambda: nc.vector.tensor_scalar(out=A_bc, in0=A_bc, scalar1=-1.0, scalar2=None, op0=ALU.mult), [R("Abc")], [R("Abc")])
            dsk_bc = rp[:, 4 * NH:4 * NH + DS]
            sng_bc = rp[:, 4 * NH + DS:4 * NH + 2 * DS]
            gng_bc = rp[:, 4 * NH + 2 * DS:NROW]
            dtr = [al([2 * NH]) for _ in range(2)]
            dtt = [al([2 * NH]) for _ in range(2)]
            dta = [al([2 * NH]) for _ in range(2)]
            dte = [al([2 * NH]) for _ in range(2)]
            decb = [al([2 * NH]) for _ in range(2)]
            xsc = [al([NH, 64]) for _ in range(2)]
            bc = [al([NB], BF16) for _ in range(2)]
            xdts = [al([NH, 64], BF16) for _ in range(2)]
            cse = [al([DS]) for _ in range(2)]
            H2 = 2 * NH
            for c in range(NT):
                i = nxt("c1", 2)
                rows = slice(c * 128, (c + 1) * 128)
                DMA("sp", dtr[i], DTR[rows, :], [R("dtd", c)], [R("dtr", i)], "ld")
                DMA("sp", xsc[i], XST[rows, :].rearrange("p (h d) -> p h d", d=64), [R("xst")], [R("xsc", i)], "ld")
                DMA("sp", bc[i], BTOK[rows, :], [R("btok")], [R("bc", i)], "ld")
                V(lambda i=i: nc.vector.tensor_tensor(out=dtr[i], in0=dtr[i], in1=dtb_bc, op=ALU.add), [R("dtr", i), R("rp")], [R("dtr", i)])
                A(lambda i=i: nc.scalar.activation(out=dtr[i], in_=dtr[i], func=AF.Exp), [R("dtr", i)], [R("dtr", i)])
                A(lambda i=i: nc.scalar.activation(out=dtt[i], in_=dtr[i], func=AF.Ln, bias=1.0), [R("dtr", i)], [R("dtt", i)])
                V(lambda i=i: nc.vector.tensor_tensor(out=dta[i], in0=dtt[i], in1=A_bc, op=ALU.mult), [R("dtt", i), R("Abc")], [R("dta", i)])
                DMA("sp", DTd[rows, :], dtt[i], [R("dtt", i)], [R("DTd", c)], "st")
                DMA("sp", DTAd[rows, :], dta[i], [R("dta", i)], [R("DTAd", c)], "st")
                p = nxt("ps", 6)

                def mmx(p=p, i=i):
                    nc.tensor.matmul(ps[p][:, 0:NH], lhsT=Uf, rhs=dta[i][:, 0:NH], start=True, stop=True)
                    nc.tensor.matmul(ps[p][:, NH:H2], lhsT=Ub, rhs=dta[i][:, NH:H2], start=True, stop=True)
                    return nc.tensor.matmul(ps[p][:, 64:64 + H2], lhsT=ones_f, rhs=dta[i], start=True, stop=True)
                T(mmx, [R("dta", i), R("c")], [R("ps", p)])
                A(lambda p=p, i=i: nc.scalar.activation(out=dte[i], in_=ps[p][:, 0:H2], func=AF.Exp), [R("ps", p)], [R("dte", i)])
                A(lambda p=p, i=i: nc.scalar.activation(out=decb[i], in_=ps[p][:, 64:64 + H2], func=AF.Exp), [R("ps", p)], [R("decb", i)])
                DMA("sp", DECd[c], decb[i], [R("decb", i)], [R("DECd", c)], "st")
                V(lambda i=i: nc.vector.tensor_tensor(out=dte[i], in0=dte[i], in1=dtt[i], op=ALU.mult), [R("dte", i), R("dtt", i)], [R("dte", i)])
                for d_ in range(2):
                    j = nxt("xdts", 2)
                    V(lambda i=i, j=j, d_=d_: nc.vector.tensor_tensor(out=xdts[j], in0=xsc[i], in1=dte[i][:, d_ * NH:(d_ + 1) * NH].to_broadcast([128, NH, 64]),
                                                                     op=ALU.mult), [R("xsc", i), R("dte", i)], [R("xdts", j)])
                    for g0 in range(0, NG, 2):
                        p = nxt("ps", 6)

                        def mms(p=p, i=i, j=j, g0=g0):
                            ins = None
                            for g in range(g0, min(g0 + 2, NG)):
                                ins = nc.tensor.matmul(ps[p][:, (g - g0) * 256:(g - g0 + 1) * 256], lhsT=bc[i][:, g * 128:(g + 1) * 128],
                                                       rhs=xdts[j][:, g * 4:(g + 1) * 4, :].rearrange("p h d -> p (h d)"), start=True, stop=True)
                            return ins
                        T(mms, [R("bc", i), R("xdts", j)], [R("ps", p)])
                        ng = min(2, NG - g0)
                        V(lambda p=p, j=j, g0=g0, ng=ng: nc.vector.tensor_copy(out=cse[j][:, g0 * 256:(g0 + ng) * 256], in_=ps[p][:, 0:ng * 256]),
                          [R("ps", p)], [R("cse", j)])
                    DMA("sp", CSd[d_, c], cse[j], [R("cse", j)], [R("CSd", d_, c)], "st")
            state = al([NH, 64])
            csl = [al([NH, 64]) for _ in range(2)]
            decl = [al([2 * NH]) for _ in range(2)]
            spb = [al([DS], BF16) for _ in range(2)]
            for d_ in range(2):
                V(lambda: nc.vector.memset(state, 0.0), [R("state")], [R("state")])
                order = range(NT) if d_ == 0 else range(NT - 1, -1, -1)
                for c in order:
                    i = nxt("rec", 2)
                    DMA("sp", csl[i], CSd[d_, c].rearrange("p (h d) -> p h d", d=64), [R("CSd", d_, c)], [R("csl", i)], "ld")
                    DMA("sp", decl[i], DECd[c], [R("DECd", c)], [R("decl", i)], "ld")
                    V(lambda i=i: nc.vector.tensor_copy(out=spb[i], in_=state.rearrange("p h d -> p (h d)")), [R("state")], [R("spb", i)])
                    DMA("sp", SPd[d_, c], spb[i], [R("spb", i)], [R("SPd", d_, c)], "st")
                    V(lambda i=i, d_=d_: nc.vector.tensor_tensor(out=state, in0=state, in1=decl[i][:, d_ * NH:(d_ + 1) * NH].to_broadcast([128, NH, 64]),
                                                               op=ALU.mult), [R("state"), R("decl", i)], [R("state")])
                    V(lambda i=i: nc.vector.tensor_tensor(out=state, in0=state, in1=csl[i], op=ALU.add), [R("state"), R("csl", i)], [R("state")])

        _ph2()
        if DBG.get('ph', 99) <= 2:
            return
        def _ph3():
            phase()
            rp2 = al([2 * DS])
            DMA("sp", rp2, rowp[l:l + 1, 4 * NH:4 * NH + 2 * DS].partition_broadcast(128), [], [R("rp")], "c")
            dsk_bc = rp2[:, 0:DS]
            sng_bc = rp2[:, DS:2 * DS]
            dtt = [al([2 * NH]) for _ in range(2)]
            dta = [al([2 * NH]) for _ in range(2)]
            xsc = [al([NH, 64]) for _ in range(2)]
            zsc = [al([DS]) for _ in range(2)]
            btc = [al([NG, 128], BF16) for _ in range(2)]
            ctc = [al([NG, 128], BF16) for _ in range(2)]
            spc = [[al([DS], BF16) for _ in range(2)] for _ in range(2)]
            xdt = [[al([NH, 64], BF16) for _ in range(2)] for _ in range(2)]
            xd = [al([DS]) for _ in range(2)]
            ysb = [al([DS]) for _ in range(2)]
            ybf = al([DS], BF16)
            ytr = al([DS // 128, 128], BF16)
            Rt = [[al([4, 128]) for _ in range(2)] for _ in range(2)]
            dcy = [[al([4, 128]) for _ in range(2)] for _ in range(2)]
            scl = [[al([4, 128]) for _ in range(2)] for _ in range(2)]
            Mb = [[al([4, 128], BF16) for _ in range(2)] for _ in range(2)]
            Cs = [[al([4, 128], BF16) for _ in range(2)] for _ in range(2)]
            cbm = [al([2, 128]) for _ in range(2)]
            Tm = [Tf, Tb]
            Um = [Uf, Ub]

            def s1(i, g, q):
                for d_ in range(2):
                    for r in range(4):
                        hcol = d_ * NH + g * 4 + r
                        if (r + d_) % 3 == 0:
                            P.op("pool", lambda i=i, q=q, r=r, hcol=hcol, d_=d_: nc.gpsimd.tensor_scalar(out=Rt[q][d_][:, r, :], in0=Tm[d_], scalar1=dta[i][:, hcol:hcol + 1],
                                                                                                      scalar2=0.0, op0=ALU.mult, op1=ALU.add),
                                 reads=[R("dta", i), R("c")], writes=[R("Rt", q, d_, r)])
                        else:
                            V(lambda i=i, q=q, r=r, hcol=hcol, d_=d_: nc.vector.tensor_scalar(out=Rt[q][d_][:, r, :], in0=Tm[d_], scalar1=dta[i][:, hcol:hcol + 1],
                                                                                           scalar2=None, op0=ALU.mult),
                              [R("dta", i), R("c")], [R("Rt", q, d_, r)])
                pp = []
                for d_ in range(2):
                    p1 = nxt("ps", 6)
                    p2 = nxt("ps", 6)
                    pp.append((p1, p2))

                    def mseg(p1=p1, p2=p2, q=q, d_=d_):
                        ins = None
                        for r in range(4):
                            nc.tensor.matmul(ps[p1][:, r * 128:(r + 1) * 128], lhsT=Um[d_], rhs=Rt[q][d_][:, r, :], start=True, stop=True)
                            ins = nc.tensor.matmul(ps[p2][:, r * 128:(r + 1) * 128], lhsT=ones_f, rhs=Rt[q][d_][:, r, :], start=True, stop=True)
                        return ins
                    T(mseg, [R("Rt", q, d_, 0), R("Rt", q, d_, 1), R("Rt", q, d_, 2), R("Rt", q, d_, 3), R("c")], [R("ps", p1), R("ps", p2)])
                for d_ in range(2):
                    p1, p2 = pp[d_]
                    A(lambda p1=p1, q=q, d_=d_: nc.scalar.activation(out=dcy[q][d_].rearrange("p a b -> p (a b)"), in_=ps[p1][:, :], func=AF.Exp),
                      [R("ps", p1)], [R("dcy", q, d_)])
                    A(lambda p2=p2, q=q, d_=d_: nc.scalar.activation(out=scl[q][d_].rearrange("p a b -> p (a b)"), in_=ps[p2][:, :], func=AF.Exp),
                      [R("ps", p2)], [R("scl", q, d_)])

            def s2(i, g, q):
                pcb = nxt("ps", 6)
                T(lambda pcb=pcb, i=i, g=g: nc.tensor.matmul(ps[pcb][:, 0:128], lhsT=btc[i][:, g, :], rhs=ctc[i][:, g, :], start=True, stop=True),
                  [R("btc", i), R("ctc", i)], [R("ps", pcb)])
                for d_ in range(2):
                    V(lambda pcb=pcb, q=q, d_=d_: nc.vector.tensor_tensor(out=cbm[q][:, d_, :], in0=ps[pcb][:, 0:128], in1=Tm[d_], op=ALU.mult),
                      [R("ps", pcb), R("c")], [R("cbm", q)])
                for d_ in range(2):
                    for r in range(4):
                        V(lambda q=q, r=r, d_=d_: nc.vector.tensor_tensor(out=Mb[q][d_][:, r, :], in0=dcy[q][d_][:, r, :], in1=cbm[q][:, d_, :], op=ALU.mult),
                          [R("dcy", q, d_), R("cbm", q)], [R("Mb", q, d_)])
                        V(lambda q=q, r=r, i=i, g=g, d_=d_: nc.vector.tensor_tensor(out=Cs[q][d_][:, r, :], in0=scl[q][d_][:, r, :], in1=ctc[i][:, g, :], op=ALU.mult),
                          [R("scl", q, d_), R("ctc", i)], [R("Cs", q, d_)])
                py = 6 + nxt("py", 2)

                def mmy(py=py, q=q, i=i, g=g):
                    ins = None
                    for r in range(4):
                        h = g * 4 + r
                        for d_ in range(2):
                            nc.tensor.matmul(ps[py][:, r * 64:(r + 1) * 64], lhsT=Mb[q][d_][:, r, :], rhs=xdt[d_][i][:, h, :],
                                             start=(d_ == 0), stop=False)
                            ins = nc.tensor.matmul(ps[py][:, r * 64:(r + 1) * 64], lhsT=Cs[q][d_][:, r, :], rhs=spc[d_][i][:, h * 64:(h + 1) * 64],
                                                   start=False, stop=(d_ == 1))
                    return ins
                T(mmy, [R("Mb", q, 0), R("Cs", q, 0), R("Mb", q, 1), R("Cs", q, 1), R("xdt", 0, i), R("xdt", 1, i), R("spc", 0, i), R("spc", 1, i)],
                  [R("ps", py)])
                V(lambda py=py, i=i, g=g: nc.vector.tensor_tensor(out=ysb[i][:, g * 256:(g + 1) * 256], in0=ps[py][:, 0:256],
                                                                 in1=xd[i][:, g * 256:(g + 1) * 256], op=ALU.add),
                  [R("ps", py), R("xd", i)], [R("ysb", i)])

            for c in range(NT):
                i = nxt("c2", 2)
                rows = slice(c * 128, (c + 1) * 128)
                cols = slice(c * 128, (c + 1) * 128)
                DMA("sp", dtt[i], DTd[rows, :], [R("DTd", c)], [R("dtt", i)], "ld")
                DMA("sp", dta[i], DTAd[rows, :], [R("DTAd", c)], [R("dta", i)], "ld")
                DMA("sp", xsc[i], XST[rows, :].rearrange("p (h d) -> p h d", d=64), [R("xst")], [R("xsc", i)], "ld")
                DMA("sp", zsc[i], ZS[rows, :], [R("zd", c)], [R("zsc", i)], "ld")
                DMA("sp", btc[i], BTd[:, cols].rearrange("(g n) s -> n g s", n=128), [R("btd")], [R("btc", i)], "ld")
                DMA("sp", ctc[i], CTd[:, cols].rearrange("(g n) s -> n g s", n=128), [R("ctd")], [R("ctc", i)], "ld")
                for d_ in range(2):
                    DMA("sp", spc[d_][i], SPd[d_, c], [R("SPd", d_, c)], [R("spc", d_, i)], "ld")
                s1(i, 0, 0)
                for d_ in range(2):
                    V(lambda i=i, d_=d_: nc.vector.tensor_tensor(out=xdt[d_][i], in0=xsc[i], in1=dtt[i][:, d_ * NH:(d_ + 1) * NH].to_broadcast([128, NH, 64]),
                                                               op=ALU.mult), [R("xsc", i), R("dtt", i)], [R("xdt", d_, i)])
                V(lambda i=i: nc.vector.tensor_tensor(out=xd[i], in0=xsc[i].rearrange("p h d -> p (h d)"), in1=dsk_bc, op=ALU.mult),
                  [R("xsc", i), R("rp")], [R("xd", i)])
                for g in range(NG):
                    if g + 1 < NG:
                        s1(i, g + 1, (g + 1) % 2)
                    s2(i, g, g % 2)
                if "DBGY" in TAPS:
                    DMA("sp", DBGY[rows, :], ysb[i], [R("ysb", i)], [R("dbgy")], "st")
                V(lambda i=i: nc.vector.tensor_tensor(out=ysb[i], in0=ysb[i], in1=zsc[i], op=ALU.mult), [R("ysb", i), R("zsc", i)], [R("ysb", i)])
                for g in range(NG):
                    sumsq(ysb[i][:, g * 256:(g + 1) * 256], xd[i][:, g * 256:(g + 1) * 256], 8 + g, R("ysb", i), R("xd", i))
                rstd_from_ss(8, 8 + NG, NG, 256)
                for g in range(NG):
                    V(lambda i=i, g=g: nc.vector.scalar_tensor_tensor(out=ybf[:, g * 256:(g + 1) * 256], in0=ysb[i][:, g * 256:(g + 1) * 256],
                                                                     scalar=st[:, 8 + NG + g:8 + NG + g + 1], in1=sng_bc[:, g * 256:(g + 1) * 256],
                                                                     op0=ALU.mult, op1=ALU.mult),
                      [R("ysb", i), R("st"), R("rp")], [R("ybf")])
                for k0 in range(0, DS // 128, 8):
                    b = nxt("ps", 6)
                    pb = ps[b][:, :].bitcast(BF16)
                    nk = min(8, DS // 128 - k0)

                    def tr2(pb=pb, k0=k0, nk=nk):
                        ins = None
                        for k in range(nk):
                            ins = nc.tensor.transpose(pb[:, k * 128:(k + 1) * 128], ybf[:, (k0 + k) * 128:(k0 + k + 1) * 128], ident_b)
                        return ins
                    T(tr2, [R("ybf"), R("c")], [R("ps", b)])
                    V(lambda pb=pb, k0=k0, nk=nk: nc.vector.tensor_copy(out=ytr[:, k0:k0 + nk, :], in_=pb[:, 0:nk * 128].rearrange("p (a b) -> p a b", a=nk)),
                      [R("ps", b)], [R("ytr")])
                DMA("sp", YCT[0:DS, cols].rearrange("(k p) t -> p k t", p=128), ytr, [R("ytr")], [R("yct")], "st")
        _ph3()
        if DBG.get('ph', 99) <= 3:
            return
        def _ph4():
            phase()
            rp = al([NROW])
            DMA("sp", rp, rowp[l:l + 1, :].partition_broadcast(128), [], [R("rp")], "c")
            gng_bc = rp[:, 4 * NH + 2 * DS:NROW]
            wsT = al([NGG, 128], BF16)
            bsr = al([NGG * 128], BF16)
            DMA("pool", wsT, gws[l].rearrange("s (g t) -> s g t", g=NGG), [], [R("wsT")], "w")
            DMA("pool", bsr[0:1, :], gbs[l:l + 1, :], [], [R("bsr")], "w")
            vg = [al([DG]) for _ in range(2)]
            vj = [al([DG]) for _ in range(2)]
            vb = [al([DG], BF16) for _ in range(2)]
            utc = [al([NGG, 128]) for _ in range(2)]
            ygt = [al([NGG, 128], BF16) for _ in range(2)]
            for c in range(NT):
                i = nxt("gm", 2)
                rows = slice(c * 128, (c + 1) * 128)
                DMA("sp", vg[i], VG[rows, :], [R("vd", c)], [R("vg", i)], "ld")
                DMA("sp", utc[i], UT[:, rows].rearrange("(g d) t -> d g t", d=128), [R("ud")], [R("utc", i)], "ld")
                sumsq(vg[i], vj[i], 0, R("vg", i), R("vj", i))
                rstd_from_ss(0, 1, 1, DG)
                V(lambda i=i: nc.vector.scalar_tensor_tensor(out=vb[i], in0=vg[i], scalar=st[:, 1:2], in1=gng_bc, op0=ALU.mult, op1=ALU.mult),
                  [R("vg", i), R("st"), R("rp")], [R("vb", i)])
                for g0 in range(0, NGG, 4):
                    p = nxt("ps", 6)

                    def mmg(p=p, i=i, g0=g0):
                        ins = None
                        for gg in range(g0, min(g0 + 4, NGG)):
                            o = (gg - g0) * 128
                            nc.tensor.matmul(ps[p][:, o:o + 128], lhsT=vb[i][:, gg * 128:(gg + 1) * 128], rhs=wsT[:, gg, :], start=(gg == g0), stop=False)
                            ins = nc.tensor.matmul(ps[p][:, o:o + 128], lhsT=ones_b[0:1, :], rhs=bsr[0:1, gg * 128:(gg + 1) * 128], start=False,
                                                   stop=(gg == min(g0 + 4, NGG) - 1))
                        return ins
                    T(mmg, [R("vb", i), R("wsT"), R("bsr"), R("c")], [R("ps", p)])
                    ng = min(4, NGG - g0)
                    V(lambda p=p, i=i, g0=g0, ng=ng: nc.vector.tensor_tensor(out=ygt[i][:, g0:g0 + ng, :], in0=ps[p][:, 0:ng * 128].rearrange("p (a b) -> p a b", a=ng),
                                                                            in1=utc[i][:, g0:g0 + ng, :], op=ALU.mult),
                      [R("ps", p), R("utc", i)], [R("ygt", i)])
                DMA("sp", YCT[DS:D, rows].rearrange("(g d) t -> d g t", d=128), ygt[i], [R("ygt", i)], [R("yct")], "st")

        _ph4()
        if DBG.get('ph', 99) <= 4:
            return
        def _ph5():
            phase()
            act = al([KT, TB], BF16)
            wsl = [al([KT, SW], BF16) for _ in range(2)]
            ev = [al([SW]) for _ in range(3)]
            for tb in range(NTB):
                DMA("sp", act, YCT[:, tb * TB:(tb + 1) * TB].rearrange("(k p) t -> p k t", p=128), [R("yct")], [R("act")], "ld")
                proj_residual(act, R("act"), wsl, ev, tb, w_out[l])


        _ph5()
    def xattn(l):
        ML, XH = C.ML, C.XH
        MT = ML // 128
        ET = KT // XH
        scale = float(C.XD) ** -0.5

        def _x1():
            phase()
            xt = [al([D]) for _ in range(2)]
            xs = [al([D], BF16) for _ in range(2)]
            memT = al([KT, ML], BF16)
            wsl = [al([KT, SW], BF16) for _ in range(2)]
            evb = [al([SW], BF16) for _ in range(3)]
            for t in range(MT):
                i = nxt("xt", 2)
                norm_T(xt[i], xs[i], i, mem_in, "mem", t * 128, 4 * l + 2, memT, R("memT"), t * 128)
            for c0 in range(0, D, SW):
                wi = load_w(wsl, w_kv[l], c0, SW, KT)
                for s0 in range(0, SW, 128):
                    p = nxt("ps", 6)
                    T(mm_feat(ps[p][:, 0:ML], memT, 0, ML, wsl[wi], s0, KT), [R("memT"), R("wsl", wi)], [R("ps", p)])
                    e = nxt("evb", 3)
                    V(lambda p=p, e=e: nc.vector.tensor_copy(out=evb[e][:, 0:ML], in_=ps[p][:, 0:ML]), [R("ps", p)], [R("evb", e)])
                    DMA("sp", KTd[c0 + s0:c0 + s0 + 128, :], evb[e][:, 0:ML], [R("evb", e)], [R("ktd")], "st")
            for c0 in range(0, D, SW):
                wi = load_w(wsl, w_kv[l], D + c0, SW, KT)
                for t in range(MT):
                    p = nxt("ps", 6)
                    T(mm_tok(ps[p][:, 0:SW], memT, t * 128, wsl[wi], 0, SW, KT), [R("memT"), R("wsl", wi)], [R("ps", p)])
                    e = nxt("evb", 3)
                    V(lambda p=p, e=e: nc.vector.tensor_copy(out=evb[e], in_=ps[p][:, 0:SW]), [R("ps", p)], [R("evb", e)])
                    DMA("sp", Vd[t * 128:(t + 1) * 128, c0:c0 + SW], evb[e], [R("evb", e)], [R("vdd")], "st")
        _x1()
        if DBG.get('xph', 99) <= 0:
            return

        def _x2a():
            phase()
            xt = [al([D]) for _ in range(2)]
            xs = [al([D], BF16) for _ in range(2)]
            hT = al([KT, TB], BF16)
            wsl = [al([KT, SWA], BF16) for _ in range(2)]
            evb = [al([512], BF16) for _ in range(3)]
            for tb in range(NTB):
                for t in range(TPB):
                    i = nxt("xt", 2)
                    norm_T(xt[i], xs[i], i, xres, "xres", (tb * TPB + t) * 128, 4 * l + 1, hT, R("hT"), t * 128)
                for c0 in range(0, D, SWA):
                    wi = load_w(wsl, w_q[l], c0, SWA, KT)
                    for s0 in range(0, SWA, 128):
                        for h0 in range(0, TB, 512):
                            hw_ = min(512, TB - h0)
                            p = nxt("ps", 6)
                            T(mm_feat(ps[p][:, 0:hw_], hT, h0, hw_, wsl[wi], s0, KT), [R("hT"), R("wsl", wi)], [R("ps", p)])
                            e = nxt("evb", 3)
                            V(lambda p=p, e=e, hw_=hw_: nc.vector.tensor_copy(out=evb[e][:, 0:hw_], in_=ps[p][:, 0:hw_]), [R("ps", p)], [R("evb", e)])
                            DMA("sp", QT[c0 + s0:c0 + s0 + 128, tb * TB + h0:tb * TB + h0 + hw_], evb[e][:, 0:hw_], [R("evb", e)], [R("qtd")], "st")
        _x2a()
        if DBG.get('xph', 99) <= 1:
            return

        def _x2b():
            phase()
            KTs = al([KT, ML], BF16)
            Vs = al([MT, D], BF16)
            DMA("sp", KTs, KTd.rearrange("(k p) m -> p k m", p=128), [R("ktd")], [R("KTs")], "ld")
            DMA("sp", Vs, Vd.rearrange("(m p) d -> p m d", p=128), [R("vdd")], [R("Vs")], "ld")
            qT = [al([KT, TBA], BF16) for _ in range(2)]
            oTb = [al([KT, TBA], BF16) for _ in range(2)]
            pT = [al([MT, TBA], BF16) for _ in range(2)]
            pr = [al([ML]) for _ in range(2)]
            pb = [al([ML], BF16) for _ in range(2)]
            for tb in range(NTBA):
                qi = nxt("qT", 2)
                DMA("sp", qT[qi], QT[:, tb * TBA:(tb + 1) * TBA].rearrange("(k p) t -> p k t", p=128), [R("qtd")], [R("qT", qi)], "ld")
                for hd in range(XH):
                    pi = nxt("pT", 2)
                    for t in range(TPBA):
                        p = nxt("ps", 6)

                        def mms(p=p, qi=qi, hd=hd, t=t):
                            ins = None
                            for e in range(ET):
                                k = hd * ET + e
                                ins = nc.tensor.matmul(ps[p][:, 0:ML], lhsT=qT[qi][:, k, t * 128:(t + 1) * 128], rhs=KTs[:, k, :],
                                                       start=(e == 0), stop=(e == ET - 1))
                            return ins
                        T(mms, [R("qT", qi), R("KTs")], [R("ps", p)])
                        k2 = nxt("pr", 2)
                        V(lambda p=p: nc.vector.tensor_reduce(out=st[:, 32:33], in_=ps[p][:, 0:ML], axis=AX.X, op=ALU.max), [R("ps", p)], [R("st")])
                        V(lambda: nc.vector.tensor_scalar(out=st[:, 33:34], in0=st[:, 32:33], scalar1=-scale, scalar2=None, op0=ALU.mult), [R("st")], [R("st")])
                        A(lambda p=p, k2=k2: nc.scalar.activation(out=pr[k2], in_=ps[p][:, 0:ML], func=AF.Exp, bias=st[:, 33:34], scale=scale,
                                                                 accum_out=st[:, 34:35]), [R("ps", p), R("st")], [R("pr", k2), R("st")])
                        V(lambda: nc.vector.reciprocal(out=st[:, 35:36], in_=st[:, 34:35]), [R("st")], [R("st")])
                        V(lambda k2=k2: nc.vector.tensor_scalar(out=pb[k2], in0=pr[k2], scalar1=st[:, 35:36], scalar2=None, op0=ALU.mult),
                          [R("pr", k2), R("st")], [R("pb", k2)])
                        b = 6 + nxt("psb", 2)
                        pbv = ps[b][:, :].bitcast(BF16)

                        def trp(pbv=pbv, k2=k2):
                            ins = None
                            for m in range(MT):
                                ins = nc.tensor.transpose(pbv[:, m * 128:(m + 1) * 128], pb[k2][:, m * 128:(m + 1) * 128], ident_b)
                            return ins
                        T(trp, [R("pb", k2), R("c")], [R("ps", b)])
                        V(lambda pbv=pbv, pi=pi, t=t: nc.vector.tensor_copy(out=pT[pi][:, :, t * 128:(t + 1) * 128],
                                                                            in_=pbv[:, 0:MT * 128].rearrange("p (a b) -> p a b", a=MT)),
                          [R("ps", b)], [R("pT", pi)])
                    for dv in range(ET):
                        p = nxt("ps", 6)
                        k = hd * ET + dv

                        def mmo(p=p, pi=pi, k=k):
                            ins = None
                            for m in range(MT):
                                ins = nc.tensor.matmul(ps[p][:, 0:TBA], lhsT=Vs[:, m, k * 128:(k + 1) * 128], rhs=pT[pi][:, m, :],
                                                       start=(m == 0), stop=(m == MT - 1))
                            return ins
                        T(mmo, [R("Vs"), R("pT", pi)], [R("ps", p)])
                        V(lambda p=p, qi=qi, k=k: nc.vector.tensor_copy(out=oTb[qi][:, k, :], in_=ps[p][:, 0:TBA]), [R("ps", p)], [R("oTb", qi)])
                DMA("sp", YCT[:, tb * TBA:(tb + 1) * TBA].rearrange("(k p) t -> p k t", p=128), oTb[qi], [R("oTb", qi)], [R("yct")], "st")
        _x2b()
        if DBG.get('xph', 99) <= 2:
            return

        def _x2c():
            phase()
            act = al([KT, TB], BF16)
            wsl = [al([KT, SW], BF16) for _ in range(2)]
            ev = [al([SW]) for _ in range(3)]
            for tb in range(NTB):
                DMA("sp", act, YCT[:, tb * TB:(tb + 1) * TB].rearrange("(k p) t -> p k t", p=128), [R("yct")], [R("act")], "ld")
                proj_residual(act, R("act"), wsl, ev, tb, w_o[l])
        _x2c()


    def moe(l):
        NE, DE, CAP = C.NE, C.DE, C.CAP
        JT = CAP // 128
        FT = DE // 128
        NQ = NE * JT
        Ub_b = c_bf[:, 512:640]

        def _m1():
            phase()
            gbc = al([D])
            DMA("sp", gbc, gmoe_row[l:l + 1, :].partition_broadcast(128), [], [R("gbc")], "c")
            wr = al([KT, NE])
            DMA("sp", wr, w_router[l].rearrange("(k p) e -> p k e", p=128), [], [R("wr")], "c")
            xt = [al([D]) for _ in range(2)]
            h32 = [al([D]) for _ in range(2)]
            hb = [al([D], BF16) for _ in range(2)]
            hT32 = al([KT, 128])
            lg = [al([NE]) for _ in range(2)]
            aft = [al([NE]) for _ in range(2)]
            afs = [al([128]) for _ in range(2)]
            for tt in range(NT):
                i = nxt("xt", 2)
                rows = slice(tt * 128, (tt + 1) * 128)
                DMA("sp", xt[i], xres[rows, :], [R("xres", tt)], [R("xt", i)], "ld")
                sumsq(xt[i], h32[i], 0, R("xt", i), R("h32", i))
                rstd_from_ss(0, 1, 1, D)
                V(lambda i=i: nc.vector.scalar_tensor_tensor(out=h32[i], in0=xt[i], scalar=st[:, 1:2], in1=gbc, op0=ALU.mult, op1=ALU.mult),
                  [R("xt", i), R("st"), R("gbc")], [R("h32", i)])
                A(lambda i=i: nc.scalar.activation(out=hb[i], in_=h32[i], func=AF.Copy), [R("h32", i)], [R("hb", i)])
                DMA("sp", H3[rows, :], hb[i], [R("hb", i)], [R("h3d")], "st")
                for k0 in range(0, KT, 4):
                    p = nxt("ps", 6)
                    nk = min(4, KT - k0)

                    def tr(p=p, k0=k0, nk=nk, i=i):
                        ins = None
                        for k in range(nk):
                            ins = nc.tensor.transpose(ps[p][:, k * 128:(k + 1) * 128], h32[i][:, (k0 + k) * 128:(k0 + k + 1) * 128], ident_f)
                        return ins
                    T(tr, [R("h32", i), R("c")], [R("ps", p)])
                    V(lambda p=p, k0=k0, nk=nk: nc.vector.tensor_copy(out=hT32[:, k0:k0 + nk, :], in_=ps[p][:, 0:nk * 128].rearrange("p (a b) -> p a b", a=nk)),
                      [R("ps", p)], [R("hT32")])
                p = nxt("ps", 6)

                def mml(p=p):
                    ins = None
                    for kt in range(KT):
                        ins = nc.tensor.matmul(ps[p][:, 0:NE], lhsT=hT32[:, kt, :], rhs=wr[:, kt, :], start=(kt == 0), stop=(kt == KT - 1))
                    return ins
                T(mml, [R("hT32"), R("wr")], [R("ps", p)])
                V(lambda p=p: nc.vector.tensor_reduce(out=st[:, 32:33], in_=ps[p][:, 0:NE], axis=AX.X, op=ALU.max), [R("ps", p)], [R("st")])
                V(lambda: nc.vector.tensor_scalar(out=st[:, 33:34], in0=st[:, 32:33], scalar1=-1.0, scalar2=None, op0=ALU.mult), [R("st")], [R("st")])
                A(lambda p=p, i=i: nc.scalar.activation(out=lg[i], in_=ps[p][:, 0:NE], func=AF.Exp, bias=st[:, 33:34], scale=1.0, accum_out=st[:, 34:35]),
                  [R("ps", p), R("st")], [R("lg", i), R("st")])
                V(lambda: nc.vector.reciprocal(out=st[:, 35:36], in_=st[:, 34:35]), [R("st")], [R("st")])
                V(lambda i=i: nc.vector.tensor_scalar(out=aft[i], in0=lg[i], scalar1=st[:, 35:36], scalar2=None, op0=ALU.mult),
                  [R("lg", i), R("st")], [R("aft", i)])
                DMA("sp", AFF[rows, :], aft[i], [R("aft", i)], [R("affd")], "st")
                p2 = nxt("ps", 6)
                T(lambda p2=p2, i=i: nc.tensor.transpose(ps[p2][0:NE, 0:128], aft[i], ident_f), [R("aft", i), R("c")], [R("ps", p2)])
                V(lambda p2=p2, i=i: nc.vector.tensor_copy(out=afs[i][0:NE, :], in_=ps[p2][0:NE, 0:128]), [R("ps", p2)], [R("afs", i)])
                DMA("sp", AFFT[:, rows], afs[i][0:NE, :], [R("afs", i)], [R("afftd")], "st")
        _m1()
        if DBG.get('mph', 99) <= 0:
            return

        def _m2():
            phase()
            affall = al([NT, NE])
            DMA("sp", affall, AFF.rearrange("(t p) e -> p t e", p=128), [R("affd")], [R("affall")], "ld")
            rowbc = [al([S]) for _ in range(2)]
            junk = al([S])
            junk2 = al([S])
            naff = al([NT, NE])
            V(lambda: nc.vector.tensor_scalar(out=naff, in0=affall, scalar1=-1.0, scalar2=None, op0=ALU.mult), [R("affall")], [R("naff")])
            rank = al([NT, NE])
            sel = al([NT, NE])
            pos = al([NT, NE])
            selb = al([NT, NE], BF16)
            ahi = al([NT, NE], BF16)
            hif = al([NT, NE])
            ahl = al([NT * NE, 4], BF16)
            pidx = al([1])
            DMA("sp", pidx, pidx_in[:, :], [], [R("pidx")], "c")
            for e in range(NE):
                i = nxt("rowbc", 2)
                DMA("sp", rowbc[i], AFFT[e:e + 1, :].partition_broadcast(128), [R("afftd")], [R("rowbc", i)], "ld")
                for tt in range(NT):
                    if tt % 5 < 2:
                        V(lambda i=i, tt=tt, e=e: nc.vector.tensor_scalar(out=junk, in0=rowbc[i], scalar1=affall[:, tt, e:e + 1], scalar2=None,
                                                                         op0=ALU.is_gt, op1=ALU.add, accum_out=rank[:, tt, e:e + 1]),
                          [R("rowbc", i), R("affall")], [R("junk"), R("rank")])
                    else:
                        A(lambda i=i, tt=tt, e=e: nc.scalar.activation(out=junk2, in_=rowbc[i], func=AF.Sign, bias=naff[:, tt, e:e + 1], scale=1.0,
                                                                      accum_out=rank[:, tt, e:e + 1]),
                          [R("rowbc", i), R("naff")], [R("junk2"), R("rank2")])
            for tt in range(NT):
                if tt % 5 >= 2:
                    V(lambda tt=tt: nc.vector.tensor_scalar(out=rank[:, tt, :], in0=rank[:, tt, :], scalar1=float(S - 1), scalar2=0.5, op0=ALU.add, op1=ALU.mult),
                      [R("rank"), R("rank2")], [R("rank"), R("rank2")])
            V(lambda: nc.vector.tensor_scalar(out=sel, in0=rank, scalar1=float(CAP), scalar2=None, op0=ALU.is_lt), [R("rank"), R("rank2")], [R("sel")])
            V(lambda: nc.vector.tensor_copy(out=selb, in_=sel), [R("sel")], [R("selb")])
            for tt in range(NT):
                p = nxt("ps", 6)

                def mmp(p=p, tt=tt):
                    ins = nc.tensor.matmul(ps[p][:, 0:NE], lhsT=Ub_b, rhs=selb[:, tt, :], start=True, stop=(tt == 0))
                    for t2 in range(tt):
                        ins = nc.tensor.matmul(ps[p][:, 0:NE], lhsT=ones_b, rhs=selb[:, t2, :], start=False, stop=(t2 == tt - 1))
                    return ins
                T(mmp, [R("selb"), R("c")], [R("ps", p)])
                V(lambda p=p, tt=tt: nc.vector.tensor_copy(out=pos[:, tt, :], in_=ps[p][:, 0:NE]), [R("ps", p)], [R("pos")])
            V(lambda: nc.vector.tensor_copy(out=ahi, in_=affall), [R("affall")], [R("ahi")])
            V(lambda: nc.vector.tensor_copy(out=hif, in_=ahi), [R("ahi")], [R("hif")])
            V(lambda: nc.vector.tensor_tensor(out=hif, in0=affall, in1=hif, op=ALU.subtract), [R("affall"), R("hif")], [R("hif")])
            V(lambda: nc.vector.tensor_copy(out=ahl[:, :, 0], in_=ahi.rearrange("p a b -> p (a b)")), [R("ahi")], [R("ahl")])
            V(lambda: nc.vector.tensor_copy(out=ahl[:, :, 1], in_=hif.rearrange("p a b -> p (a b)")), [R("hif")], [R("ahl")])
            for tt in range(NT):
                V(lambda tt=tt: nc.vector.memset(ahl[:, tt * NE:(tt + 1) * NE, 2], float(tt)), [R("ahl")], [R("ahl")])
            V(lambda: nc.vector.tensor_scalar(out=ahl[:, :, 3], in0=hif.rearrange("p a b -> p (a b)"), scalar1=0.0, scalar2=pidx[:, 0:1],
                                              op0=ALU.mult, op1=ALU.add), [R("hif"), R("pidx"), R("ahl")], [R("ahl")])
            DMA("sp", SELd[:, :], sel.rearrange("p a b -> p (a b)"), [R("sel")], [R("seld")], "st")
            DMA("sp", POSd[:, :], pos.rearrange("p a b -> p (a b)"), [R("pos")], [R("posd")], "st")
            DMA("sp", AHLd[:, :], ahl.rearrange("p a b -> p (a b)"), [R("ahl")], [R("ahld")], "st")
        _m2()
        if DBG.get('mph', 99) <= 1:
            return

        phase()
        sel = al([NT * NE]); pos = al([NT * NE]); ahl = al([NT * NE, 4], BF16); iot = al([CAP])
        DMA("sp", sel, SELd[:, :], [R("seld")], [R("sel3")], "ld")
        DMA("sp", pos, POSd[:, :], [R("posd")], [R("pos3")], "ld")
        DMA("sp", ahl, AHLd.rearrange("p (a b) -> p a b", b=4), [R("ahld")], [R("ahl3")], "ld")
        DMA("sp", iot, iota_in[:, :], [], [R("iot")], "c")
        xsT = al([KT, CAP], BF16)
        actT = al([FT, CAP], BF16)
        gl = al([2, JT])
        gtmp = al([4])
        idxf = al([2])
        GCH = min(1024, D // 2)
        NCH = D // GCH
        H3v = H3.rearrange("s (c g) -> (s c) g", g=GCH)
        idxc = al([NCH])
        idxi = al([2 * JT * NCH]).bitcast(I32)
        xg = al([D], BF16)
        oh = al([NT, CAP], BF16)
        stg = [al([S], FP8) for _ in range(1)]
        bigb = [al([max(NT, KT), SW], BF16) for _ in range(2)]
        h3s = [bigb[i][:, 0:NT, :] for i in range(2)]
        wsl = [bigb[i][:, 0:KT, :] for i in range(2)]
        sg = [al([CAP]) for _ in range(1)]
        ygs = [al([SW], BF16) for _ in range(2)]

        def stA(e):
            for tt in range(NT):
                q = tt * NE + e
                V(lambda tt=tt, q=q: nc.vector.tensor_scalar(out=oh[:, tt, :], in0=iot, scalar1=pos[:, q:q + 1], scalar2=sel[:, q:q + 1],
                                                           op0=ALU.is_equal, op1=ALU.mult),
                  [R("iot"), R("pos3"), R("sel3")], [R("oh")])

        def stB(e):
            ep = e % 2
            for jt in range(JT):
                p = nxt("ps", 6)

                def mmg(p=p, jt=jt):
                    ins = None
                    for tt in range(NT):
                        ins = nc.tensor.matmul(ps[p][:, 0:4], lhsT=oh[:, tt, jt * 128:(jt + 1) * 128], rhs=ahl[:, tt * NE + e, :],
                                               start=(tt == 0), stop=(tt == NT - 1))
                    return ins
                T(mmg, [R("oh"), R("ahl3")], [R("ps", p)])
                V(lambda p=p: nc.vector.tensor_copy(out=gtmp, in_=ps[p][:, 0:4]), [R("ps", p)], [R("gtmp")])
                V(lambda jt=jt, ep=ep: nc.vector.tensor_tensor(out=gl[:, ep, jt:jt + 1], in0=gtmp[:, 0:1], in1=gtmp[:, 1:2], op=ALU.add), [R("gtmp")], [R("gl", ep)])
                V(lambda: nc.vector.scalar_tensor_tensor(out=idxf[:, 0:1], in0=gtmp[:, 2:3], scalar=128.0, in1=gtmp[:, 3:4], op0=ALU.mult, op1=ALU.add),
                  [R("gtmp")], [R("idxf")])
                for c_ in range(NCH):
                    V(lambda c_=c_: nc.vector.tensor_scalar(out=idxc[:, c_:c_ + 1], in0=idxf[:, 0:1], scalar1=float(NCH), scalar2=float(c_),
                                                          op0=ALU.mult, op1=ALU.add), [R("idxf")], [R("idxc")])
                io = (ep * JT + jt) * NCH
                V(lambda io=io: nc.vector.tensor_copy(out=idxi[:, io:io + NCH], in_=idxc), [R("idxc")], [R("idxi", ep, jt)])

        def stC(e):
            for jt in range(JT):
                si = nxt("stg", 1)
                for t0 in range(0, NT, 8):
                    b = 6 + nxt("psb", 2)
                    pbv = ps[b][:, :].bitcast(BF16)
                    nk = min(8, NT - t0)

                    def tro(pbv=pbv, t0=t0, nk=nk, jt=jt):
                        ins = None
                        for k in range(nk):
                            ins = nc.tensor.transpose(pbv[:, k * 128:(k + 1) * 128], oh[:, t0 + k, jt * 128:(jt + 1) * 128], ident_b)
                        return ins
                    T(tro, [R("oh"), R("c")], [R("ps", b)])
                    V(lambda pbv=pbv, t0=t0, nk=nk, si=si: nc.vector.tensor_copy(out=stg[si][:, t0 * 128:(t0 + nk) * 128], in_=pbv[:, 0:nk * 128], saturate=False),
                      [R("ps", b)], [R("stg", si)])
                qq = e * JT + jt
                DMA("sp", OHT[:, :, qq * 128:(qq + 1) * 128].rearrange("t p c -> p t c"), stg[si].rearrange("p (t c) -> p t c", c=128),
                    [R("stg", si)], [R("ohtd")], "st")

        def stD(e):
            ep = e % 2
            for jt in range(JT):
                for c_ in range(NCH):
                    io = (ep * JT + jt) * NCH + c_
                    P.op("pool", lambda io=io, c_=c_: nc.gpsimd.indirect_dma_start(out=xg[:, c_ * GCH:(c_ + 1) * GCH], out_offset=None, in_=H3v[:, :],
                                                                                 in_offset=bass.IndirectOffsetOnAxis(ap=idxi[:, io:io + 1], axis=0),
                                                                                 bounds_check=None, oob_is_err=True),
                         reads=[R("idxi", ep, jt), R("h3d")], writes=[R("xg")], stream="g")
                for k0 in range(0, KT, 8):
                    b2 = 6 + nxt("psb", 2)
                    pbv2 = ps[b2][:, :].bitcast(BF16)
                    nk2 = min(8, KT - k0)

                    def trg(pbv2=pbv2, k0=k0, nk2=nk2):
                        ins = None
                        for k in range(nk2):
                            ins = nc.tensor.transpose(pbv2[:, k * 128:(k + 1) * 128], xg[:, (k0 + k) * 128:(k0 + k + 1) * 128], ident_b)
                        return ins
                    T(trg, [R("xg"), R("c")], [R("ps", b2)])
                    V(lambda pbv2=pbv2, k0=k0, nk2=nk2, jt=jt: nc.vector.tensor_copy(out=xsT[:, k0:k0 + nk2, jt * 128:(jt + 1) * 128],
                                                                                   in_=pbv2[:, 0:nk2 * 128].rearrange("p (a b) -> p a b", a=nk2)),
                      [R("ps", b2)], [R("xsT")])

        def stE(e, mid=None):
            wgu = w_gate_up[l, e]
            for f2 in range(DE // 256):
                if f2 == 1 and mid is not None:
                    mid()
                    mid = None
                wi = nxt("w", 2)
                DMA("pool", wsl[wi][:, :, 0:256], wgu[:, f2 * 256:(f2 + 1) * 256].rearrange("(kt p) c -> p kt c", p=128), [], [R("wsl", wi)], "w")
                DMA("pool", wsl[wi][:, :, 256:512], wgu[:, DE + f2 * 256:DE + (f2 + 1) * 256].rearrange("(kt p) c -> p kt c", p=128), [], [R("wsl", wi)], "w")
                for sub in range(2):
                    pg = nxt("ps", 6)
                    pu = nxt("ps", 6)
                    T(mm_feat(ps[pg][:, 0:CAP], xsT, 0, CAP, wsl[wi], sub * 128, KT), [R("xsT"), R("wsl", wi)], [R("ps", pg)])
                    T(mm_feat(ps[pu][:, 0:CAP], xsT, 0, CAP, wsl[wi], 256 + sub * 128, KT), [R("xsT"), R("wsl", wi)], [R("ps", pu)])
                    gi_ = nxt("sg", 1)
                    A(lambda pg=pg, gi_=gi_: nc.scalar.activation(out=sg[gi_], in_=ps[pg][:, 0:CAP], func=AF.Silu), [R("ps", pg)], [R("sg", gi_)])
                    fi = f2 * 2 + sub
                    V(lambda pu=pu, gi_=gi_, fi=fi: nc.vector.tensor_tensor(out=actT[:, fi, :], in0=sg[gi_], in1=ps[pu][:, 0:CAP], op=ALU.mult),
                      [R("sg", gi_), R("ps", pu)], [R("actT")])
            if mid is not None:
                mid()

        def stF(e):
            ep = e % 2
            for dblk in range(D // SW):
                wi = load_w(wsl, w_down[l, e], dblk * SW, SW, FT)
                for jt in range(JT):
                    p = nxt("ps", 6)
                    T(mm_tok(ps[p][:, 0:SW], actT, jt * 128, wsl[wi], 0, SW, FT), [R("actT"), R("wsl", wi)], [R("ps", p)])
                    yi = nxt("ygs", 2)
                    V(lambda p=p, yi=yi, jt=jt: nc.vector.tensor_scalar(out=ygs[yi], in0=ps[p][:, 0:SW], scalar1=gl[:, ep, jt:jt + 1], scalar2=None, op0=ALU.mult),
                      [R("ps", p), R("gl", ep)], [R("ygs", yi)])
                    DMA("sp", YG[e * CAP + jt * 128:e * CAP + (jt + 1) * 128, dblk * SW:(dblk + 1) * SW], ygs[yi], [R("ygs", yi)], [R("ygd")], "st")

        stA(0); stB(0); stC(0); stD(0)
        for e in range(NE):
            if e + 1 < NE:
                stE(e, mid=lambda e=e: stA(e + 1))
                stB(e + 1)
            else:
                stE(e)
            stF(e)
            if e + 1 < NE:
                stC(e + 1)
                stD(e + 1)
        if DBG.get('mph', 99) <= 2:
            return

        def _m4():
            phase()
            ygs = al([NQ, SW], BF16)
            oht = [al([NQ, 128], FP8) for _ in range(2)]
            ev = [al([SW]) for _ in range(3)]
            for dblk in range(D // SW):
                DMA("sp", ygs, YG[:, dblk * SW:(dblk + 1) * SW].rearrange("(q p) c -> p q c", p=128), [R("ygd")], [R("ygs4")], "ld")
                for tt in range(NT):
                    oi = nxt("oht", 2)
                    DMA("sp", oht[oi], OHT[tt].rearrange("p (q t) -> p q t", t=128), [R("ohtd")], [R("oht", oi)], "ld")
                    p = nxt("ps", 6)

                    def mmc(p=p, oi=oi):
                        ins = None
                        for q in range(NQ):
                            ins = nc.tensor.matmul(ps[p][:, 0:SW], lhsT=oht[oi][:, q, :], rhs=ygs[:, q, :], start=(q == 0), stop=(q == NQ - 1))
                        return ins
                    T(mmc, [R("oht", oi), R("ygs4")], [R("ps", p)])
                    ei = nxt("ev", 3)
                    DMA("sp", ev[ei], xres[tt * 128:(tt + 1) * 128, dblk * SW:(dblk + 1) * SW], [R("xres", tt)], [R("ev", ei)], "ld")
                    V(lambda p=p, ei=ei: nc.vector.tensor_tensor(out=ev[ei], in0=ps[p][:, 0:SW], in1=ev[ei], op=ALU.add),
                      [R("ps", p), R("ev", ei)], [R("ev", ei)])
                    DMA("act", xres[tt * 128:(tt + 1) * 128, dblk * SW:(dblk + 1) * SW], ev[ei], [R("ev", ei)], [R("xres", tt)], "st")
        _m4()


    for tt in range(NT):
        DMA("sp", xres[tt * 128:(tt + 1) * 128, :], x_in[tt * 128:(tt + 1) * 128, :], [], [R("xres", tt)], "cp")

    for l in range(nlayers):
        if nstage >= 1:
            mixer(l)
        if nstage >= 2:
            xattn(l)
        if nstage >= 3:
            moe(l)
    phase()
    gfin = al([D])
    xt2 = [al([D]) for _ in range(2)]
    xs2 = [al([D], BF16) for _ in range(2)]
    DMA("sp", gfin, gfin_d[0:1, :].partition_broadcast(128), [], [R("gfin")], "c")
    for tt in range(NT):
        i = nxt("xt", 2)
        DMA("sp", xt2[i], xres[tt * 128:(tt + 1) * 128, :], [R("xres", tt)], [R("xt", i)], "ld")
        sumsq(xt2[i], xs2[i], 0, R("xt", i), R("xs", i))
        rstd_from_ss(0, 1, 1, D)
        V(lambda i=i: nc.vector.scalar_tensor_tensor(out=xt2[i], in0=xt2[i], scalar=st[:, 1:2], in1=gfin, op0=ALU.mult, op1=ALU.mult),
          [R("xt", i), R("st"), R("gfin")], [R("xt", i)])
        DMA("sp", out[tt * 128:(tt + 1) * 128, :], xt2[i], [R("xt", i)], [R("out")], "out")
    P.wait_all("sp", [R("out")])
    P.run()
    es.close()
    P.close()
    return nc


def host_consts():
    i = np.arange(128)
    ident = np.eye(128, dtype=np.float32)
    Tf = (i[:, None] <= i[None, :]).astype(np.float32)
    Uf = (i[:, None] > i[None, :]).astype(np.float32)
    Tb = (i[:, None] >= i[None, :]).astype(np.float32)
    Ub = (i[:, None] < i[None, :]).astype(np.float32)
    ones = np.ones((128, 128), np.float32)
    return np.concatenate([ident, Tf, Uf, Tb, Ub, ones], axis=1)


def host_layout(C, inp, b):
    L, KT = C.L, C.KT
    f = lambda a: np.ascontiguousarray(a, dtype=np.float32)
    gl = []
    for l in range(L):
        for n in ("norm_mix_g", "norm_xattn_g", "norm_mem_g", "norm_moe_g"):
            gl.append(inp[n][l].reshape(KT, 128).T)
    gl.append(inp["final_norm_g"].reshape(KT, 128).T)
    m = {
        "x": f(inp["x"][b]),
        "gk": f(np.concatenate(gl, axis=1)),
        "consts": host_consts(),
        "gfin_d": f(inp["final_norm_g"].reshape(1, -1)),
        "w_in": f(inp["w_in"]), "w_out": f(inp["w_out"]),
        "convp": f(np.concatenate([np.concatenate([inp["conv_w"][l].T, inp["conv_b"][l][:, None]], axis=1)
                                   .reshape(C.CC // 128, 128, 6).transpose(1, 0, 2).reshape(128, -1) for l in range(L)], axis=1)),
        "rowp": f(np.stack([np.concatenate([inp["dt_bias"][l].reshape(-1), inp["a_log"][l].reshape(-1), np.repeat(inp["d_skip"][l], 64),
                                            inp["ssd_norm_g"][l], inp["gmlp_norm_g"][l]]) for l in range(L)], axis=0)),
        "gws": f(np.stack([inp["gmlp_ws"][l].transpose(2, 0, 1).reshape(128, -1) for l in range(L)], axis=0)),
        "gbs": f(inp["gmlp_bs"].reshape(L, -1)),
        "gmoe_row": f(inp["norm_moe_g"]), "w_router": f(inp["w_router"]), "w_gate_up": f(inp["w_gate_up"]), "w_down": f(inp["w_down"]),
        "iota_in": np.ascontiguousarray(np.broadcast_to(np.arange(C.CAP, dtype=np.float32)[None, :], (128, C.CAP))),
        "pidx_in": np.arange(128, dtype=np.float32).reshape(128, 1),
        "mem": f(inp["mem"][b]), "w_q": f(inp["w_q"]), "w_kv": f(inp["w_kv"]), "w_o": f(inp["w_o"]),
        "w_in": f(inp["w_in"]),
        "w_out": f(inp["w_out"]),
    }
    return m


_NC_CACHE = {}


def run(C, inputs, n_cores=2, stop_after=None):
    key = (id(C), stop_after)
    if key not in _NC_CACHE:
        _NC_CACHE[key] = build(C, stop_after)
    nc = _NC_CACHE[key]
    used = set()
    for alloc in nc.allocations:
        try:
            if alloc.kind == "ExternalInput":
                used.add(alloc.memorylocations[0].name)
        except Exception:
            pass
    maps = []
    for b in range(n_cores):
        m = host_layout(C, inputs, b)
        maps.append({k: v for k, v in m.items() if (not used) or k in used})
    res = run_bass_kernel_spmd(nc, maps, core_ids=list(range(n_cores)))
    LAST["res"] = res.results
    return np.stack([res.results[b]["out"] for b in range(n_cores)], axis=0)


def kernel(**inputs):
    inputs = {k: np.asarray(v) for k, v in inputs.items()}
    return run(FULL, inputs, n_cores=2).astype(np.float32)
```
